# Optimizing a Trainium2 kernel written in Bass

```python
import math
import jax
import jax.numpy as jnp
from jax import lax
import numpy as np

D_MODEL = 1024
BATCH = 4
SEQ = 8192
DEPTH = 2

GRID_W = 64
CTX_LEN = 256

N_MOD = 6
NORM_EPS = 1e-6
LN_EPS = 1e-5

A_WIDTH = 3 * D_MODEL // 4
A_HEAD_DIM = 64
A_HEADS = A_WIDTH // A_HEAD_DIM
A_RANK_W = 64
A_RANK_A = 64
A_RANK_G = 128
A_LNX_EPS = 64e-5
A_COLS = 3 * A_WIDTH + 2 * A_RANK_W + 2 * A_RANK_A + A_RANK_G
A_SPLITS = (A_WIDTH, 2 * A_WIDTH, 3 * A_WIDTH, 3 * A_WIDTH + 2 * A_RANK_W,
            3 * A_WIDTH + 2 * A_RANK_W + 2 * A_RANK_A)

B_WIDTH = D_MODEL - A_WIDTH
B_GROUPS = 4
B_GROUP_DIM = B_WIDTH // B_GROUPS
EVEN_IN_COLS = A_COLS + B_WIDTH

C_WIDTH = 3 * D_MODEL // 4
C_HEAD_DIM = 64
C_HEADS = C_WIDTH // C_HEAD_DIM
C_KV_HEADS = 4
C_GROUP = C_HEADS // C_KV_HEADS
KV_WIDTH = C_KV_HEADS * C_HEAD_DIM
ROPE_AXIS_DIM = C_HEAD_DIM // 2
ROPE_THETA = 10000.0
Q_BLOCK = 128
ATTN_SCALE = C_HEAD_DIM ** -0.5

D_WIDTH = D_MODEL - C_WIDTH
D_CONV_WIDTH = 31
D_PAD = D_CONV_WIDTH // 2
ODD_SPLITS = (C_WIDTH, C_WIDTH + KV_WIDTH, C_WIDTH + 2 * KV_WIDTH)
ODD_IN_COLS = C_WIDTH + 2 * KV_WIDTH + 2 * D_WIDTH

PK_HEADS = 8
PK_DIM = 256
PK_HALF = PK_DIM // 2
N_KEYS = 128
N_EXPERTS = N_KEYS * N_KEYS
PK_TOPK = 16
PEER_CHUNK = 256

kernel_name = "hybrid_rwkv7_fnet_gqa_conformer_peer_dit"


def rms_norm(x, g):
    xf = x.astype(jnp.float32)
    y = xf * lax.rsqrt(jnp.mean(xf * xf, axis=-1, keepdims=True) + NORM_EPS)
    return (y * g.astype(jnp.float32)).astype(x.dtype)


def layer_norm(x, g, b):
    xf = x.astype(jnp.float32)
    mu = jnp.mean(xf, axis=-1, keepdims=True)
    var = jnp.mean(jnp.square(xf - mu), axis=-1, keepdims=True)
    return ((xf - mu) * lax.rsqrt(var + LN_EPS) * g + b).astype(x.dtype)


def centred_shift(z, mu_prev, mu_next):
    z_prev = jnp.pad(z[:, :-1], ((0, 0), (1, 0), (0, 0)))
    z_next = jnp.pad(z[:, 1:], ((0, 0), (0, 1), (0, 0)))
    return z + mu_prev * (z_prev - z) + mu_next * (z_next - z)


def wkv7_scan(r, decay, k, v, a, b, s0, reverse):
    def step(state, inp):
        r_t, w_t, k_t, v_t, a_t, b_t = inp
        sa = jnp.einsum("bhvk,bhk->bhv", state, a_t)
        state = (state * w_t[:, :, None, :] + sa[..., None] * b_t[:, :, None, :]
                 + v_t[..., None] * k_t[:, :, None, :])
        return state, jnp.einsum("bhvk,bhk->bhv", state, r_t)
    xs = tuple(jnp.swapaxes(t, 0, 1) for t in (r, decay, k, v, a, b))
    state, y = lax.scan(step, s0, xs, reverse=reverse)
    return jnp.swapaxes(y, 0, 1), state


def rwkv7_terms(zr, w0, w2, a0, a2, g2, k_k, k_a, r_k):
    zr = zr.astype(jnp.float32)
    Bn, T, _ = zr.shape
    hd = lambda t: t.reshape(Bn, T, A_HEADS, A_HEAD_DIM)
    r, k, v, xw, xa, xg = jnp.split(zr, A_SPLITS, axis=-1)
    xw = xw.reshape(Bn, T, 2, A_RANK_W)
    xa = xa.reshape(Bn, T, 2, A_RANK_A)
    g = jax.nn.sigmoid(xg) @ g2.astype(jnp.float32)
    kk = hd(k * k_k)
    kk = kk / jnp.maximum(jnp.sqrt(jnp.sum(kk * kk, axis=-1, keepdims=True)), 1e-12)
    rh, vh = hd(r), hd(v)
    dirs = []
    bonus = 0.0
    for d in range(2):
        w_log = -jax.nn.softplus(-(w0[d] + jnp.tanh(xw[:, :, d]) @ w2[d])) - 0.5
        decay = jnp.exp(-jnp.exp(w_log))
        a_gate = jax.nn.sigmoid(a0[d] + xa[:, :, d] @ a2[d])
        k_d = hd(k * (1.0 + (a_gate - 1.0) * k_a))
        dirs.append((hd(decay), k_d, -kk, kk * hd(a_gate)))
        bonus = bonus + jnp.sum(rh * k_d * r_k, axis=-1, keepdims=True) * vh
    return rh, vh, g, dirs, bonus.reshape(Bn, T, A_WIDTH)


def head_group_norm(y, g, b):
    mu = jnp.mean(y, axis=-1, keepdims=True)
    var = jnp.mean(jnp.square(y - mu), axis=-1, keepdims=True)
    yn = (y - mu) * lax.rsqrt(var + A_LNX_EPS)
    Bn, T = y.shape[:2]
    return yn.reshape(Bn, T, A_WIDTH) * g + b


def fourier_mix(f):
    Bn, T, _ = f.shape
    fg = f.reshape(Bn, T, B_GROUPS, B_GROUP_DIM).astype(jnp.float32)
    y = jnp.fft.fftn(fg, axes=(1, 3), norm="ortho").real
    return y.reshape(Bn, T, B_WIDTH).astype(f.dtype)


def rwkv_fourier_mixer(h, hc, w_in, shift_prev, shift_next, w0, w2, a0, a2, g2,
                       k_k, k_a, r_k, lnx_g, lnx_b, w_out, need_ctx):
    def project(hs):
        z = hs @ w_in
        return centred_shift(z[..., :A_COLS], shift_prev, shift_next), z[..., A_COLS:]
    zc, fc = project(hc)
    zl, fl = project(h)
    rc, vc, gc, dirs_c, bonus_c = rwkv7_terms(zc, w0, w2, a0, a2, g2, k_k, k_a, r_k)
    rl, vl, gl, dirs_l, bonus_l = rwkv7_terms(zl, w0, w2, a0, a2, g2, k_k, k_a, r_k)
    s0 = jnp.zeros((h.shape[0], A_HEADS, A_HEAD_DIM, A_HEAD_DIM), jnp.float32)
    y_ctx = 0.0
    y_lat = 0.0
    for d, reverse in enumerate((False, True)):
        dec_c, k_c, a_c, b_c = dirs_c[d]
        dec_l, k_l, a_l, b_l = dirs_l[d]
        yc, s_ctx = wkv7_scan(rc, dec_c, k_c, vc, a_c, b_c, s0, reverse)
        yl, _ = wkv7_scan(rl, dec_l, k_l, vl, a_l, b_l, s_ctx, reverse)
        y_ctx = y_ctx + yc
        y_lat = y_lat + yl

    def finish(y, bonus, g, f):
        o = (head_group_norm(y, lnx_g, lnx_b) + bonus) * g
        return jnp.concatenate([o.astype(f.dtype), fourier_mix(f)], axis=-1) @ w_out

    out = finish(y_lat, bonus_l, gl, fl)
    if not need_ctx:
        return out, None
    return out, finish(y_ctx, bonus_c, gc, fc)


def axial_rope(n):
    rows = n // GRID_W
    row = jnp.broadcast_to(jnp.arange(rows)[:, None], (rows, GRID_W)).reshape(-1)
    col = jnp.broadcast_to(jnp.arange(GRID_W)[None, :], (rows, GRID_W)).reshape(-1)
    inv = ROPE_THETA ** (-jnp.arange(0, ROPE_AXIS_DIM, 2, dtype=jnp.float32) / ROPE_AXIS_DIM)
    ang = jnp.stack([row[:, None] * inv, col[:, None] * inv], axis=1)
    return jnp.cos(ang), jnp.sin(ang)


def apply_rope(x, cos, sin):
    Bn, T, H, _ = x.shape
    xr = x.astype(jnp.float32).reshape(Bn, T, H, 2, 2, ROPE_AXIS_DIM // 2)
    x1, x2 = xr[..., 0, :], xr[..., 1, :]
    c = cos[None, :, None]
    s = sin[None, :, None]
    out = jnp.stack([x1 * c - x2 * s, x2 * c + x1 * s], axis=-2)
    return out.reshape(Bn, T, H, C_HEAD_DIM).astype(x.dtype)


def attend(q, k, v):
    Bn, Tq, _, _ = q.shape
    nb = Tq // Q_BLOCK
    qb = q.reshape(Bn, nb, Q_BLOCK, C_KV_HEADS, C_GROUP, C_HEAD_DIM).transpose(1, 0, 2, 3, 4, 5)

    def block(qblk):
        s = jnp.einsum("bqkgd,blkd->bkgql", qblk, k).astype(jnp.float32) * ATTN_SCALE
        p = jax.nn.softmax(s, axis=-1).astype(v.dtype)
        return jnp.einsum("bkgql,blkd->bqkgd", p, v)

    o = lax.map(block, qb)
    return o.transpose(1, 0, 2, 3, 4, 5).reshape(Bn, Tq, C_WIDTH)


def conformer_conv(u, dw_w, dw_b, cn_g, cn_b):
    val, gate = jnp.split(u, 2, axis=-1)
    y = val * jax.nn.sigmoid(gate)
    y = lax.conv_general_dilated(y, dw_w[:, None, :], window_strides=(1,), padding=[(D_PAD, D_PAD)],
                                 dimension_numbers=("NWC", "WIO", "NWC"),
                                 feature_group_count=D_WIDTH) + dw_b
    return jax.nn.silu(layer_norm(y, cn_g, cn_b))


def attention_conv_mixer(h, hc, w_in, q_norm, k_norm, dw_w, dw_b, cn_g, cn_b, w_out, need_ctx):
    Bn, S, _ = h.shape
    CL = hc.shape[1]
    q, k, v, u = jnp.split(h @ w_in, ODD_SPLITS, axis=-1)
    q = rms_norm(q.reshape(Bn, S, C_HEADS, C_HEAD_DIM), q_norm)
    k = rms_norm(k.reshape(Bn, S, C_KV_HEADS, C_HEAD_DIM), k_norm)
    cos, sin = axial_rope(S)
    q = apply_rope(q, cos, sin)
    k = apply_rope(k, cos, sin)
    v = v.reshape(Bn, S, C_KV_HEADS, C_HEAD_DIM)
    kc, vc = jnp.split(hc @ w_in[:, C_WIDTH:C_WIDTH + 2 * KV_WIDTH], 2, axis=-1)
    kc = rms_norm(kc.reshape(Bn, CL, C_KV_HEADS, C_HEAD_DIM), k_norm)
    vc = vc.reshape(Bn, CL, C_KV_HEADS, C_HEAD_DIM)
    k_all = jnp.concatenate([k, kc], axis=1)
    v_all = jnp.concatenate([v, vc], axis=1)
    out = jnp.concatenate([attend(q, k_all, v_all),
                           conformer_conv(u, dw_w, dw_b, cn_g, cn_b)], axis=-1) @ w_out
    if not need_ctx:
        return out, None
    qc = rms_norm((hc @ w_in[:, :C_WIDTH]).reshape(Bn, CL, C_HEADS, C_HEAD_DIM), q_norm)
    uc = hc @ w_in[:, ODD_SPLITS[2]:]
    out_c = jnp.concatenate([attend(qc, kc, vc),
                             conformer_conv(uc, dw_w, dw_b, cn_g, cn_b)], axis=-1) @ w_out
    return out, out_c


def peer_ffn(h, pq, sk1, sk2, pu, pv):
    shape = h.shape
    D = shape[-1]
    t = h.reshape(-1, D)
    n = t.shape[0]
    t = jnp.pad(t, ((0, (-n) % PEER_CHUNK), (0, 0))).reshape(-1, PEER_CHUNK, D)

    def chunk(tc):
        q = (tc @ pq).reshape(PEER_CHUNK, PK_HEADS, 2, PK_HALF).astype(jnp.float32)
        s1 = jnp.einsum("thd,hnd->thn", q[:, :, 0], sk1.astype(jnp.float32))
        s2 = jnp.einsum("thd,hnd->thn", q[:, :, 1], sk2.astype(jnp.float32))
        v1, i1 = lax.top_k(s1, PK_TOPK)
        v2, i2 = lax.top_k(s2, PK_TOPK)
        cand = (v1[..., :, None] + v2[..., None, :]).reshape(PEER_CHUNK, PK_HEADS, PK_TOPK * PK_TOPK)
        score, ci = lax.top_k(cand, PK_TOPK)
        e1 = jnp.take_along_axis(i1, ci // PK_TOPK, axis=-1)
        e2 = jnp.take_along_axis(i2, ci % PK_TOPK, axis=-1)
        expert = e1 * N_KEYS + e2
        gate = jax.nn.softmax(score, axis=-1)
        act = jax.nn.gelu(jnp.einsum("thkd,td->thk", pu[expert], tc).astype(jnp.float32),
                          approximate=False) * gate
        return jnp.einsum("thk,thkd->td", act.astype(pv.dtype), pv[expert])

    out = lax.map(chunk, t).reshape(-1, D)[:n]
    return out.reshape(shape).astype(h.dtype)


def trunk_layer(x, ctx, c, c_ctx, mixer, mod_w, mod_b, norm1, mix_p, norm2, peer_p, last):
    sh1, sc1, g1, sh2, sc2, g2 = jnp.split((jax.nn.silu(c) @ mod_w + mod_b)[:, None, :], N_MOD, axis=-1)
    csh1, csc1, cg1, csh2, csc2, cg2 = jnp.split(jax.nn.silu(c_ctx) @ mod_w + mod_b, N_MOD, axis=-1)
    h = rms_norm(x, norm1) * (1.0 + sc1) + sh1
    hc = rms_norm(ctx, norm1) * (1.0 + csc1) + csh1
    mix, mix_c = mixer(h, hc, *mix_p, need_ctx=not last)
    x = x + g1 * mix
    x = x + g2 * peer_ffn(rms_norm(x, norm2) * (1.0 + sc2) + sh2, *peer_p)
    if last:
        return x, None
    ctx = ctx + cg1 * mix_c
    ctx = ctx + cg2 * peer_ffn(rms_norm(ctx, norm2) * (1.0 + csc2) + csh2, *peer_p)
    return x, ctx


def setup_inputs(seed: int = 0) -> dict:
    key = jax.random.key(seed)
    keys = iter(jax.random.split(key, 64))
    nrm = lambda shape, s: s * jax.random.normal(next(keys), shape, jnp.float32)
    uni = lambda shape, lo, hi: jax.random.uniform(next(keys), shape, jnp.float32, lo, hi)
    D = D_MODEL
    fan = D ** -0.5
    return {
        "x": nrm((BATCH, SEQ, D), 1.0),
        "c": nrm((BATCH, D), 1.0),
        "ctx": nrm((BATCH, CTX_LEN, D), 1.0),
        "c_ctx": nrm((D,), 1.0),
        "l0_mod_w": nrm((D, N_MOD * D), 0.5 * fan),
        "l0_mod_b": nrm((N_MOD * D,), 0.02),
        "l0_norm1": 1.0 + nrm((D,), 0.02),
        "l0_w_in": nrm((D, EVEN_IN_COLS), fan),
        "l0_shift_prev": uni((A_COLS,), 0.0, 0.5),
        "l0_shift_next": uni((A_COLS,), 0.0, 0.5),
        "l0_w0": uni((2, A_WIDTH), -4.0, 0.0),
        "l0_w2": nrm((2, A_RANK_W, A_WIDTH), 0.1 * A_RANK_W ** -0.5),
        "l0_a0": nrm((2, A_WIDTH), 0.1),
        "l0_a2": nrm((2, A_RANK_A, A_WIDTH), 0.3 * A_RANK_A ** -0.5),
        "l0_g2": nrm((A_RANK_G, A_WIDTH), A_RANK_G ** -0.5),
        "l0_k_k": 0.85 + nrm((A_WIDTH,), 0.05),
        "l0_k_a": 1.0 + nrm((A_WIDTH,), 0.05),
        "l0_r_k": nrm((A_HEADS, A_HEAD_DIM), 0.1),
        "l0_lnx_g": 1.0 + nrm((A_WIDTH,), 0.02),
        "l0_lnx_b": nrm((A_WIDTH,), 0.02),
        "l0_w_out": nrm((A_WIDTH + B_WIDTH, D), fan),
        "l0_norm2": 1.0 + nrm((D,), 0.02),
        "l0_pq": nrm((D, PK_HEADS * PK_DIM), fan),
        "l0_sk1": nrm((PK_HEADS, N_KEYS, PK_HALF), PK_HALF ** -0.5),
        "l0_sk2": nrm((PK_HEADS, N_KEYS, PK_HALF), PK_HALF ** -0.5),
        "l0_pu": nrm((N_EXPERTS, D), fan),
        "l0_pv": nrm((N_EXPERTS, D), 0.5),
        "l1_mod_w": nrm((D, N_MOD * D), 0.5 * fan),
        "l1_mod_b": nrm((N_MOD * D,), 0.02),
        "l1_norm1": 1.0 + nrm((D,), 0.02),
        "l1_w_in": nrm((D, ODD_IN_COLS), fan),
        "l1_q_norm": 1.0 + nrm((C_HEAD_DIM,), 0.02),
        "l1_k_norm": 1.0 + nrm((C_HEAD_DIM,), 0.02),
        "l1_dw_w": nrm((D_CONV_WIDTH, D_WIDTH), D_CONV_WIDTH ** -0.5),
        "l1_dw_b": nrm((D_WIDTH,), 0.02),
        "l1_cn_g": 1.0 + nrm((D_WIDTH,), 0.02),
        "l1_cn_b": nrm((D_WIDTH,), 0.02),
        "l1_w_out": nrm((C_WIDTH + D_WIDTH, D), fan),
        "l1_norm2": 1.0 + nrm((D,), 0.02),
        "l1_pq": nrm((D, PK_HEADS * PK_DIM), fan),
        "l1_sk1": nrm((PK_HEADS, N_KEYS, PK_HALF), PK_HALF ** -0.5),
        "l1_sk2": nrm((PK_HEADS, N_KEYS, PK_HALF), PK_HALF ** -0.5),
        "l1_pu": nrm((N_EXPERTS, D), fan),
        "l1_pv": nrm((N_EXPERTS, D), 0.5),
        "norm_f": 1.0 + nrm((D,), 0.02),
    }


def reference(x, c, ctx, c_ctx,
              l0_mod_w, l0_mod_b, l0_norm1, l0_w_in, l0_shift_prev, l0_shift_next,
              l0_w0, l0_w2, l0_a0, l0_a2, l0_g2, l0_k_k, l0_k_a, l0_r_k, l0_lnx_g, l0_lnx_b,
              l0_w_out, l0_norm2, l0_pq, l0_sk1, l0_sk2, l0_pu, l0_pv,
              l1_mod_w, l1_mod_b, l1_norm1, l1_w_in, l1_q_norm, l1_k_norm, l1_dw_w, l1_dw_b,
              l1_cn_g, l1_cn_b, l1_w_out, l1_norm2, l1_pq, l1_sk1, l1_sk2, l1_pu, l1_pv,
              norm_f):
    layers = (
        (rwkv_fourier_mixer, l0_mod_w, l0_mod_b, l0_norm1,
         (l0_w_in, l0_shift_prev, l0_shift_next, l0_w0, l0_w2, l0_a0, l0_a2, l0_g2,
          l0_k_k, l0_k_a, l0_r_k, l0_lnx_g, l0_lnx_b, l0_w_out),
         l0_norm2, (l0_pq, l0_sk1, l0_sk2, l0_pu, l0_pv)),
        (attention_conv_mixer, l1_mod_w, l1_mod_b, l1_norm1,
         (l1_w_in, l1_q_norm, l1_k_norm, l1_dw_w, l1_dw_b, l1_cn_g, l1_cn_b, l1_w_out),
         l1_norm2, (l1_pq, l1_sk1, l1_sk2, l1_pu, l1_pv)),
    )
    for i in range(DEPTH):
        mixer, mod_w, mod_b, norm1, mix_p, norm2, peer_p = layers[i]
        x, ctx = trunk_layer(x, ctx, c, c_ctx, mixer, mod_w, mod_b, norm1, mix_p, norm2, peer_p,
                             i == DEPTH - 1)
    return rms_norm(x, norm_f)
```

```python
import numpy as np
import concourse.bass as bass
import concourse.mybir as mybir
from concourse.bass_utils import run_bass_kernel_spmd

F32 = mybir.dt.float32
BF16 = mybir.dt.bfloat16
I32 = mybir.dt.int32
U32 = mybir.dt.uint32
AF = mybir.ActivationFunctionType
ALU = mybir.AluOpType
AX = mybir.AxisListType


class T:
    def __init__(self, P, ap, name):
        self.P = P
        self.ap = ap
        self.name = name
        self.w = None
        self.r = {}
        self.dsem = None
        self.dcnt = 0
        self.psum = False

    def __getitem__(self, idx):
        return V(self, self.ap[idx])

    def sub(self, idx, tag):
        t = T(self.P, self.ap[idx], self.name + "_" + str(tag))
        t.psum = self.psum
        return t


class V:
    def __init__(self, t, ap):
        self.t = t
        self.ap = ap

    def __getitem__(self, idx):
        return V(self.t, self.ap[idx])


class Prog:
    ENG = ("pe", "dve", "act", "pool", "sp")

    def __init__(self, strict=True):
        self.nc = bass.Bass("TRN2", target_bir_lowering=False)
        nc = self.nc
        self.e = {"pe": nc.tensor, "dve": nc.vector, "act": nc.scalar, "pool": nc.gpsimd, "sp": nc.sync}
        self.sem = {}
        self.cnt = {}
        self.seen = {k: {} for k in self.ENG}
        self.strict = strict
        self._ctx = []
        self._sctx = []
        for k in ("pe", "dve", "act", "pool"):
            self.sem[k] = self._senter(nc.semaphore("sem_" + k))
            self.cnt[k] = 0
        self.n_ins = 0
        self.out_tiles = []
        self._dcnt = {}

    def _enter(self, cm):
        v = cm.__enter__()
        self._ctx.append(cm)
        return v

    def _senter(self, cm):
        v = cm.__enter__()
        self._sctx.append(cm)
        return v

    def sb(self, name, shape, dt=F32):
        h = self._enter(self.nc.sbuf_tensor(name, list(shape), dt))
        return T(self, h[:], name)

    def ps(self, name, shape, dt=F32):
        h = self._enter(self.nc.psum_tensor(name, list(shape), dt))
        t = T(self, h[:], name)
        t.psum = True
        return t

    def dram(self, name, shape, dt=F32, kind="Internal"):
        h = self.nc.dram_tensor(name, list(shape), dt, kind=kind)
        t = T(self, h.ap(), name)
        if kind == "ExternalOutput":
            self.out_tiles.append(t)
        return t

    def _dsem(self, t):
        if t.dsem is None:
            t.dsem = self._senter(self.nc.semaphore("ds_" + t.name))
            self.sem[("d", t.name)] = t.dsem
        return t.dsem

    def _wait(self, eng, key, val, skip_self=False):
        if key == eng and (skip_self or not self.strict):
            return
        if self.seen[eng].get(key, 0) >= val:
            return
        self.seen[eng][key] = val
        self.e[eng].wait_ge(self.sem[key], val)

    def _deps(self, eng, reads, writes, acc=False):
        for v in reads:
            t = v.t
            if t.w is not None:
                self._wait(eng, *t.w)
            if t.psum:
                for k, c in t.r.items():
                    if k != eng:
                        self._wait(eng, k, c)
        for v in writes:
            t = v.t
            if t.w is not None:
                self._wait(eng, *t.w, skip_self=acc)
            for k, c in t.r.items():
                self._wait(eng, k, c)

    def _mark(self, key, val, reads, writes):
        for v in reads:
            t = v.t
            t.r[key] = max(t.r.get(key, 0), val)
        for v in writes:
            t = v.t
            t.w = (key, val)
            t.r = {}

    def op(self, eng, fn, reads, writes, acc=False):
        self._deps(eng, reads, writes, acc)
        ins = fn()
        self.cnt[eng] += 1
        ins.then_inc(self.sem[eng], 1)
        self._mark(eng, self.cnt[eng], reads, writes)
        self.n_ins += 1
        return ins

    def dma(self, q, out, in_, **kw):
        owner = out.t
        sem = self._dsem(owner)
        self._deps(q, [in_], [out])
        ins = self.e[q].dma_start(out=out.ap, in_=in_.ap, **kw)
        owner.dcnt += 16
        ins.then_inc(sem, 16)
        self._dcnt[("d", owner.name)] = owner.dcnt
        self._mark(("d", owner.name), owner.dcnt, [in_], [out])
        self.n_ins += 1
        return ins

    def gather(self, out, table, idx):
        owner = out.t
        sem = self._dsem(owner)
        self._deps("pool", [table, idx], [out])
        ins = self.nc.gpsimd.indirect_dma_start(
            out=out.ap, out_offset=None, in_=table.ap,
            in_offset=bass.IndirectOffsetOnAxis(ap=idx.ap, axis=0))
        owner.dcnt += 16
        ins.then_inc(sem, 16)
        self._dcnt[("d", owner.name)] = owner.dcnt
        self._mark(("d", owner.name), owner.dcnt, [table, idx], [out])
        self.n_ins += 1
        return ins

    def mm(self, out, lhsT, rhs, start=True, stop=True):
        nc = self.nc
        return self.op("pe", lambda: nc.tensor.matmul(out.ap, lhsT.ap, rhs.ap, start=start, stop=stop),
                       [lhsT, rhs], [out], acc=True)

    def tr(self, out, in_, ident):
        nc = self.nc
        return self.op("pe", lambda: nc.tensor.transpose(out.ap, in_.ap, ident.ap), [in_, ident], [out], acc=True)

    def act(self, out, in_, func, bias=None, scale=1.0, accum=None, eng="act"):
        nc = self.nc
        kw = {}
        rd = [in_]
        wr = [out]
        if bias is not None:
            if isinstance(bias, V):
                kw["bias"] = bias.ap
                rd.append(bias)
            else:
                kw["bias"] = bias
        if isinstance(scale, V):
            kw["scale"] = scale.ap
            rd.append(scale)
        else:
            kw["scale"] = scale
        if accum is not None:
            kw["accum_out"] = accum.ap
            wr.append(accum)
        return self.op("act", lambda: nc.scalar.activation(out=out.ap, in_=in_.ap, func=func, **kw), rd, wr)

    def tt(self, eng, out, a, b, op):
        e = self.e[eng]
        return self.op(eng, lambda: e.tensor_tensor(out=out.ap, in0=a.ap, in1=b.ap, op=op), [a, b], [out])

    def ts(self, eng, out, a, s1, op0, s2=None, op1=None):
        e = self.e[eng]
        rd = [a]
        a1 = s1.ap if isinstance(s1, V) else s1
        a2 = s2.ap if isinstance(s2, V) else s2
        if isinstance(s1, V):
            rd.append(s1)
        if isinstance(s2, V):
            rd.append(s2)
        if op1 is None:
            return self.op(eng, lambda: e.tensor_scalar(out=out.ap, in0=a.ap, scalar1=a1, scalar2=None, op0=op0), rd, [out])
        return self.op(eng, lambda: e.tensor_scalar(out=out.ap, in0=a.ap, scalar1=a1, scalar2=a2, op0=op0, op1=op1), rd, [out])

    def stt(self, eng, out, a, s, b, op0, op1, accum=None):
        e = self.e[eng]
        rd = [a, b]
        sa = s.ap if isinstance(s, V) else s
        if isinstance(s, V):
            rd.append(s)
        wr = [out]
        kw = {}
        if accum is not None:
            kw["accum_out"] = accum.ap
            wr.append(accum)
        return self.op(eng, lambda: e.scalar_tensor_tensor(out=out.ap, in0=a.ap, scalar=sa, in1=b.ap, op0=op0, op1=op1, **kw), rd, wr)

    def copy(self, eng, out, in_):
        if eng == "act":
            nc = self.nc
            return self.op("act", lambda: nc.scalar.copy(out=out.ap, in_=in_.ap), [in_], [out])
        e = self.e[eng]
        return self.op(eng, lambda: e.tensor_copy(out=out.ap, in_=in_.ap), [in_], [out])

    def memset(self, eng, out, val):
        e = self.e[eng]
        return self.op(eng, lambda: e.memset(out.ap, val), [], [out])

    def rsqrt(self, out, in_, scale, eps):
        nc = self.nc
        self.ts("dve", out, in_, scale, ALU.mult, eps, ALU.add)
        self.act(out, out, AF.Sqrt)
        self.op("dve", lambda: nc.vector.reciprocal(out=out.ap, in_=out.ap), [out], [out])

    def mark(self):
        return len(self._ctx)

    def barrier(self):
        for eng in self.ENG:
            for k in list(self.sem.keys()):
                if isinstance(k, tuple):
                    c = self._dcnt.get(k, 0)
                else:
                    c = self.cnt[k]
                if c > 0:
                    self._wait(eng, k, c)

    def release(self, mark):
        while len(self._ctx) > mark:
            cm = self._ctx.pop()
            cm.__exit__(None, None, None)

    def finish(self):
        for t in self.out_tiles:
            if t.w is not None:
                self._wait("sp", *t.w)
        for cm in reversed(self._ctx):
            cm.__exit__(None, None, None)
        for cm in reversed(self._sctx):
            cm.__exit__(None, None, None)
        return self.nc


D = 1024
NEXP = 16384


def build_post(NT, n_lat, n_part, final):
    P = Prog()
    nc = P.nc
    N = NT * 128
    x = P.dram("x", [N, D], F32, kind="ExternalInput")
    parts = [P.dram("p%d" % i, [N, D], F32, kind="ExternalInput") for i in range(n_part)]
    cT = P.dram("cT", [128, 8, 2], F32, kind="ExternalInput")
    modw = P.dram("modw", [D, 4096], F32, kind="ExternalInput")
    modb2 = P.dram("modb2", [2, 4096], F32, kind="ExternalInput")
    sel = P.dram("sel", [2, 2, 128], F32, kind="ExternalInput")
    n2 = P.dram("n2", [2, D], F32, kind="ExternalInput")
    nf = P.dram("nf", [2, D], F32, kind="ExternalInput")
    pq = P.dram("pq", [D, 2048], F32, kind="ExternalInput")
    skT = P.dram("skT", [128, 8, 2, 128], F32, kind="ExternalInput")
    pu = P.dram("pu", [NEXP, D], F32, kind="ExternalInput")
    pv = P.dram("pv", [NEXP, D], F32, kind="ExternalInput")
    identd = P.dram("ident", [128, 128], F32, kind="ExternalInput")
    zseld = P.dram("zsel", [128, 255], F32, kind="ExternalInput")
    iotad = P.dram("iota16", [128, 16], F32, kind="ExternalInput")
    y = P.dram("y", [N, D], F32, kind="ExternalOutput")

    ident = P.sb("ident_sb", [128, 128])
    identb = P.sb("identb", [128, 128], BF16)
    zsel = P.sb("zsel_sb", [128, 255])
    iota16 = P.sb("iota_sb", [128, 16])
    sk_sb = P.sb("sk_sb", [128, 8, 2, 128])
    sel_sb = P.sb("sel_sb", [2, 2, 128])
    modb_sb = P.sb("modb_sb", [2, 4096])
    n2_sb = P.sb("n2_sb", [2, D])
    nf_sb = P.sb("nf_sb", [2, D])
    c_sb = P.sb("c_sb", [128, 8, 2])
    sc_sb = P.sb("sc_sb", [128, 8, 2])
    rows = P.sb("rows", [2, 4096])
    P.dma("sp", ident[:], identd[:])
    P.dma("sp", zsel[:], zseld[:])
    P.dma("sp", iota16[:], iotad[:])
    P.dma("sp", sk_sb[:], skT[:])
    P.dma("sp", sel_sb[:], sel[:])
    P.dma("sp", modb_sb[:], modb2[:])
    P.dma("sp", n2_sb[:], n2[:])
    P.dma("sp", nf_sb[:], nf[:])
    P.dma("sp", c_sb[:], cT[:])
    P.copy("dve", identb[:], ident[:])
    P.act(sc_sb[:], c_sb[:], AF.Silu)

    psA = [P.ps("psA%d" % i, [128, 512]) for i in range(2)]
    psR = [P.ps("psR%d" % i, [128, 512]) for i in range(4)]
    psO = [P.ps("psO%d" % i, [128, 512]) for i in range(2)]

    wbuf = [P.sb("wbuf%d" % i, [128, 8, 256]) for i in range(2)]
    modw_v = modw.ap.rearrange("(k p) n -> p k n", p=128)
    pq_v = pq.ap.rearrange("(k p) n -> p k n", p=128)
    for cb in range(16):
        wb = wbuf[cb % 2]
        P.dma("sp", wb[:], V(modw, modw_v[:, :, cb * 256:(cb + 1) * 256]))
        pr = psA[cb % 2]
        for k in range(8):
            P.mm(V(pr, pr.ap[0:2, 0:256]), sc_sb[:, k, :], wb[:, k, :], start=(k == 0), stop=(k == 7))
        P.tt("dve", rows[:, cb * 256:(cb + 1) * 256], V(pr, pr.ap[0:2, 0:256]), modb_sb[:, cb * 256:(cb + 1) * 256], ALU.add)
    P.stt("dve", rows[:, 2048:3072], rows[:, 2048:3072], 1.0, n2_sb[:], ALU.add, ALU.mult)

    rep = [P.sb("rep%d" % i, [128, D]) for i in range(4)]
    nf_rep = P.sb("nf_rep", [128, D])

    def load_class(cls):
        for i in range(4):
            for hf in range(2):
                pr = psA[hf]
                P.mm(pr[:], sel_sb[:, cls, :], rows[:, i * 1024 + hf * 512: i * 1024 + hf * 512 + 512])
                P.copy("act", rep[i][:, hf * 512:(hf + 1) * 512], pr[:])

    if final:
        for hf in range(2):
            pr = psA[hf]
            P.mm(pr[:], sel_sb[:, 0, :], nf_sb[:, hf * 512:(hf + 1) * 512])
            P.copy("act", nf_rep[:, hf * 512:(hf + 1) * 512], pr[:])

    xt = P.sb("xt", [128, D])
    pt = [P.sb("pt%d" % i, [128, D]) for i in range(n_part)]
    x1 = P.sb("x1", [128, D])
    hn = P.sb("hn", [128, D])
    hnb = P.sb("hnb", [128, D], BF16)
    hnT = P.sb("hnT", [128, 8, 128])
    qT = P.sb("qT", [128, 16, 128])
    S4 = [P.sb("S4_%d" % g, [128, 4, 128]) for g in range(4)]
    scrA = P.sb("scrA", [128, 2048])
    scrB = P.sb("scrB", [128, 2048])
    tmpj = [scrA.sub((slice(None), slice(j * 128, (j + 1) * 128)), j) for j in range(16)]
    v16 = P.sb("v16", [128, 16, 16])
    i16 = P.sb("i16", [128, 16, 16], U32)
    v16j = [v16.sub((slice(None), j, slice(None)), j) for j in range(16)]
    i16j = [i16.sub((slice(None), j, slice(None)), j) for j in range(16)]
    candh = [scrB.sub((slice(None), slice(h * 256, (h + 1) * 256)), h) for h in range(8)]
    c16 = P.sb("c16", [128, 8, 16])
    ci16 = P.sb("ci16", [128, 8, 16], U32)
    c16h = [c16.sub((slice(None), h, slice(None)), h) for h in range(8)]
    ci16h = [ci16.sub((slice(None), h, slice(None)), h) for h in range(8)]
    hi_u = P.sb("hi_u", [128, 8, 16], U32)
    lo_u = P.sb("lo_u", [128, 8, 16], U32)
    hi_f = P.sb("hi_f", [128, 8, 16])
    lo_f = P.sb("lo_f", [128, 8, 16])
    i16f = P.sb("i16f", [128, 16, 16])
    e1 = P.sb("e1", [128, 8, 16])
    e2 = P.sb("e2", [128, 8, 16])
    ef = P.sb("ef", [128, 128])
    gate = P.sb("gate", [128, 8, 16])
    gmx = P.sb("gmx", [128, 8])
    eiT = P.sb("eiT", [128, 128], I32)
    gateT = P.sb("gateT", [128, 128])
    A = P.sb("A", [128, 128])
    Ag = P.sb("Ag", [128, 128])
    ss = P.sb("ss", [128, 1])
    rstd = P.sb("rstd", [128, 1])
    NB = 4
    U = [P.sb("U%d" % i, [128, D]) for i in range(NB)]
    Vg = [P.sb("Vg%d" % i, [128, D]) for i in range(NB)]
    Vgb = [P.sb("Vgb%d" % i, [128, D], BF16) for i in range(NB)]
    At = [P.sb("At%d" % i, [128, 128], BF16) for i in range(NB)]
    xo = P.sb("xo", [128, D])
    junk = xo

    cur_cls = None
    for it in range(NT):
        cls = 0 if it < n_lat else 1
        if cls != cur_cls:
            load_class(cls)
            cur_cls = cls
        g1r, sh2r, w2r, g2r = rep
        r0 = it * 128
        P.dma("sp", xt[:], x[r0:r0 + 128, :])
        for i in range(n_part):
            P.dma("sp", pt[i][:], parts[i][r0:r0 + 128, :])
        for i in range(1, n_part):
            P.tt("pool", pt[0][:], pt[0][:], pt[i][:], ALU.add)
        P.tt("dve", x1[:], pt[0][:], g1r[:], ALU.mult)
        P.tt("dve", x1[:], x1[:], xt[:], ALU.add)
        P.act(junk[:], x1[:], AF.Square, accum=ss[:])
        P.rsqrt(rstd[:], ss[:], 1.0 / D, 1e-6)
        P.stt("dve", hn[:], x1[:], rstd[:, 0:1], w2r[:], ALU.mult, ALU.mult)
        P.tt("pool", hn[:], hn[:], sh2r[:], ALU.add)
        P.copy("act", hnb[:], hn[:])
        for g in range(2):
            pr = psA[g]
            for kk in range(4):
                k = g * 4 + kk
                P.tr(pr[:, kk * 128:(kk + 1) * 128], hn[:, k * 128:(k + 1) * 128], ident[:])
            P.copy("act" if g == 0 else "dve", V(hnT, hnT.ap[:, g * 4:(g + 1) * 4, :]),
                   V(pr, pr.ap.rearrange("p (a b) -> p a b", a=4)))
        for c8 in range(8):
            wb = wbuf[c8 % 2]
            P.dma("sp", wb[:], V(pq, pq_v[:, :, c8 * 256:(c8 + 1) * 256]))
            c4 = c8 // 2
            pr = psA[c4 % 2]
            for qq in range(2):
                pos = (c8 % 2) * 2 + qq
                for k in range(8):
                    P.mm(pr[:, pos * 128:(pos + 1) * 128], wb[:, k, qq * 128:(qq + 1) * 128], hnT[:, k, :],
                         start=(k == 0), stop=(k == 7))
            if c8 % 2 == 1:
                P.copy("act" if c4 % 2 == 0 else "dve", V(qT, qT.ap[:, c4 * 4:(c4 + 1) * 4, :]),
                       V(pr, pr.ap.rearrange("p (a b) -> p a b", a=4)))
        for g in range(4):
            pr = psA[g % 2]
            for jj in range(4):
                j = g * 4 + jj
                P.mm(pr[:, jj * 128:(jj + 1) * 128], qT[:, j, :], sk_sb[:, j // 2, j % 2, :])
            P.copy("act" if g % 2 == 0 else "dve", S4[g][:], V(pr, pr.ap.rearrange("p (a b) -> p a b", a=4)))
        Sj = [S4[j // 4][:, j % 4, :] for j in range(16)]
        for j in range(16):
            P.op("dve", (lambda j=j: nc.vector.max(out=v16j[j].ap[:, 0:8], in_=Sj[j].ap)), [Sj[j]], [v16j[j][:]])
        for j in range(16):
            P.op("dve", (lambda j=j: nc.vector.match_replace(out=tmpj[j].ap, in_to_replace=v16j[j].ap[:, 0:8],
                                                             in_values=Sj[j].ap, imm_value=-1e30)),
                 [Sj[j], v16j[j][:]], [tmpj[j][:]])
        for j in range(16):
            P.op("dve", (lambda j=j: nc.vector.max(out=v16j[j].ap[:, 8:16], in_=tmpj[j].ap)), [tmpj[j][:]], [v16j[j][:]])
        for j in range(16):
            P.op("dve", (lambda j=j: nc.vector.max_index(out=i16j[j].ap[:, 0:8], in_max=v16j[j].ap[:, 0:8],
                                                         in_values=Sj[j].ap)), [Sj[j], v16j[j][:]], [i16j[j][:]])
        for j in range(16):
            P.op("dve", (lambda j=j: nc.vector.max_index(out=i16j[j].ap[:, 8:16], in_max=v16j[j].ap[:, 8:16],
                                                         in_values=Sj[j].ap)), [Sj[j], v16j[j][:]], [i16j[j][:]])
        for h in range(8):
            a0 = v16j[2 * h].ap.unsqueeze(2).broadcast_to([128, 16, 16])
            a1 = v16j[2 * h + 1].ap.unsqueeze(1).broadcast_to([128, 16, 16])
            co = candh[h].ap.rearrange("p (a b) -> p a b", a=16)
            P.op("dve", (lambda co=co, a0=a0, a1=a1: nc.vector.tensor_tensor(out=co, in0=a0, in1=a1, op=ALU.add)),
                 [v16j[2 * h][:], v16j[2 * h + 1][:]], [candh[h][:]])
        tmp2 = [scrA.sub((slice(None), slice(h * 256, (h + 1) * 256)), "c%d" % h) for h in range(8)]
        t2dep = lambda h: [tmpj[2 * h][:], tmpj[2 * h + 1][:]]
        for h in range(8):
            P.op("dve", (lambda h=h: nc.vector.max(out=c16h[h].ap[:, 0:8], in_=candh[h].ap)), [candh[h][:]], [c16h[h][:]])
        for h in range(8):
            P.op("dve", (lambda h=h: nc.vector.match_replace(out=tmp2[h].ap, in_to_replace=c16h[h].ap[:, 0:8],
                                                             in_values=candh[h].ap, imm_value=-1e30)),
                 [candh[h][:], c16h[h][:]], t2dep(h))
        for h in range(8):
            P.op("dve", (lambda h=h: nc.vector.max(out=c16h[h].ap[:, 8:16], in_=tmp2[h].ap)), t2dep(h), [c16h[h][:]])
        for h in range(8):
            P.op("dve", (lambda h=h: nc.vector.max_index(out=ci16h[h].ap[:, 0:8], in_max=c16h[h].ap[:, 0:8],
                                                         in_values=candh[h].ap)), [candh[h][:], c16h[h][:]], [ci16h[h][:]])
        for h in range(8):
            P.op("dve", (lambda h=h: nc.vector.max_index(out=ci16h[h].ap[:, 8:16], in_max=c16h[h].ap[:, 8:16],
                                                         in_values=candh[h].ap)), [candh[h][:], c16h[h][:]], [ci16h[h][:]])
        allci = [t[:] for t in ci16h]
        allc = [t[:] for t in c16h]
        alli = [t[:] for t in i16j]
        P.op("dve", lambda: nc.vector.tensor_single_scalar(out=hi_u.ap, in_=ci16.ap, scalar=4, op=ALU.logical_shift_right),
             allci, [hi_u[:]])
        P.op("dve", lambda: nc.vector.tensor_single_scalar(out=lo_u.ap, in_=ci16.ap, scalar=15, op=ALU.bitwise_and),
             allci, [lo_u[:]])
        P.copy("dve", hi_f[:], hi_u[:])
        P.copy("dve", lo_f[:], lo_u[:])
        P.op("dve", lambda: nc.vector.tensor_copy(out=i16f.ap, in_=i16.ap), alli, [i16f[:]])
        i16f_v = i16f.ap.rearrange("p (h two) k -> p h two k", two=2)
        oh = scrA.ap.rearrange("p (h k i) -> p h k i", h=8, k=16)
        pr4 = scrB.ap.rearrange("p (h k i) -> p h k i", h=8, k=16)
        scrA_all = [t[:] for t in tmpj]
        scrB_all = [t[:] for t in candh]
        iota_b = iota16.ap.unsqueeze(1).unsqueeze(1).broadcast_to([128, 8, 16, 16])
        for (src_f, half, dst) in ((hi_f, 0, e1), (lo_f, 1, e2)):
            sb_ = src_f.ap.unsqueeze(3).broadcast_to([128, 8, 16, 16])
            P.op("dve", (lambda sb_=sb_: nc.vector.tensor_tensor(out=oh, in0=sb_, in1=iota_b, op=ALU.is_equal)),
                 [src_f[:], iota16[:]], scrA_all)
            ib = i16f_v[:, :, half, :].unsqueeze(2).broadcast_to([128, 8, 16, 16])
            P.op("dve", (lambda ib=ib: nc.vector.tensor_tensor(out=pr4, in0=oh, in1=ib, op=ALU.mult)),
                 scrA_all + [i16f[:]], scrB_all)
            P.op("dve", (lambda dst=dst: nc.vector.tensor_reduce(out=dst.ap, in_=pr4, axis=AX.X, op=ALU.add)),
                 scrB_all, [dst[:]])
        ef3 = ef.ap.rearrange("p (h k) -> p h k", h=8)
        P.op("dve", lambda: nc.vector.scalar_tensor_tensor(out=ef3, in0=e1.ap, scalar=128.0, in1=e2.ap,
                                                           op0=ALU.mult, op1=ALU.add), [e1[:], e2[:]], [ef[:]])
        P.op("dve", lambda: nc.vector.tensor_copy(out=gmx.ap, in_=c16.ap[:, :, 0]), allc, [gmx[:]])
        P.op("dve", lambda: nc.vector.tensor_tensor(out=gate.ap, in0=c16.ap, in1=gmx.ap.unsqueeze(2).broadcast_to([128, 8, 16]),
                                                    op=ALU.subtract), allc + [gmx[:]], [gate[:]])
        P.act(gate[:], gate[:], AF.Exp)
        P.op("dve", lambda: nc.vector.tensor_reduce(out=gmx.ap, in_=gate.ap, axis=AX.X, op=ALU.add), [gate[:]], [gmx[:]])
        P.op("dve", lambda: nc.vector.reciprocal(out=gmx.ap, in_=gmx.ap), [gmx[:]], [gmx[:]])
        P.op("dve", lambda: nc.vector.tensor_tensor(out=gate.ap, in0=gate.ap, in1=gmx.ap.unsqueeze(2).broadcast_to([128, 8, 16]),
                                                    op=ALU.mult), [gate[:], gmx[:]], [gate[:]])
        pr = psA[0]
        P.tr(pr[:, 0:128], ef[:], ident[:])
        P.op("dve", lambda: nc.vector.tensor_copy(out=eiT.ap, in_=pr.ap[:, 0:128]), [pr[:]], [eiT[:]])
        pr2 = psA[1]
        P.tr(pr2[:, 0:128], V(gate, gate.ap.rearrange("p h k -> p (h k)")), ident[:])
        P.copy("act", gateT[:], pr2[:, 0:128])
        P.memset("dve", A[:], 0.0)
        for t in range(128):
            b = t % NB
            P.gather(U[b][:], pu[:], eiT[:, t:t + 1])
            rb = (t % 2) * 2
            lt = V(identb, identb.ap[:, t:t + 1].broadcast_to([128, 128]))
            for hf in range(2):
                P.mm(psR[rb + hf][:], lt, hnb[:, hf * 512:(hf + 1) * 512])
            for hf in range(2):
                P.stt("dve", junk[:, hf * 512:(hf + 1) * 512], U[b][:, hf * 512:(hf + 1) * 512], 1.0, psR[rb + hf][:],
                      ALU.mult, ALU.mult, accum=scrB[:, t * 2 + hf: t * 2 + hf + 1])
        P.op("dve", lambda: nc.vector.tensor_reduce(out=A.ap, in_=scrB.ap[:, 0:256].rearrange("p (t two) -> p t two", two=2),
                                                    axis=AX.X, op=ALU.add), [scrB[:]], [A[:]])
        P.act(Ag[:], A[:], AF.Gelu)
        P.tt("dve", Ag[:], Ag[:], gateT[:], ALU.mult)
        for t in range(128):
            b = t % NB
            P.gather(Vg[b][:], pv[:], eiT[:, t:t + 1])
            P.copy("act", Vgb[b][:], Vg[b][:])
            P.ts("pool", At[b][:], zsel[:, 127 - t:255 - t], Ag[:, t:t + 1], ALU.mult)
            for hf in range(2):
                P.mm(psO[hf][:], At[b][:], Vgb[b][:, hf * 512:(hf + 1) * 512], start=(t == 0), stop=(t == 127))
        for hf in range(2):
            sl = slice(hf * 512, (hf + 1) * 512)
            P.tt("dve", xo[:, sl], psO[hf][:], g2r[:, sl], ALU.mult)
        P.tt("pool", xo[:], xo[:], x1[:], ALU.add)
        if final:
            P.act(hn[:], xo[:], AF.Square, accum=ss[:])
            P.rsqrt(rstd[:], ss[:], 1.0 / D, 1e-6)
            P.stt("dve", xo[:], xo[:], rstd[:, 0:1], nf_rep[:], ALU.mult, ALU.mult)
        P.dma("sp", y[r0:r0 + 128, :], xo[:])
    P.finish()
    return P


def post_consts(mod_w, mod_b, norm2, norm_f, pq, sk1, sk2, pu, pv):
    f = np.float32
    sel = np.zeros((2, 2, 128), f)
    sel[0, 0, :] = 1
    sel[1, 1, :] = 1
    zsel = np.zeros((128, 255), f)
    zsel[:, 127] = 1
    skT = np.ascontiguousarray(np.stack([sk1, sk2], 0).transpose(3, 1, 0, 2))
    return {
        "modw": np.ascontiguousarray(mod_w[:, 2048:6144]),
        "modb2": np.ascontiguousarray(np.tile(mod_b[None, 2048:6144], (2, 1))),
        "sel": sel,
        "n2": np.ascontiguousarray(np.tile(norm2[None], (2, 1))),
        "nf": np.ascontiguousarray(np.tile(norm_f[None], (2, 1))),
        "pq": pq, "skT": skT, "pu": pu, "pv": pv,
        "ident": np.eye(128, dtype=f), "zsel": zsel,
        "iota16": np.ascontiguousarray(np.tile(np.arange(16, dtype=f)[None], (128, 1))),
    }


def c_cols(c_b, c_ctx):
    cc = np.stack([c_b, c_ctx], -1).astype(np.float32)
    return np.ascontiguousarray(cc.reshape(8, 128, 2).transpose(1, 0, 2))


def build_l1(S=8192, CL=256, CONV_T=4096, upto=9):
    P = Prog()
    nc = P.nc
    NQB = S // 512
    NKT = (S + CL) // 128
    NCB = CONV_T // 256
    XC = CONV_T + 30
    di = lambda n, s, dt=F32: P.dram(n, s, dt, kind="ExternalInput")
    xT = di("xT", [D, S]); ctxT = di("ctxT", [D, CL]); xcT = di("xcT", [D, XC])
    edge = di("edge", [128, 2]); cT = di("cT", [128, 8, 2]); modw = di("modw", [D, 2048])
    modbc = di("modbc", [128, 16]); n1c = di("n1c", [128, 8])
    wq = di("wq", [D, 384]); wk = di("wk", [D, 128]); wv = di("wv", [D, 128]); wu = di("wu", [D, 512])
    gq = di("gq", [128, 1]); gk = di("gk", [128, 1])
    ropeC = di("ropeC", [128, S]); ropeS = di("ropeS", [128, S]); Rm = di("Rm", [128, 128])
    bones = di("bones", [128, 128]); onesD = di("onesD", [128, 128]); ones256 = di("ones256", [128, 128])
    dww = di("dww", [128, 2, 31]); dwb = di("dwb", [128, 2]); cng = di("cng", [128, 2]); cnb = di("cnb", [128, 2])
    woa = di("woa", [64, 6, D]); woc = di("woc", [128, 2, D]); shiftm = di("shiftm", [128, 64])
    paT = P.dram("paT", [D, S], F32, kind="ExternalOutput")
    pcT = P.dram("pcT", [D, CONV_T], F32, kind="ExternalOutput")

    ps = [P.ps("ps%d" % i, [128, 512]) for i in range(8)]
    def ld(name, src, shape, dt=F32):
        t = P.sb(name, shape, dt)
        P.dma("sp", t[:], src[:])
        return t
    edge_s = ld("edge_s", edge, [128, 2]); c_sb = ld("c_sb", cT, [128, 8, 2]); modb_s = ld("modb_s", modbc, [128, 16])
    n1_s = ld("n1_s", n1c, [128, 8]); gq_s = ld("gq_s", gq, [128, 1]); gk_s = ld("gk_s", gk, [128, 1])
    Rm_s = ld("Rm_s", Rm, [128, 128]); bones_s = ld("bones_s", bones, [128, 128]); onesD_s = ld("onesD_s", onesD, [128, 128])
    ones256_s = ld("ones256_s", ones256, [128, 128]); dww_s = ld("dww_s", dww, [128, 2, 31]); dwb_s = ld("dwb_s", dwb, [128, 2])
    cng_s = ld("cng_s", cng, [128, 2]); cnb_s = ld("cnb_s", cnb, [128, 2]); shift_s = ld("shift_s", shiftm, [128, 64])
    sc_sb = P.sb("sc_sb", [128, 8, 2])
    P.act(sc_sb[:], c_sb[:], AF.Silu)
    xblk = P.sb("xblk", [128, 8, 512])
    stage = xblk
    def ldw(name, src, ncol):
        t = P.sb(name, [128, 8, ncol], BF16)
        P.dma("sp", stage[:, :, 0:ncol], V(src, src.ap.rearrange("(k p) n -> p k n", p=128)))
        P.copy("dve", t[:], stage[:, :, 0:ncol])
        return t
    wq_s = ldw("wq_s", wq, 384); wk_s = ldw("wk_s", wk, 128); wv_s = ldw("wv_s", wv, 128); wu_s = ldw("wu_s", wu, 512)
    woa_s = P.sb("woa_s", [64, 6, D], BF16)
    woc_s = P.sb("woc_s", [128, 2, D], BF16)
    for hh in range(3):
        st_v = V(stage, stage.ap[0:64].rearrange("p k n -> p (k n)")[:, 0:2048].rearrange("p (a n) -> p a n", a=2))
        P.dma("sp", st_v, woa[:, hh * 2:(hh + 1) * 2, :])
        P.copy("dve", woa_s[:, hh * 2:(hh + 1) * 2, :], st_v)
    st_v = V(stage, stage.ap.rearrange("p k n -> p (k n)")[:, 0:2048].rearrange("p (a n) -> p a n", a=2))
    P.dma("sp", st_v, woc[:])
    P.copy("dve", woc_s[:], st_v)
    modc = P.sb("modc", [128, 16, 2])
    modw_v = modw.ap.rearrange("(k p) n -> p k n", p=128)
    for c4 in range(4):
        wb = xblk
        P.dma("sp", wb[:], V(modw, modw_v[:, :, c4 * 512:(c4 + 1) * 512]))
        for q4 in range(4):
            cc = c4 * 4 + q4
            pr = ps[cc % 2]
            for k in range(8):
                P.mm(pr[:, 0:2], wb[:, k, q4 * 128:(q4 + 1) * 128], sc_sb[:, k, :], start=(k == 0), stop=(k == 7))
            P.ts("dve", modc[:, cc, :], pr[:, 0:2], modb_s[:, cc:cc + 1], ALU.add)
    wmod = P.sb("wmod", [128, 8, 2])
    P.op("dve", lambda: nc.vector.scalar_tensor_tensor(out=wmod.ap, in0=modc.ap[:, 8:16, :], scalar=1.0,
                                                       in1=n1_s.ap.unsqueeze(2).broadcast_to([128, 8, 2]),
                                                       op0=ALU.add, op1=ALU.mult), [modc[:], n1_s[:]], [wmod[:]])
    if upto < 1:
        P.finish()
        return P
    qT_r = P.sb("qT_r", [128, 3, S], BF16)
    kT_r = P.sb("kT_r", [128, S + CL], BF16)
    Va = [P.sb("Va%d" % i, [128, NKT, 128], BF16) for i in range(2)]
    for i in range(2):
        P.memset("pool", Va[i][:, :, 64:128], 1.0)
    hT = P.sb("hT", [128, 8, 512], BF16)
    sqb = [P.sb("sqb%d" % i, [128, 512]) for i in range(2)]
    rstd = P.sb("rstd", [128, 512])
    tmpf = [P.sb("tmpf%d" % i, [128, 512]) for i in range(2)]
    rC = P.sb("rC", [128, 512]); rS = P.sb("rS", [128, 512])
    raw = P.sb("raw", [128, 512]); qn = P.sb("qn", [128, 512]); r2 = P.sb("r2", [128, 512])

    def hblock(src, c0, w, cls):
        P.dma("sp", xblk[:, :, 0:w], V(src, src.ap.rearrange("(k p) t -> p k t", p=128)[:, :, c0:c0 + w]))
        pr = ps[0]
        for k in range(8):
            sq = sqb[k % 2]
            P.act(sq[:, 0:w], xblk[:, k, 0:w], AF.Square)
            P.mm(pr[:, 0:w], onesD_s[:], sq[:, 0:w], start=(k == 0), stop=(k == 7))
        P.rsqrt(rstd[:, 0:w], pr[:, 0:w], 1.0 / D, 1e-6)
        for k in range(8):
            tf = tmpf[k % 2]
            P.tt("dve", tf[:, 0:w], xblk[:, k, 0:w], rstd[:, 0:w], ALU.mult)
            P.ts("pool", hT[:, k, 0:w], tf[:, 0:w], wmod[:, k, cls:cls + 1], ALU.mult, modc[:, k, cls:cls + 1], ALU.add)

    def headnorm(pr, w, g_s, dest, rope_c0):
        P.copy("act", raw[:, 0:w], pr[:, 0:w])
        P.act(r2[:, 0:w], raw[:, 0:w], AF.Square)
        p2 = ps[2]
        P.mm(p2[:, 0:w], bones_s[:], r2[:, 0:w])
        P.rsqrt(r2[:, 0:w], p2[:, 0:w], 1.0 / 64, 1e-6)
        if rope_c0 is None:
            P.stt("dve", dest, raw[:, 0:w], g_s[:, 0:1], r2[:, 0:w], ALU.mult, ALU.mult)
            return
        P.stt("dve", qn[:, 0:w], raw[:, 0:w], g_s[:, 0:1], r2[:, 0:w], ALU.mult, ALU.mult)
        p3 = ps[3]
        P.mm(p3[:, 0:w], Rm_s[:], qn[:, 0:w])
        P.tt("dve", r2[:, 0:w], p3[:, 0:w], rS[:, 0:w], ALU.mult)
        P.tt("pool", qn[:, 0:w], qn[:, 0:w], rC[:, 0:w], ALU.mult)
        P.tt("pool", dest, qn[:, 0:w], r2[:, 0:w], ALU.add)

    def kv_proj(w, tok0, rope_c0):
        pr = ps[1]
        for k in range(8):
            P.mm(pr[:, 0:w], wk_s[:, k, :], hT[:, k, 0:w], start=(k == 0), stop=(k == 7))
        headnorm(pr, w, gk_s, kT_r[:, tok0:tok0 + w], rope_c0)
        for tt_ in range(w // 128):
            pv_ = ps[4 + tt_ % 2]
            for k in range(8):
                P.mm(pv_[:, 0:128], hT[:, k, tt_ * 128:(tt_ + 1) * 128], wv_s[:, k, :], start=(k == 0), stop=(k == 7))
            kt = tok0 // 128 + tt_
            P.copy("act", Va[0][:, kt, 0:64], pv_[:, 0:64])
            P.copy("dve", Va[1][:, kt, 0:64], pv_[:, 64:128])

    import os
    dbg = int(os.environ.get("L1DBG", "9"))
    for qb in range(NQB):
        c0 = qb * 512
        hblock(xT, c0, 512, 0)
        if dbg < 1:
            continue
        P.dma("sp", rC[:], ropeC[:, c0:c0 + 512])
        P.dma("sp", rS[:], ropeS[:, c0:c0 + 512])
        for ti in range(3):
            pr = ps[1]
            for k in range(8):
                P.mm(pr[:], wq_s[:, k, ti * 128:(ti + 1) * 128], hT[:, k, :], start=(k == 0), stop=(k == 7))
            if dbg >= 2:
                headnorm(pr, 512, gq_s, qT_r[:, ti, c0:c0 + 512], c0)
        if dbg >= 3:
            kv_proj(512, c0, c0)
    if dbg >= 4:
        hblock(ctxT, 0, CL, 1)
        kv_proj(CL, S, None)

    if upto < 2:
        P.finish()
        return P
    gl = P.sb("gl", [128, 2, 286]); sg = P.sb("sg", [128, 286])
    acc = P.sb("acc", [128, 2, 256]); dd = P.sb("dd", [128, 2, 256]); sqd = P.sb("sqd", [128, 2, 256])
    cact = P.sb("cact", [128, 2, 256], BF16)
    ostg = [P.sb("ostg%d" % i, [128, 512]) for i in range(2)]
    accs = [acc.sub((slice(None), c, slice(None)), c) for c in range(2)]
    gls = [gl.sub((slice(None), c, slice(None)), c) for c in range(2)]
    for j in range(NCB):
        hblock(xcT, 256 * j, 286, 0)
        for c in range(2):
            pv_ = ps[1]
            pg_ = ps[2]
            for k in range(8):
                P.mm(pv_[:, 0:286], wu_s[:, k, c * 128:(c + 1) * 128], hT[:, k, 0:286], start=(k == 0), stop=(k == 7))
            for k in range(8):
                P.mm(pg_[:, 0:286], wu_s[:, k, 256 + c * 128:256 + (c + 1) * 128], hT[:, k, 0:286], start=(k == 0), stop=(k == 7))
            P.act(sg[:], pg_[:, 0:286], AF.Sigmoid)
            P.tt("dve", gls[c][:], pv_[:, 0:286], sg[:], ALU.mult)
            if j == 0:
                P.ts("dve", gls[c][:, 0:15], gls[c][:, 0:15], edge_s[:, 0:1], ALU.mult)
            if j == NCB - 1:
                P.ts("dve", gls[c][:, 271:286], gls[c][:, 271:286], edge_s[:, 1:2], ALU.mult)
        for c in range(2):
            P.ts("dve", accs[c][:], gls[c][:, 0:256], dww_s[:, c, 0:1], ALU.mult, dwb_s[:, c:c + 1], ALU.add)
        for jj in range(1, 31):
            for c in range(2):
                P.stt("dve", accs[c][:], gls[c][:, jj:jj + 256], dww_s[:, c, jj:jj + 1], accs[c][:], ALU.mult, ALU.add)
        pm = ps[3]
        for c in range(2):
            P.mm(pm[:, 0:256], ones256_s[:], accs[c][:], start=(c == 0), stop=(c == 1))
        for c in range(2):
            P.tt("dve", dd[:, c, :], accs[c][:], pm[:, 0:256], ALU.subtract)
        P.act(sqd[:], dd[:], AF.Square)
        pvv = ps[4]
        for c in range(2):
            P.mm(pvv[:, 0:256], ones256_s[:], sqd[:, c, :], start=(c == 0), stop=(c == 1))
        P.rsqrt(rstd[:, 0:256], pvv[:, 0:256], 1.0, 1e-5)
        for c in range(2):
            P.stt("dve", dd[:, c, :], dd[:, c, :], cng_s[:, c:c + 1], rstd[:, 0:256], ALU.mult, ALU.mult)
            P.act(cact[:, c, :], dd[:, c, :], AF.Silu, bias=cnb_s[:, c:c + 1])
        for cc in range(8):
            pw = ps[5 + cc % 2]
            for c in range(2):
                P.mm(pw[:, 0:256], woc_s[:, c, cc * 128:(cc + 1) * 128], cact[:, c, :], start=(c == 0), stop=(c == 1))
            og = ostg[cc % 2]
            P.copy("act", og[:, 0:256], pw[:, 0:256])
            P.dma("sp", pcT[cc * 128:(cc + 1) * 128, 256 * j:256 * j + 256], og[:, 0:256])

    if upto < 3:
        P.finish()
        return P
    Pt = [P.sb("Pt%d" % i, [128, 512], BF16) for i in range(3)]
    Osb = P.sb("Osb", [128, 512]); rden = P.sb("rden", [64, 512])
    oT = [P.sb("oT%d" % j, [64, 512], BF16) for j in range(6)]
    for qb in range(NQB):
        c0 = qb * 512
        for j in range(6):
            half = j // 3
            ti = j % 3
            pl = slice(half * 64, half * 64 + 64)
            pO = ps[2]
            for kt in range(NKT):
                pS = ps[kt % 2]
                P.mm(pS[:], kT_r[pl, kt * 128:(kt + 1) * 128], qT_r[pl, ti, c0:c0 + 512])
                pt_ = Pt[kt % 3]
                P.act(pt_[:], pS[:], AF.Exp, scale=0.125)
                P.mm(pO[:], Va[half][:, kt, :], pt_[:], start=(kt == 0), stop=(kt == NKT - 1))
            P.copy("dve", Osb[:], pO[:])
            pD = ps[3]
            P.mm(pD[0:64, :], shift_s[:], Osb[:])
            P.op("dve", lambda pD=pD: nc.vector.reciprocal(out=rden.ap, in_=pD.ap[0:64, :]), [pD[:]], [rden[:]])
            P.tt("dve", oT[j][:], Osb[0:64, :], rden[:], ALU.mult)
        for cc in range(8):
            pw = ps[5 + cc % 2]
            for j in range(6):
                P.mm(pw[:], woa_s[:, j, cc * 128:(cc + 1) * 128], oT[j][:], start=(j == 0), stop=(j == 5))
            og = ostg[cc % 2]
            P.copy("act", og[:], pw[:])
            P.dma("sp", paT[cc * 128:(cc + 1) * 128, c0:c0 + 512], og[:])
    P.finish()
    return P


def rope_tables(S):
    f = np.float32
    t = np.arange(S)
    row = (t // 64).astype(f)
    col = (t % 64).astype(f)
    inv = (f(10000.0) ** (-np.arange(0, 32, 2, dtype=f) / f(32))).astype(f)
    ang = np.stack([row[:, None] * inv, col[:, None] * inv], 1)
    cs, sn = np.cos(ang).astype(f), np.sin(ang).astype(f)
    C = np.zeros((64, S), f)
    Sn = np.zeros((64, S), f)
    for ax in range(2):
        for hf in range(2):
            C[ax * 32 + hf * 16: ax * 32 + hf * 16 + 16] = cs[:, ax, :].T
            Sn[ax * 32 + hf * 16: ax * 32 + hf * 16 + 16] = sn[:, ax, :].T
    Rm = np.zeros((128, 128), f)
    for m in range(128):
        if (m % 32) < 16:
            Rm[m + 16, m] = -1.0
        else:
            Rm[m - 16, m] = 1.0
    return np.ascontiguousarray(np.tile(C, (2, 1))), np.ascontiguousarray(np.tile(Sn, (2, 1))), Rm


def colmat(v, nk):
    return np.ascontiguousarray(np.asarray(v, np.float32).reshape(nk, 128).T)


def l1_consts(mod_w, mod_b, norm1, w_in, q_norm, k_norm, dw_w, dw_b, cn_g, cn_b, w_out, s, S):
    f = np.float32
    C, Sn, Rm = rope_tables(S)
    bones = np.zeros((128, 128), f)
    bones[0:64, 0:64] = 1
    bones[64:128, 64:128] = 1
    shiftm = np.zeros((128, 64), f)
    for m in range(64):
        shiftm[m + 64, m] = 1
    qcols = []
    for i in range(3):
        for hh in (6 * s + i, 6 * s + 3 + i):
            qcols.append(w_in[:, hh * 64:(hh + 1) * 64])
    wq = np.ascontiguousarray(np.concatenate(qcols, 1))
    wk = np.ascontiguousarray(w_in[:, 768 + 128 * s: 768 + 128 * (s + 1)])
    wv = np.ascontiguousarray(w_in[:, 1024 + 128 * s: 1024 + 128 * (s + 1)])
    wu = np.ascontiguousarray(w_in[:, 1280:1792])
    woa = np.ascontiguousarray(w_out[384 * s:384 * (s + 1)].reshape(6, 64, D).transpose(1, 0, 2))
    woc = np.ascontiguousarray(w_out[768:1024].reshape(2, 128, D).transpose(1, 0, 2))
    dww = np.ascontiguousarray(dw_w.T.reshape(2, 128, 31).transpose(1, 0, 2))
    return {
        "modw": np.ascontiguousarray(mod_w[:, 0:2048]), "modbc": colmat(mod_b[0:2048], 16), "n1c": colmat(norm1, 8),
        "wq": wq, "wk": wk, "wv": wv, "wu": wu,
        "gq": np.ascontiguousarray(np.tile(q_norm, 2)[:, None].astype(f)),
        "gk": np.ascontiguousarray(np.tile(k_norm, 2)[:, None].astype(f)),
        "ropeC": C, "ropeS": Sn, "Rm": Rm, "bones": bones, "onesD": np.ones((128, 128), f),
        "ones256": np.full((128, 128), 1.0 / 256, f),
        "dww": dww, "dwb": colmat(dw_b, 2), "cng": colmat(cn_g, 2), "cnb": colmat(cn_b, 2),
        "woa": woa, "woc": woc, "shiftm": shiftm,
    }


def l1_core_inputs(xb, ctxb, c_b, c_ctx, s, conv_t):
    f = np.float32
    S = xb.shape[0]
    lo = conv_t * s - 15
    hi = conv_t * s + conv_t + 15
    xc = np.zeros((conv_t + 30, D), f)
    a, b_ = max(lo, 0), min(hi, S)
    xc[a - lo:b_ - lo] = xb[a:b_]
    edge = np.zeros((128, 2), f)
    edge[:, 0] = 1.0 if lo >= 0 else 0.0
    edge[:, 1] = 1.0 if hi <= S else 0.0
    return {"xT": np.ascontiguousarray(xb.T), "ctxT": np.ascontiguousarray(ctxb.T), "xcT": np.ascontiguousarray(xc.T),
            "edge": edge, "cT": c_cols(c_b, c_ctx)}


CH = 64
NEG_EXP_HALF = -0.6065306597126334


def build_l0(S=8192, CL=256, upto=9, dbg=False):
    P = Prog()
    nc = P.nc
    TT = CL + S
    NCH = TT // CH
    segs = [(0, CL, 1), (CL, S, 0)]
    di = lambda n, s, dt=F32: P.dram(n, s, dt, kind="ExternalInput")
    xT = di("xT", [D, S]); ctxT = di("ctxT", [D, CL])
    cT = di("cT", [128, 8, 2]); modw = di("modw", [D, 2048]); modbc = di("modbc", [128, 16]); n1c = di("n1c", [128, 8])
    wsel = di("wsel", [D, 1664]); mup = di("mup", [1, 1536]); mun = di("mun", [1, 1536])
    vecs = di("vecs", [128, 3, 5])
    w0a0 = di("w0a0", [128, 2, 2, 3])
    w2d = di("w2", [128, 384]); a2d = di("a2", [128, 384]); g2d = di("g2", [128, 384])
    wod = di("wo", [128, 4, D])
    mAT = di("mAT", [2, 128, 128]); mN = di("mN", [2, 64, 64]); id64 = di("id64", [64, 64]); mAK = di("mAK", [2, 128, 64])
    rmaskd = di("rmask", [128, 512]); bonesd = di("bones", [128, 128]); onesDd = di("onesD", [128, 128])
    W64d = di("W64", [2, 128, 256])
    tabA = di("tabA", [S, 128]); tabB = di("tabB", [S, S // 128]); ctxCS = di("ctxCS", [2, CL, CL])
    paT = P.dram("paT", [D, TT], F32, kind="ExternalOutput")
    sk = "ExternalOutput" if dbg else "Internal"
    hT_s = [P.dram("hT_s%d" % i, [128, 8, ln + 2], BF16, kind=sk) for i, (_, ln, _) in enumerate(segs)]
    A_s = [P.dram("A_s%d" % d, [384, TT], F32, kind=sk) for d in range(2)]
    R_s = [P.dram("R_s%d" % d, [384, TT], F32, kind=sk) for d in range(2)]
    B_s = [P.dram("B_s%d" % d, [384, TT], F32, kind=sk) for d in range(2)]
    K_s = [P.dram("K_s%d" % d, [384, TT], F32, kind=sk) for d in range(2)]
    eL_s = [P.dram("eL_s%d" % d, [384, NCH], F32, kind=sk) for d in range(2)]
    V_s = P.dram("V_s", [TT, 384], F32, kind=sk)
    g_s = P.dram("g_s", [384, TT], F32, kind=sk)
    bon_s = P.dram("bon_s", [384, TT], F32, kind=sk)
    Y_s = [P.dram("Y_s%d" % d, [TT, 384], F32, kind=sk) for d in range(2)]

    ps = [P.ps("ps%d" % i, [128, 512]) for i in range(8)]

    def ld(name, src, shape, dt=F32, view=None):
        t = P.sb(name, shape, dt)
        P.dma("sp", t[:], src[:] if view is None else view)
        return t
    c_sb = ld("c_sb", cT, [128, 8, 2]); modb_s = ld("modb_s", modbc, [128, 16]); n1_s = ld("n1_s", n1c, [128, 8])
    vec_s = ld("vec_s", vecs, [128, 3, 5]); wa_s = ld("wa_s", w0a0, [128, 2, 2, 3])
    w2_s = ld("w2_s", w2d, [128, 384]); a2_s = ld("a2_s", a2d, [128, 384]); g2_s = ld("g2_s", g2d, [128, 384])
    bones_s = ld("bones_s", bonesd, [128, 128]); onesD_s = ld("onesD_s", onesDd, [128, 128])
    rmask_s = ld("rmask_s", rmaskd, [128, 512])
    omk = P.sb("omk", [128, 3])
    P.ts("dve", omk[:], vec_s[:, :, 1], -1.0, ALU.mult, 1.0, ALU.add)
    sc_sb = P.sb("sc_sb", [128, 8, 2])
    P.act(sc_sb[:], c_sb[:], AF.Silu)
    W64f = P.sb("W64f", [128, 2, 256])
    P.dma("sp", W64f[:], V(W64d, W64d.ap.rearrange("s p n -> p s n")))
    W64b = P.sb("W64b", [128, 2, 256], BF16)
    P.copy("dve", W64b[:], W64f[:])
    Zs = [P.sb("Zs%d" % i, [128, ln // 128, 256], BF16) for i, (_, ln, _) in enumerate(segs)]
    zero_b = P.sb("zero_b", [128, 8, 1], BF16)
    P.memset("dve", zero_b[:], 0.0)
    FT = P.sb("FT", [128, TT], BF16)

    m_w = P.mark()
    modc = P.sb("modc", [128, 16, 2])
    wmod = P.sb("wmod", [128, 8, 2])
    Wj = [P.sb("Wj%d" % i, [128, 8, 1536], BF16) for i in range(3)]
    Wf = P.sb("Wf", [128, 8, 128], BF16)
    m_phase = P.mark()
    xblk = P.sb("xblk", [128, 8, 512])
    hT = P.sb("hT", [128, 8, 512], BF16)
    sqb = [P.sb("sqb%d" % i, [128, 512]) for i in range(2)]
    rstd = P.sb("rstd", [128, 512])
    tmpf = [P.sb("tmpf%d" % i, [128, 512]) for i in range(2)]
    modw_v = modw.ap.rearrange("(k p) n -> p k n", p=128)
    for c4 in range(4):
        P.dma("sp", xblk[:], V(modw, modw_v[:, :, c4 * 512:(c4 + 1) * 512]))
        for q4 in range(4):
            cc = c4 * 4 + q4
            pr = ps[cc % 2]
            for k in range(8):
                P.mm(pr[:, 0:2], xblk[:, k, q4 * 128:(q4 + 1) * 128], sc_sb[:, k, :], start=(k == 0), stop=(k == 7))
            P.ts("dve", modc[:, cc, :], pr[:, 0:2], modb_s[:, cc:cc + 1], ALU.add)
    P.op("dve", lambda: nc.vector.scalar_tensor_tensor(out=wmod.ap, in0=modc.ap[:, 8:16, :], scalar=1.0,
                                                       in1=n1_s.ap.unsqueeze(2).broadcast_to([128, 8, 2]),
                                                       op0=ALU.add, op1=ALU.mult), [modc[:], n1_s[:]], [wmod[:]])
    mk_mu = P.mark()
    mu_r = [P.sb("mu_r%d" % i, [128, 1536]) for i in range(3)]
    P.dma("sp", mu_r[1][:], V(mup, mup.ap.partition_broadcast(128)))
    P.dma("sp", mu_r[2][:], V(mun, mun.ap.partition_broadcast(128)))
    P.tt("dve", mu_r[0][:], mu_r[1][:], mu_r[2][:], ALU.add)
    P.ts("dve", mu_r[0][:], mu_r[0][:], -1.0, ALU.mult, 1.0, ALU.add)
    wsel_v = wsel.ap.rearrange("(k p) n -> p k n", p=128)
    for c3 in range(3):
        P.dma("sp", xblk[:], V(wsel, wsel_v[:, :, c3 * 512:(c3 + 1) * 512]))
        for j in range(3):
            mb = mu_r[j].ap[:, c3 * 512:(c3 + 1) * 512].unsqueeze(1).broadcast_to([128, 8, 512])
            P.op("dve" if j != 1 else "pool",
                 (lambda j=j, mb=mb: P.e["dve" if j != 1 else "pool"].tensor_tensor(
                     out=Wj[j].ap[:, :, c3 * 512:(c3 + 1) * 512], in0=xblk.ap, in1=mb, op=ALU.mult)),
                 [xblk[:], mu_r[j][:]], [Wj[j][:]])
    P.dma("sp", xblk[:, :, 0:128], V(wsel, wsel_v[:, :, 1536:1664]))
    P.copy("dve", Wf[:], xblk[:, :, 0:128])
    P.barrier()
    P.release(mk_mu)

    for si, (tok0, ln, cls) in enumerate(segs):
        src = ctxT if si == 0 else xT
        P.dma("sp", hT_s[si][:, :, 0:1], zero_b[:], allow_slow_non_contiguous=True)
        P.dma("sp", hT_s[si][:, :, ln + 1:ln + 2], zero_b[:], allow_slow_non_contiguous=True)
        for c0 in range(0, ln, 512):
            w = min(512, ln - c0)
            P.dma("sp", xblk[:, :, 0:w], V(src, src.ap.rearrange("(k p) t -> p k t", p=128)[:, :, c0:c0 + w]))
            pr = ps[0]
            for k in range(8):
                sq = sqb[k % 2]
                P.act(sq[:, 0:w], xblk[:, k, 0:w], AF.Square)
                P.mm(pr[:, 0:w], onesD_s[:], sq[:, 0:w], start=(k == 0), stop=(k == 7))
            P.rsqrt(rstd[:, 0:w], pr[:, 0:w], 1.0 / D, 1e-6)
            for k in range(8):
                tf = tmpf[k % 2]
                P.tt("dve", tf[:, 0:w], xblk[:, k, 0:w], rstd[:, 0:w], ALU.mult)
                P.ts("pool", hT[:, k, 0:w], tf[:, 0:w], wmod[:, k, cls:cls + 1], ALU.mult, modc[:, k, cls:cls + 1], ALU.add)
            P.dma("sp", hT_s[si][:, :, 1 + c0:1 + c0 + w], hT[:, :, 0:w])
    if upto < 1:
        P.finish()
        return P
    P.barrier()
    P.release(m_phase)

    hwin = P.sb("hwin", [128, 8, 514], BF16)
    zt_ = {nm: [P.sb("z%s%d" % (nm, hp), [128, 512]) for hp in range(3)] for nm in ("r", "k", "v")}
    tw = P.sb("tw", [128, 512]); xa_s = P.sb("xa_s", [128, 512]); sg = P.sb("sg", [128, 512])
    fTb = P.sb("fTb", [128, 512], BF16)
    NTMP = 14
    tp = [P.sb("tp%d" % i, [128, 512]) for i in range(NTMP)]
    eLt = [P.sb("eLt%d" % i, [128, 8]) for i in range(2)]
    vst = [P.sb("vst%d" % i, [128, 128]) for i in range(2)]
    SHIFTS = ((0, 0), (1, -1), (2, 1))
    for si, (tok0, ln, cls) in enumerate(segs):
        for c0 in range(0, ln, 512):
            n = min(512, ln - c0)
            ncb = n // CH
            g0 = tok0 + c0
            P.dma("sp", hwin[:, :, 0:n + 2], hT_s[si][:, :, c0:c0 + n + 2])
            pi = [0]

            def nps():
                pi[0] = (pi[0] + 1) % 8
                return ps[pi[0]]

            def proj(col0, shifted=True):
                pr = nps()
                sh = SHIFTS if shifted else ((None, 0),)
                nmm = len(sh) * 8
                i = 0
                for (j, dj) in sh:
                    for k in range(8):
                        wt = Wj[j][:, k, col0:col0 + 128] if shifted else Wf[:, k, :]
                        P.mm(pr[:, 0:n], wt, hwin[:, k, 1 + dj:1 + dj + n], start=(i == 0), stop=(i == nmm - 1))
                        i += 1
                return pr
            for hp in range(3):
                P.copy("act", zt_["r"][hp][:, 0:n], proj(hp * 128)[:, 0:n])
                P.copy("dve", zt_["k"][hp][:, 0:n], proj(384 + hp * 128)[:, 0:n])
                P.copy("act", zt_["v"][hp][:, 0:n], proj(768 + hp * 128)[:, 0:n])
            P.act(tw[:, 0:n], proj(1152)[:, 0:n], AF.Tanh)
            P.copy("dve", xa_s[:, 0:n], proj(1280)[:, 0:n])
            P.act(sg[:, 0:n], proj(1408)[:, 0:n], AF.Sigmoid)
            P.copy("act", fTb[:, 0:n], proj(0, shifted=False)[:, 0:n])
            for tt_ in range(n // 128):
                pr = nps()
                P.mm(pr[:, 0:256], fTb[:, tt_ * 128:(tt_ + 1) * 128], W64b[:, si, :])
                P.copy("dve", Zs[si][:, c0 // 128 + tt_, :], pr[:, 0:256])
            for hp in range(3):
                zr, zk, zv = zt_["r"][hp], zt_["k"][hp], zt_["v"][hp]
                rows = slice(hp * 128, (hp + 1) * 128)
                cols = slice(g0, g0 + n)
                kkc, kac, rkc = vec_s[:, hp, 0:1], vec_s[:, hp, 1:2], vec_s[:, hp, 2:3]
                pr = nps()
                P.mm(pr[:, 0:n], g2_s[:, hp * 128:(hp + 1) * 128], sg[:, 0:n])
                P.copy("act", tp[0][:, 0:n], pr[:, 0:n])
                P.dma("sp", g_s[rows, cols], tp[0][:, 0:n])
                kk, kkn, aneg = tp[1], tp[2], tp[3]
                P.ts("dve", kk[:, 0:n], zk[:, 0:n], kkc, ALU.mult)
                P.act(tp[4][:, 0:n], kk[:, 0:n], AF.Square)
                pr = nps()
                P.mm(pr[:, 0:n], bones_s[:], tp[4][:, 0:n])
                P.act(tp[4][:, 0:n], pr[:, 0:n], AF.Sqrt)
                P.ts("dve", tp[4][:, 0:n], tp[4][:, 0:n], 1e-12, ALU.max)
                P.op("dve", lambda: nc.vector.reciprocal(out=tp[4].ap[:, 0:n], in_=tp[4].ap[:, 0:n]), [tp[4][:]], [tp[4][:]])
                P.tt("dve", kkn[:, 0:n], kk[:, 0:n], tp[4][:, 0:n], ALU.mult)
                P.ts("pool", aneg[:, 0:n], kkn[:, 0:n], -1.0, ALU.mult)
                bon = tp[5]
                for d in range(2):
                    dl = slice(d * 64, (d + 1) * 64)
                    lw, ag, kd, bb, F_, L_, Lx = tp[6], tp[7], tp[8], tp[9], tp[10], tp[11], tp[12]
                    pr = nps()
                    P.mm(pr[:, 0:n], w2_s[dl, hp * 128:(hp + 1) * 128], tw[dl, 0:n])
                    P.act(lw[:, 0:n], pr[:, 0:n], AF.Sigmoid, bias=wa_s[:, 0, d, hp:hp + 1])
                    P.ts("dve", lw[:, 0:n], lw[:, 0:n], NEG_EXP_HALF, ALU.mult)
                    pr = nps()
                    P.mm(pr[:, 0:n], a2_s[dl, hp * 128:(hp + 1) * 128], xa_s[dl, 0:n])
                    P.act(ag[:, 0:n], pr[:, 0:n], AF.Sigmoid, bias=wa_s[:, 1, d, hp:hp + 1])
                    P.ts("dve", kd[:, 0:n], ag[:, 0:n], kac, ALU.mult, omk[:, hp:hp + 1], ALU.add)
                    P.tt("dve", kd[:, 0:n], kd[:, 0:n], zk[:, 0:n], ALU.mult)
                    P.tt("pool", bb[:, 0:n], kkn[:, 0:n], ag[:, 0:n], ALU.mult)
                    P.stt("dve", tp[13][:, 0:n], zr[:, 0:n], rkc, kd[:, 0:n], ALU.mult, ALU.mult)
                    pr = nps()
                    P.mm(pr[:, 0:n], bones_s[:], tp[13][:, 0:n])
                    if d == 0:
                        P.tt("dve", bon[:, 0:n], pr[:, 0:n], zv[:, 0:n], ALU.mult)
                    else:
                        P.tt("dve", tp[13][:, 0:n], pr[:, 0:n], zv[:, 0:n], ALU.mult)
                        P.tt("pool", bon[:, 0:n], bon[:, 0:n], tp[13][:, 0:n], ALU.add)
                        P.dma("sp", bon_s[rows, cols], bon[:, 0:n])
                    P.op("dve", lambda: nc.vector.tensor_tensor_scan(out=F_.ap[:, 0:n], data0=rmask_s.ap[:, 0:n], data1=lw.ap[:, 0:n],
                                                                     initial=0.0, op0=ALU.mult, op1=ALU.add),
                         [rmask_s[:], lw[:]], [F_[:]])
                    if d == 0:
                        P.tt("pool", Lx[:, 0:n], F_[:, 0:n], lw[:, 0:n], ALU.subtract)
                        Lsrc = F_
                    else:
                        P.tt("dve", Lx[:, 0:n], lw[:, 0:n], F_[:, 0:n], ALU.subtract)
                        f3 = F_.ap[:, 0:n].rearrange("p (c t) -> p c t", t=CH)
                        l3 = L_.ap[:, 0:n].rearrange("p (c t) -> p c t", t=CH)
                        x3 = Lx.ap[:, 0:n].rearrange("p (c t) -> p c t", t=CH)
                        tot = f3[:, :, CH - 1:CH].broadcast_to([128, ncb, CH])
                        P.op("dve", (lambda l3=l3, x3=x3, tot=tot: nc.vector.tensor_tensor(out=l3, in0=x3, in1=tot, op=ALU.add)),
                             [Lx[:], F_[:]], [L_[:]])
                        P.tt("pool", Lx[:, 0:n], L_[:, 0:n], lw[:, 0:n], ALU.subtract)
                        Lsrc = L_
                    e1, e2, e3 = tp[13], tp[6], tp[10] if d == 1 else tp[11]
                    P.act(e1[:, 0:n], Lx[:, 0:n], AF.Exp)
                    P.act(e3[:, 0:n], Lsrc[:, 0:n], AF.Exp, scale=-1.0)
                    P.act(e2[:, 0:n], Lsrc[:, 0:n], AF.Exp)
                    elt = eLt[d]
                    e23 = e2.ap[:, 0:n].rearrange("p (c t) -> p c t", t=CH)
                    pos = CH - 1 if d == 0 else 0
                    P.op("pool", (lambda elt=elt, e23=e23, pos=pos: nc.gpsimd.tensor_copy(out=elt.ap[:, 0:ncb], in_=e23[:, :, pos])),
                         [e2[:]], [elt[:]])
                    P.dma("sp", eL_s[d][rows, g0 // CH:g0 // CH + ncb], elt[:, 0:ncb])
                    P.tt("dve", e1[:, 0:n], e1[:, 0:n], aneg[:, 0:n], ALU.mult)
                    P.dma("sp", A_s[d][rows, cols], e1[:, 0:n])
                    P.tt("pool", e2[:, 0:n], e2[:, 0:n], zr[:, 0:n], ALU.mult)
                    P.dma("sp", R_s[d][rows, cols], e2[:, 0:n])
                    P.tt("dve", bb[:, 0:n], bb[:, 0:n], e3[:, 0:n], ALU.mult)
                    P.dma("sp", B_s[d][rows, cols], bb[:, 0:n])
                    P.tt("pool", kd[:, 0:n], kd[:, 0:n], e3[:, 0:n], ALU.mult)
                    P.dma("sp", K_s[d][rows, cols], kd[:, 0:n])
                for tt_ in range(n // 128):
                    pr = nps()
                    i = 0
                    for (j, dj) in SHIFTS:
                        for k in range(8):
                            P.mm(pr[:, 0:128], hwin[:, k, 1 + dj + tt_ * 128:1 + dj + tt_ * 128 + 128],
                                 Wj[j][:, k, 768 + hp * 128:768 + (hp + 1) * 128], start=(i == 0), stop=(i == 23))
                            i += 1
                    vs_ = vst[tt_ % 2]
                    P.copy("act", vs_[:], pr[:, 0:128])
                    P.dma("sp", V_s[g0 + tt_ * 128:g0 + (tt_ + 1) * 128, hp * 128:(hp + 1) * 128], vs_[:])
    if upto < 2:
        P.finish()
        return P
    P.barrier()
    P.release(m_w)

    m_s = P.mark()
    ncc = CL // CH
    order = [list(range(NCH)), list(range(ncc - 1, -1, -1)) + list(range(NCH - 1, ncc - 1, -1))]
    mAT_s = P.sb("mAT_s", [128, 2, 128]); mN_s = P.sb("mN_s", [64, 2, 64]); id_s = P.sb("id_s", [64, 64])
    P.dma("sp", mAT_s[:], V(mAT, mAT.ap.rearrange("d p n -> p d n")))
    P.dma("sp", mN_s[:], V(mN, mN.ap.rearrange("d p n -> p d n")))
    P.dma("sp", id_s[:], id64[:])
    mAK_s = P.sb("mAK_s", [128, 2, 64])
    P.dma("sp", mAK_s[:], V(mAK, mAK.ap.rearrange("d p n -> p d n")))
    kj = lambda t_: t_.ap.rearrange("(j k) t -> k j t", k=64)
    sbd = lambda nm, shp: [[P.sb("%s_%d_%d" % (nm, d, i), shp) for i in range(2)] for d in range(2)]
    AR = sbd("AR", [128, 6, 2, 64]); BK = sbd("BK", [64, 6, 2, 64]); UV = sbd("UV", [128, 6, 64])
    for d in range(2):
        for i in range(2):
            P.memset("pool", AR[d][i][:], 0.0)
            P.memset("pool", UV[d][i][:], 0.0)
    eLa = [P.sb("eLa%d" % d, [64, 6, NCH]) for d in range(2)]
    for d in range(2):
        P.dma("sp", eLa[d][:], V(eL_s[d], kj(eL_s[d])))
    Xb = sbd("Xb", [64, 6, 64]); Zb = sbd("Zb", [64, 6, 64])
    one = lambda nm, shp: [P.sb("%s_%d" % (nm, d), shp) for d in range(2)]
    ATs = one("ATs", [128, 6, 128]); Rm = one("Rm", [64, 6, 64]); BKt = one("BKt", [128, 6, 64])
    W0s = one("W0s", [64, 6, 64]); Ys = one("Ys", [64, 6, 64]); tS = one("tS", [64, 6, 64]); ST = one("ST", [128, 6, 64])
    AakP = one("AakP", [128, 6, 64])
    for d in range(2):
        P.memset("pool", ST[d][:], 0.0)
    ring = [0, 0]

    def nb(d):
        ring[d] = (ring[d] + 1) % 4
        return ps[d * 4 + ring[d]]

    def loads(n):
        for d in range(2):
            c = order[d][n]
            cs = slice(c * CH, (c + 1) * CH)
            pb = n % 2
            P.dma("sp", AR[d][pb][0:64, :, 0, :], V(A_s[d], kj(A_s[d])[:, :, cs]))
            P.dma("sp", AR[d][pb][0:64, :, 1, :], V(R_s[d], kj(R_s[d])[:, :, cs]))
            P.dma("sp", BK[d][pb][:, :, 0, :], V(B_s[d], kj(B_s[d])[:, :, cs]))
            P.dma("sp", BK[d][pb][:, :, 1, :], V(K_s[d], kj(K_s[d])[:, :, cs]))
            P.dma("sp", UV[d][pb][64:128, :, :], V(V_s, V_s.ap[cs, :].rearrange("t (j v) -> t j v", v=64)))

    def v3(t_, rows=slice(0, 64), width=384):
        return V(t_, t_.ap[rows, 0:width].rearrange("p (j v) -> p j v", j=6))

    loads(0)
    for n in range(NCH):
        pb = n % 2
        if n + 1 < NCH:
            loads(n + 1)
        ar = [AR[d][pb] for d in range(2)]; bk = [BK[d][pb] for d in range(2)]; uv = [UV[d][pb] for d in range(2)]
        for d in range(2):
            for g in range(2):
                pa = nb(d)
                for jj in range(3):
                    j = g * 3 + jj
                    P.mm(pa[:, jj * 128:(jj + 1) * 128], V(bk[d], bk[d].ap[:, j].rearrange("p a i -> p (a i)")),
                         V(ar[d], ar[d].ap[0:64, j].rearrange("p a i -> p (a i)")))
                mk_ = mAK_s.ap[:, d, :].unsqueeze(1).broadcast_to([128, 3, 64])
                P.op("dve", (lambda pa=pa, d=d, g=g, mk_=mk_: nc.vector.tensor_tensor(
                    out=AakP[d].ap[:, g * 3:(g + 1) * 3, :], in0=pa.ap[:, 0:384].rearrange("p (j n) -> p j n", j=3)[:, :, 0:64], in1=mk_, op=ALU.mult)),
                    [pa[:], mAK_s[:]], [AakP[d][:]])
                mb = mAT_s.ap[:, d, :].unsqueeze(1).broadcast_to([128, 3, 128])
                P.op("dve", (lambda pa=pa, d=d, g=g, mb=mb: nc.vector.tensor_tensor(
                    out=ATs[d].ap[:, g * 3:(g + 1) * 3, :], in0=pa.ap[:, 0:384].rearrange("p (j n) -> p j n", j=3), in1=mb, op=ALU.mult)),
                    [pa[:], mAT_s[:]], [ATs[d][:]])
        pn = [nb(d) for d in range(2)]
        for d in range(2):
            for j in range(6):
                P.mm(pn[d][0:64, j * 64:(j + 1) * 64], ar[d][0:64, j, 0, :], bk[d][:, j, 0, :])
            mb = mN_s.ap[:, d, :].unsqueeze(1).broadcast_to([64, 6, 64])
            P.op("dve", (lambda d=d, mb=mb: nc.vector.tensor_tensor(out=Xb[d][0].ap, in0=pn[d].ap[0:64, 0:384].rearrange("p (j n) -> p j n", j=6),
                                                                    in1=mb, op=ALU.mult)), [pn[d][:], mN_s[:]], [Xb[d][0][:]])
            ib = id_s.ap.unsqueeze(1).broadcast_to([64, 6, 64])
            P.op("pool", (lambda d=d, ib=ib: nc.gpsimd.tensor_tensor(out=Rm[d].ap, in0=ATs[d].ap[0:64, :, 0:64], in1=ib, op=ALU.add)),
                 [ATs[d][:], id_s[:]], [Rm[d][:]])
        pk = [nb(d) for d in range(2)]
        for d in range(2):
            for j in range(6):
                P.mm(pk[d][:, j * 64:(j + 1) * 64], V(bk[d], bk[d].ap[:, j].rearrange("p a i -> p (a i)")), id_s[:])
            P.copy("act", BKt[d][:], v3(pk[d], slice(0, 128)))
        Xp = [Xb[d][0] for d in range(2)]
        Zp = [V(ATs[d], ATs[d].ap[0:64, :, 0:64]) for d in range(2)]
        for lv in range(1, 6):
            px = [nb(d) for d in range(2)]
            for d in range(2):
                for j in range(6):
                    P.mm(px[d][0:64, j * 64:(j + 1) * 64], V(Zp[d].t, Zp[d].ap[:, j, :]), Xp[d][:, j, :])
            Xn = [Xb[d][lv % 2] for d in range(2)]
            for d in range(2):
                P.copy("act", Xn[d][:], v3(px[d]))
            if lv < 5:
                pz = [nb(d) for d in range(2)]
                for d in range(2):
                    for j in range(6):
                        P.mm(pz[d][0:64, j * 64:(j + 1) * 64], Xp[d][:, j, :], V(Zp[d].t, Zp[d].ap[:, j, :]))
                Zn = [Zb[d][lv % 2] for d in range(2)]
                for d in range(2):
                    P.copy("dve", Zn[d][:], v3(pz[d]))
            prr = [nb(d) for d in range(2)]
            for d in range(2):
                for j in range(6):
                    P.mm(prr[d][0:64, j * 64:(j + 1) * 64], Xn[d][:, j, :], Rm[d][:, j, :])
            for d in range(2):
                P.tt("dve", Rm[d][:], v3(prr[d]), Rm[d][:], ALU.add)
            Xp = Xn
            if lv < 5:
                Zp = [Zn[d][:] for d in range(2)]
        pw = [nb(d) for d in range(2)]
        for d in range(2):
            for j in range(6):
                o_ = pw[d][0:64, j * 64:(j + 1) * 64]
                P.mm(o_, ar[d][:, j, 0, :], ST[d][:, j, :], start=True, stop=False)
                P.mm(o_, AakP[d][:, j, :], uv[d][:, j, :], start=False, stop=True)
            P.copy("act", W0s[d][:], v3(pw[d]))
        pu_ = [nb(d) for d in range(2)]
        for d in range(2):
            for j in range(6):
                P.mm(pu_[d][0:64, j * 64:(j + 1) * 64], Rm[d][:, j, :], W0s[d][:, j, :])
            P.copy("dve", uv[d][0:64, :, :], v3(pu_[d]))
        py = [nb(d) for d in range(2)]
        for d in range(2):
            for j in range(6):
                o_ = py[d][0:64, j * 64:(j + 1) * 64]
                P.mm(o_, ar[d][:, j, 1, :], ST[d][:, j, :], start=True, stop=False)
                P.mm(o_, ATs[d][:, j, 64:128], uv[d][:, j, :], start=False, stop=True)
            P.copy("act", Ys[d][:], v3(py[d]))
            c = order[d][n]
            P.dma("sp", V(Y_s[d], Y_s[d].ap[c * CH:(c + 1) * CH, :].rearrange("t (j v) -> t j v", v=64)), Ys[d][:])
        pst = [nb(d) for d in range(2)]
        for d in range(2):
            for j in range(6):
                P.mm(pst[d][0:64, j * 64:(j + 1) * 64], BKt[d][:, j, :], uv[d][:, j, :])
            P.tt("dve", tS[d][:], v3(pst[d]), ST[d][0:64, :, :], ALU.add)
            eb = eLa[d].ap[:, :, order[d][n]].unsqueeze(2).broadcast_to([64, 6, 64])
            P.op("pool", (lambda d=d, eb=eb: nc.gpsimd.tensor_tensor(out=ST[d].ap[0:64], in0=tS[d].ap, in1=eb, op=ALU.mult)),
                 [tS[d][:], eLa[d][:]], [ST[d][:]])
    if upto < 3:
        P.finish()
        return P
    P.barrier()
    P.release(m_s)

    m_n = P.mark()
    import math
    ccs = P.sb("ccs", [128, CL // 128, 2, CL], BF16)
    cstage = P.sb("cstage", [128, CL // 128, 2, CL])
    for c_ in range(2):
        P.dma("sp", cstage[:, :, c_, :], V(ctxCS, ctxCS.ap[c_].rearrange("(a p) n -> p a n", p=128)))
    P.copy("dve", ccs[:], cstage[:])
    pr = ps[0]
    ntc = CL // 128
    for ti in range(ntc):
        P.mm(pr[:, 0:CL], Zs[0][:, ti, 0:128], ccs[:, ti, 0, :], start=(ti == 0), stop=False)
        P.mm(pr[:, 0:CL], Zs[0][:, ti, 128:256], ccs[:, ti, 1, :], start=False, stop=(ti == ntc - 1))
    P.copy("act", FT[:, 0:CL], pr[:, 0:CL])
    NTT = S // 128
    PW = min(S, 2048)
    HP = PW // 128
    NBLK = PW // 512
    tA = P.sb("tA", [128, NTT, 128]); tB = P.sb("tB", [128, NTT, S // 128])
    P.dma("sp", tA[:], V(tabA, tabA.ap.rearrange("(a p) n -> p a n", p=128)))
    P.dma("sp", tB[:], V(tabB, tabB.ap.rearrange("(a p) n -> p a n", p=128)))
    negpi = P.sb("negpi", [128, 2])
    SHR = 1.0 - 1e-6
    P.memset("dve", negpi[:, 0:1], -math.pi * SHR)
    P.memset("dve", negpi[:, 1:2], -0.5 * math.pi * SHR)
    kang = 2.0 * math.pi / S * SHR
    mm_ = [P.sb("mm_%d" % i, [128, PW]) for i in range(2)]
    mc_ = [P.sb("mc_%d" % i, [128, PW]) for i in range(2)]
    Ct = [P.sb("Ct%d" % i, [128, PW], BF16) for i in range(2)]
    St = [P.sb("St%d" % i, [128, PW], BF16) for i in range(2)]
    for pz_ in range(S // PW):
        for ti in range(NTT):
            b2 = ti % 2
            m3 = mm_[b2].ap.rearrange("p (h l) -> p h l", l=128)
            i0 = tB.ap[:, ti, pz_ * HP:(pz_ + 1) * HP].unsqueeze(2).broadcast_to([128, HP, 128])
            i1 = tA.ap[:, ti, :].unsqueeze(1).broadcast_to([128, HP, 128])
            P.op("dve", (lambda m3=m3, i0=i0, i1=i1: nc.vector.tensor_tensor(out=m3, in0=i0, in1=i1, op=ALU.add)),
                 [tA[:], tB[:]], [mm_[b2][:]])
            P.ts("dve", mc_[b2][:], mm_[b2][:], float(S), ALU.is_ge, -float(S), ALU.mult)
            P.tt("pool", mm_[b2][:], mm_[b2][:], mc_[b2][:], ALU.add)
            P.act(St[b2][:], mm_[b2][:], AF.Sin, bias=negpi[:, 0:1], scale=kang)
            P.ts("dve", mc_[b2][:], mm_[b2][:], 0.75 * S, ALU.is_ge, -float(S), ALU.mult)
            P.tt("pool", mc_[b2][:], mc_[b2][:], mm_[b2][:], ALU.add)
            P.act(Ct[b2][:], mc_[b2][:], AF.Sin, bias=negpi[:, 1:2], scale=kang)
            for blk in range(NBLK):
                P.mm(ps[blk][:], Zs[1][:, ti, 0:128], Ct[b2][:, blk * 512:(blk + 1) * 512], start=(ti == 0), stop=False)
                P.mm(ps[blk][:], Zs[1][:, ti, 128:256], St[b2][:, blk * 512:(blk + 1) * 512], start=False, stop=(ti == NTT - 1))
        for blk in range(NBLK):
            c0 = CL + pz_ * PW + blk * 512
            P.copy("act" if blk % 2 == 0 else "dve", FT[:, c0:c0 + 512], ps[blk][:])
    if upto < 4:
        P.finish()
        return P
    P.barrier()
    P.release(m_n)

    wo_s = P.sb("wo_s", [128, 4, D], BF16)
    wstage = P.sb("wstage", [128, 4, D])
    P.dma("sp", wstage[:], wod[:])
    P.copy("dve", wo_s[:], wstage[:])
    yf = [P.sb("yf%d" % i, [128, 384]) for i in range(2)]
    yb = [P.sb("yb%d" % i, [128, 384]) for i in range(2)]
    dd = P.sb("dd", [128, 384]); sq = P.sb("sq", [128, 384]); st6 = P.sb("st6", [128, 6]); st6b = P.sb("st6b", [128, 6])
    idf = P.sb("idf", [128, 128])
    P.dma("sp", idf[0:64, 0:64], id64[:])
    bonT = [P.sb("bonT%d" % i, [128, 512]) for i in range(3)]
    gT = [P.sb("gT%d" % i, [128, 512]) for i in range(3)]
    oT = [P.sb("oT%d" % i, [128, 512], BF16) for i in range(3)]
    ot32 = P.sb("ot32", [128, 512])
    ostg = [P.sb("ostg%d" % i, [128, 512]) for i in range(2)]
    P.memset("dve", idf[0:64, 64:128], 0.0)
    P.memset("dve", idf[64:128, 0:64], 0.0)
    P.dma("sp", idf[64:128, 64:128], id64[:])
    for (tok0, ln, cls) in segs:
        for c0 in range(0, ln, 512):
            n = min(512, ln - c0)
            g0 = tok0 + c0
            for hp in range(3):
                P.dma("sp", bonT[hp][:, 0:n], bon_s[hp * 128:(hp + 1) * 128, g0:g0 + n])
                P.dma("sp", gT[hp][:, 0:n], g_s[hp * 128:(hp + 1) * 128, g0:g0 + n])
            for tt_ in range(n // 128):
                r0 = g0 + tt_ * 128
                a_, b_ = yf[tt_ % 2], yb[tt_ % 2]
                P.dma("sp", a_[:], Y_s[0][r0:r0 + 128, :])
                P.dma("sp", b_[:], Y_s[1][r0:r0 + 128, :])
                P.tt("pool", a_[:], a_[:], b_[:], ALU.add)
                a3 = a_.ap.rearrange("p (j v) -> p j v", v=64)
                d3 = dd.ap.rearrange("p (j v) -> p j v", v=64)
                P.op("dve", (lambda a3=a3: nc.vector.tensor_reduce(out=st6.ap, in_=a3, axis=AX.X, op=ALU.add)), [a_[:]], [st6[:]])
                P.ts("dve", st6[:], st6[:], 1.0 / 64, ALU.mult)
                P.op("dve", (lambda a3=a3, d3=d3: nc.vector.tensor_tensor(out=d3, in0=a3, in1=st6.ap.unsqueeze(2).broadcast_to([128, 6, 64]),
                                                                          op=ALU.subtract)), [a_[:], st6[:]], [dd[:]])
                P.act(sq[:], dd[:], AF.Square)
                P.op("dve", lambda: nc.vector.tensor_reduce(out=st6b.ap, in_=sq.ap.rearrange("p (j v) -> p j v", v=64), axis=AX.X, op=ALU.add),
                     [sq[:]], [st6b[:]])
                P.rsqrt(st6b[:], st6b[:], 1.0 / 64, 64e-5)
                P.op("dve", (lambda d3=d3: nc.vector.tensor_tensor(out=d3, in0=d3, in1=st6b.ap.unsqueeze(2).broadcast_to([128, 6, 64]),
                                                                   op=ALU.mult)), [dd[:], st6b[:]], [dd[:]])
                for hp in range(3):
                    P.tr(ps[hp][:, tt_ * 128:(tt_ + 1) * 128], dd[:, hp * 128:(hp + 1) * 128], idf[:])
            for hp in range(3):
                P.ts("dve", ot32[:, 0:n], ps[hp][:, 0:n], vec_s[:, hp, 3:4], ALU.mult, vec_s[:, hp, 4:5], ALU.add)
                P.tt("pool", ot32[:, 0:n], ot32[:, 0:n], bonT[hp][:, 0:n], ALU.add)
                P.tt("dve", oT[hp][:, 0:n], ot32[:, 0:n], gT[hp][:, 0:n], ALU.mult)
            for cc in range(8):
                pw_ = ps[4 + cc % 2]
                for hp in range(3):
                    P.mm(pw_[:, 0:n], wo_s[:, hp, cc * 128:(cc + 1) * 128], oT[hp][:, 0:n], start=(hp == 0), stop=False)
                P.mm(pw_[:, 0:n], wo_s[:, 3, cc * 128:(cc + 1) * 128], FT[:, g0:g0 + n], start=False, stop=True)
                og = ostg[cc % 2]
                P.copy("act", og[:, 0:n], pw_[:, 0:n])
                P.dma("sp", paT[cc * 128:(cc + 1) * 128, g0:g0 + n], og[:, 0:n])
    P.finish()
    return P


def l0_consts(mod_w, mod_b, norm1, w_in, shift_prev, shift_next, w0, w2, a0, a2, g2, k_k, k_a, r_k, lnx_g, lnx_b,
              w_out, s, S, CL):
    f = np.float32
    c384 = slice(384 * s, 384 * (s + 1))
    sel = np.concatenate([np.arange(384 * s, 384 * (s + 1)), 768 + np.arange(384 * s, 384 * (s + 1)),
                          1536 + np.arange(384 * s, 384 * (s + 1)), np.arange(2304, 2688)])
    fsel = 2688 + np.arange(128 * s, 128 * (s + 1))
    wsel = np.ascontiguousarray(np.concatenate([w_in[:, sel], w_in[:, fsel]], 1))
    ch = lambda v: np.asarray(v, f).reshape(-1)[c384].reshape(3, 128).T
    vecs = np.ascontiguousarray(np.stack([ch(k_k), ch(k_a), ch(r_k), ch(lnx_g), ch(lnx_b)], -1))
    w0a0 = np.zeros((128, 2, 2, 3), f)
    for d in range(2):
        w0a0[:, 0, d, :] = ch(w0[d])
        w0a0[:, 1, d, :] = ch(a0[d])
    wo = np.zeros((128, 4, D), f)
    wo[:, 0:3, :] = w_out[c384].reshape(3, 128, D).transpose(1, 0, 2)
    wo[:, 3, :] = w_out[768 + 128 * s:768 + 128 * (s + 1)]
    ii = np.arange(64)
    mAT = np.zeros((2, 128, 128), f)
    mN = np.zeros((2, 64, 64), f)
    mAK = np.zeros((2, 128, 64), f)
    for d in range(2):
        before = (ii[:, None] < ii[None, :]) if d == 0 else (ii[:, None] > ii[None, :])
        beq = before | (ii[:, None] == ii[None, :])
        for r0 in (0, 64):
            mAT[d, r0:r0 + 64, 0:64] = before
            mAT[d, r0:r0 + 64, 64:128] = beq
        mN[d] = before.T
        mAK[d, 64:128, :] = before
    rmask = np.ones((128, 512), f)
    rmask[:, ::64] = 0
    bones = np.zeros((128, 128), f)
    bones[0:64, 0:64] = 1
    bones[64:, 64:] = 1
    cc = np.arange(64)
    ang = 2 * np.pi * ((cc[:, None] * cc[None, :]) % 64) / 64.0
    C64 = np.zeros((128, 128)); S64 = np.zeros((128, 128))
    for g in range(2):
        C64[g * 64:(g + 1) * 64, g * 64:(g + 1) * 64] = np.cos(ang)
        S64[g * 64:(g + 1) * 64, g * 64:(g + 1) * 64] = np.sin(ang)
    al_c = 1.0 / np.sqrt(CL * 64.0)
    al_l = 1.0 / np.sqrt(S * 64.0)
    W64 = np.stack([np.concatenate([al_c * C64, -al_c * S64], 1), np.concatenate([-al_l * C64, al_l * S64], 1)], 0).astype(f)
    t = np.arange(S, dtype=np.int64)
    tabA = ((t[:, None] * np.arange(128)[None, :]) % S).astype(f)
    tabB = ((128 * t[:, None] * np.arange(S // 128)[None, :]) % S).astype(f)
    tc = np.arange(CL, dtype=np.int64)
    angc = 2 * np.pi * ((tc[:, None] * tc[None, :]) % CL) / float(CL)
    ctxCS = np.stack([np.cos(angc), np.sin(angc)], 0).astype(f)
    return {
        "modw": np.ascontiguousarray(mod_w[:, 0:2048]), "modbc": colmat(mod_b[0:2048], 16), "n1c": colmat(norm1, 8),
        "wsel": wsel, "mup": np.ascontiguousarray(shift_prev[sel][None].astype(f)),
        "mun": np.ascontiguousarray(shift_next[sel][None].astype(f)),
        "vecs": vecs, "w0a0": w0a0,
        "w2": np.ascontiguousarray(w2[:, :, c384].reshape(128, 384)), "a2": np.ascontiguousarray(a2[:, :, c384].reshape(128, 384)),
        "g2": np.ascontiguousarray(g2[:, c384]), "wo": wo, "mAT": mAT, "mN": mN, "mAK": mAK, "id64": np.eye(64, dtype=f),
        "rmask": rmask, "bones": bones, "onesD": np.ones((128, 128), f), "W64": W64,
        "tabA": tabA, "tabB": tabB, "ctxCS": ctxCS,
    }


def l0_core_inputs(xb, ctxb, c_b, c_ctx):
    return {"xT": np.ascontiguousarray(xb.T), "ctxT": np.ascontiguousarray(ctxb.T), "cT": c_cols(c_b, c_ctx)}


_L0N = ["mod_w", "mod_b", "norm1", "w_in", "shift_prev", "shift_next", "w0", "w2", "a0", "a2", "g2", "k_k", "k_a", "r_k",
        "lnx_g", "lnx_b", "w_out"]
_L1N = ["mod_w", "mod_b", "norm1", "w_in", "q_norm", "k_norm", "dw_w", "dw_b", "cn_g", "cn_b", "w_out"]
_PN = ["mod_w", "mod_b", "norm2", None, "pq", "sk1", "sk2", "pu", "pv"]
_PROGS = {}


def _prog(key, fn):
    if key not in _PROGS:
        _PROGS[key] = fn()
    return _PROGS[key]


def _run(P, in_maps):
    res = run_bass_kernel_spmd(P.nc, in_maps, core_ids=list(range(8)))
    return res.results


def kernel(**inp):
    f = np.float32
    g = lambda k: np.ascontiguousarray(np.asarray(inp[k], dtype=f))
    x = g("x"); ctx = g("ctx"); c = g("c"); c_ctx = g("c_ctx")
    B, S, _ = x.shape
    CL = ctx.shape[1]
    H = S // 2
    HC = CL // 2
    cores = [(b, s) for b in range(B) for s in range(2)]

    P0 = _prog("l0", lambda: build_l0(S, CL))
    cons0 = [l0_consts(*[g("l0_" + n) for n in _L0N], s, S, CL) for s in range(2)]
    ims = []
    for (b, s) in cores:
        im = dict(cons0[s])
        im.update(l0_core_inputs(x[b], ctx[b], c[b], c_ctx))
        ims.append(im)
    r0 = _run(P0, ims)
    mixT = [np.ascontiguousarray(r["paT"].T) for r in r0]
    del r0

    NT0 = (H + HC) // 128
    Pp0 = _prog("post0", lambda: build_post(NT0, H // 128, 2, False))
    consp = post_consts(g("l0_mod_w"), g("l0_mod_b"), g("l0_norm2"), g("norm_f"), g("l0_pq"), g("l0_sk1"), g("l0_sk2"),
                        g("l0_pu"), g("l0_pv"))
    ims = []
    for (b, s) in cores:
        rows = lambda m: np.concatenate([m[CL + H * s:CL + H * (s + 1)], m[HC * s:HC * (s + 1)]], 0)
        im = dict(consp)
        im["x"] = np.ascontiguousarray(np.concatenate([x[b, H * s:H * (s + 1)], ctx[b, HC * s:HC * (s + 1)]], 0))
        im["p0"] = np.ascontiguousarray(rows(mixT[2 * b]))
        im["p1"] = np.ascontiguousarray(rows(mixT[2 * b + 1]))
        im["cT"] = c_cols(c[b], c_ctx)
        ims.append(im)
    r1 = _run(Pp0, ims)
    x1 = np.zeros_like(x)
    ctx1 = np.zeros_like(ctx)
    for i, (b, s) in enumerate(cores):
        y = r1[i]["y"]
        x1[b, H * s:H * (s + 1)] = y[0:H]
        ctx1[b, HC * s:HC * (s + 1)] = y[H:H + HC]
    del r1, mixT

    P1 = _prog("l1", lambda: build_l1(S, CL, H))
    cons1 = [l1_consts(*[g("l1_" + n) for n in _L1N], s, S) for s in range(2)]
    ims = []
    for (b, s) in cores:
        im = dict(cons1[s])
        im.update(l1_core_inputs(x1[b], ctx1[b], c[b], c_ctx, s, H))
        ims.append(im)
    r2 = _run(P1, ims)
    paT = [np.ascontiguousarray(r["paT"].T) for r in r2]
    pcT = [np.ascontiguousarray(r["pcT"].T) for r in r2]
    del r2

    Pp1 = _prog("post1", lambda: build_post(H // 128, H // 128, 3, True))
    consp = post_consts(g("l1_mod_w"), g("l1_mod_b"), g("l1_norm2"), g("norm_f"), g("l1_pq"), g("l1_sk1"), g("l1_sk2"),
                        g("l1_pu"), g("l1_pv"))
    ims = []
    for (b, s) in cores:
        im = dict(consp)
        im["x"] = np.ascontiguousarray(x1[b, H * s:H * (s + 1)])
        im["p0"] = np.ascontiguousarray(paT[2 * b][H * s:H * (s + 1)])
        im["p1"] = np.ascontiguousarray(paT[2 * b + 1][H * s:H * (s + 1)])
        im["p2"] = pcT[2 * b + s]
        im["cT"] = c_cols(c[b], c_ctx)
        ims.append(im)
    r3 = _run(Pp1, ims)
    out = np.zeros_like(x)
    for i, (b, s) in enumerate(cores):
        out[b, H * s:H * (s + 1)] = r3[i]["y"]
    return out
```

```python
import numpy as np
import concourse.bass as bass
import concourse.mybir as mybir
from concourse.bass_utils import run_bass_kernel_spmd

F32 = mybir.dt.float32
BF16 = mybir.dt.bfloat16
I32 = mybir.dt.int32
U32 = mybir.dt.uint32
AF = mybir.ActivationFunctionType
ALU = mybir.AluOpType
AX = mybir.AxisListType


class T:
    def __init__(self, P, ap, name):
        self.P = P
        self.ap = ap
        self.name = name
        self.w = None
        self.r = {}
        self.dsem = None
        self.dcnt = 0
        self.psum = False

    def __getitem__(self, idx):
        return V(self, self.ap[idx])

    def sub(self, idx, tag):
        t = T(self.P, self.ap[idx], self.name + "_" + str(tag))
        t.psum = self.psum
        return t


class V:
    def __init__(self, t, ap):
        self.t = t
        self.ap = ap

    def __getitem__(self, idx):
        return V(self.t, self.ap[idx])


class Prog:
    ENG = ("pe", "dve", "act", "pool", "sp")

    def __init__(self, strict=True, num_devices=None):
        if num_devices is None:
            self.nc = bass.Bass("TRN2", target_bir_lowering=False)
        else:
            self.nc = bass.Bass("TRN2", target_bir_lowering=False, num_devices=num_devices)
        self.pfx = ""
        self.fused = num_devices is not None
        self._banks = None
        self._ps_i = 0
        self._sem_pool = []
        self._stage_tiles = None
        self._nsem = 0
        nc = self.nc
        self.e = {"pe": nc.tensor, "dve": nc.vector, "act": nc.scalar, "pool": nc.gpsimd, "sp": nc.sync}
        self.sem = {}
        self.cnt = {}
        self.seen = {k: {} for k in self.ENG}
        self.strict = strict
        self._ctx = []
        self._sctx = []
        for k in ("pe", "dve", "act", "pool"):
            self.sem[k] = self._senter(nc.semaphore("sem_" + k))
            self.cnt[k] = 0
        self.n_ins = 0
        self.out_tiles = []
        self._dcnt = {}

    def _enter(self, cm):
        v = cm.__enter__()
        self._ctx.append(cm)
        return v

    def _senter(self, cm):
        v = cm.__enter__()
        self._sctx.append(cm)
        return v

    def sb(self, name, shape, dt=F32):
        name = self.pfx + name
        h = self._enter(self.nc.sbuf_tensor(name, list(shape), dt))
        return T(self, h[:], name)

    def ps(self, name, shape, dt=F32):
        if self.fused:
            if self._banks is None:
                self._banks = []
                for i in range(8):
                    h = self._senter(self.nc.psum_tensor("bank%d" % i, [128, 512], F32))
                    t = T(self, h[:], "bank%d" % i)
                    t.psum = True
                    self._banks.append(t)
            t = self._banks[self._ps_i % 8]
            self._ps_i += 1
            return t
        h = self._enter(self.nc.psum_tensor(name, list(shape), dt))
        t = T(self, h[:], name)
        t.psum = True
        return t

    def dram(self, name, shape, dt=F32, kind="Internal", shared=False, persist=False):
        name = self.pfx + name
        if shared:
            h = self.nc.dram_tensor(name, list(shape), dt, kind=kind, addr_space="Shared")
        else:
            h = self.nc.dram_tensor(name, list(shape), dt, kind=kind)
        t = T(self, h.ap(), name)
        t.persist = persist or kind == "ExternalOutput"
        if kind == "ExternalOutput":
            self.out_tiles.append(t)
        return t

    def _dsem(self, t):
        if t.dsem is None:
            if self._sem_pool and not getattr(t, "persist", False):
                t.dsem, t.dcnt, t.dkey = self._sem_pool.pop()
            else:
                self._nsem += 1
                t.dkey = ("d", self._nsem)
                t.dsem = self._senter(self.nc.semaphore("ds%d" % self._nsem))
                t.dcnt = 0
                self.sem[t.dkey] = t.dsem
            if self._stage_tiles is not None and not getattr(t, "persist", False):
                self._stage_tiles.append(t)
        return t.dsem

    def stage_begin(self, pfx):
        self.pfx = pfx
        self._ps_i = 0
        self._stage_tiles = []
        self._stage_mark = self.mark()

    def stage_end(self, core_sync=True):
        self.barrier()
        self.release(self._stage_mark)
        for t in self._stage_tiles:
            self._sem_pool.append((t.dsem, t.dcnt, t.dkey))
            t.dsem = None
        self._stage_tiles = None
        if core_sync:
            self.nc.all_core_barrier()

    def _wait(self, eng, key, val, skip_self=False):
        if key == eng and (skip_self or not self.strict):
            return
        if self.seen[eng].get(key, 0) >= val:
            return
        self.seen[eng][key] = val
        self.e[eng].wait_ge(self.sem[key], val)

    def _deps(self, eng, reads, writes, acc=False):
        for v in reads:
            t = v.t
            if t.w is not None:
                self._wait(eng, *t.w)
            if t.psum:
                for k, c in t.r.items():
                    if k != eng:
                        self._wait(eng, k, c)
        for v in writes:
            t = v.t
            if t.w is not None:
                self._wait(eng, *t.w, skip_self=acc)
            for k, c in t.r.items():
                self._wait(eng, k, c)

    def _mark(self, key, val, reads, writes):
        for v in reads:
            t = v.t
            t.r[key] = max(t.r.get(key, 0), val)
        for v in writes:
            t = v.t
            t.w = (key, val)
            t.r = {}

    def op(self, eng, fn, reads, writes, acc=False):
        self._deps(eng, reads, writes, acc)
        ins = fn()
        self.cnt[eng] += 1
        ins.then_inc(self.sem[eng], 1)
        self._mark(eng, self.cnt[eng], reads, writes)
        self.n_ins += 1
        return ins

    def dma(self, q, out, in_, **kw):
        owner = out.t
        sem = self._dsem(owner)
        self._deps(q, [in_], [out])
        ins = self.e[q].dma_start(out=out.ap, in_=in_.ap, **kw)
        owner.dcnt += 16
        ins.then_inc(sem, 16)
        self._dcnt[owner.dkey] = owner.dcnt
        self._mark(owner.dkey, owner.dcnt, [in_], [out])
        self.n_ins += 1
        return ins

    def gather(self, out, table, idx):
        owner = out.t
        sem = self._dsem(owner)
        self._deps("pool", [table, idx], [out])
        ins = self.nc.gpsimd.indirect_dma_start(
            out=out.ap, out_offset=None, in_=table.ap,
            in_offset=bass.IndirectOffsetOnAxis(ap=idx.ap, axis=0))
        owner.dcnt += 16
        ins.then_inc(sem, 16)
        self._dcnt[owner.dkey] = owner.dcnt
        self._mark(owner.dkey, owner.dcnt, [table, idx], [out])
        self.n_ins += 1
        return ins

    def mm(self, out, lhsT, rhs, start=True, stop=True):
        nc = self.nc
        return self.op("pe", lambda: nc.tensor.matmul(out.ap, lhsT.ap, rhs.ap, start=start, stop=stop),
                       [lhsT, rhs], [out], acc=True)

    def tr(self, out, in_, ident):
        nc = self.nc
        return self.op("pe", lambda: nc.tensor.transpose(out.ap, in_.ap, ident.ap), [in_, ident], [out], acc=True)

    def act(self, out, in_, func, bias=None, scale=1.0, accum=None, eng="act"):
        nc = self.nc
        kw = {}
        rd = [in_]
        wr = [out]
        if bias is not None:
            if isinstance(bias, V):
                kw["bias"] = bias.ap
                rd.append(bias)
            else:
                kw["bias"] = bias
        if isinstance(scale, V):
            kw["scale"] = scale.ap
            rd.append(scale)
        else:
            kw["scale"] = scale
        if accum is not None:
            kw["accum_out"] = accum.ap
            wr.append(accum)
        return self.op("act", lambda: nc.scalar.activation(out=out.ap, in_=in_.ap, func=func, **kw), rd, wr)

    def tt(self, eng, out, a, b, op):
        e = self.e[eng]
        return self.op(eng, lambda: e.tensor_tensor(out=out.ap, in0=a.ap, in1=b.ap, op=op), [a, b], [out])

    def ts(self, eng, out, a, s1, op0, s2=None, op1=None):
        e = self.e[eng]
        rd = [a]
        a1 = s1.ap if isinstance(s1, V) else s1
        a2 = s2.ap if isinstance(s2, V) else s2
        if isinstance(s1, V):
            rd.append(s1)
        if isinstance(s2, V):
            rd.append(s2)
        if op1 is None:
            return self.op(eng, lambda: e.tensor_scalar(out=out.ap, in0=a.ap, scalar1=a1, scalar2=None, op0=op0), rd, [out])
        return self.op(eng, lambda: e.tensor_scalar(out=out.ap, in0=a.ap, scalar1=a1, scalar2=a2, op0=op0, op1=op1), rd, [out])

    def stt(self, eng, out, a, s, b, op0, op1, accum=None):
        e = self.e[eng]
        rd = [a, b]
        sa = s.ap if isinstance(s, V) else s
        if isinstance(s, V):
            rd.append(s)
        wr = [out]
        kw = {}
        if accum is not None:
            kw["accum_out"] = accum.ap
            wr.append(accum)
        return self.op(eng, lambda: e.scalar_tensor_tensor(out=out.ap, in0=a.ap, scalar=sa, in1=b.ap, op0=op0, op1=op1, **kw), rd, wr)

    def copy(self, eng, out, in_):
        if eng == "act":
            nc = self.nc
            return self.op("act", lambda: nc.scalar.copy(out=out.ap, in_=in_.ap), [in_], [out])
        e = self.e[eng]
        return self.op(eng, lambda: e.tensor_copy(out=out.ap, in_=in_.ap), [in_], [out])

    def memset(self, eng, out, val):
        e = self.e[eng]
        return self.op(eng, lambda: e.memset(out.ap, val), [], [out])

    def rsqrt(self, out, in_, scale, eps):
        nc = self.nc
        self.ts("dve", out, in_, scale, ALU.mult, eps, ALU.add)
        self.act(out, out, AF.Sqrt)
        self.op("dve", lambda: nc.vector.reciprocal(out=out.ap, in_=out.ap), [out], [out])

    def mark(self):
        return len(self._ctx)

    def barrier(self):
        for eng in self.ENG:
            for k in list(self.sem.keys()):
                if isinstance(k, tuple):
                    c = self._dcnt.get(k, 0)
                else:
                    c = self.cnt[k]
                if c > 0:
                    self._wait(eng, k, c)

    def release(self, mark):
        while len(self._ctx) > mark:
            cm = self._ctx.pop()
            cm.__exit__(None, None, None)

    def finish(self):
        for t in self.out_tiles:
            if t.w is not None:
                self._wait("sp", *t.w)
        for cm in reversed(self._ctx):
            cm.__exit__(None, None, None)
        for cm in reversed(self._sctx):
            cm.__exit__(None, None, None)
        return self.nc


D = 1024
NEXP = 16384


def build_post(NT, n_lat, n_part, final, P=None, hooks=None):
    standalone = P is None
    if standalone:
        P = Prog()
    nc = P.nc
    N = NT * 128
    hooks = hooks or {}
    if "x_src" not in hooks:
        x = P.dram("x", [N, D], F32, kind="ExternalInput")
        hooks["x_src"] = lambda it: x[it * 128:(it + 1) * 128, :]
    if "part_src" not in hooks:
        parts = [P.dram("p%d" % i, [N, D], F32, kind="ExternalInput") for i in range(n_part)]
        hooks["part_src"] = lambda i, it: parts[i][it * 128:(it + 1) * 128, :]
    if "cT" in hooks:
        cT = hooks["cT"]
    else:
        cT = P.dram("cT", [128, 8, 2], F32, kind="ExternalInput")
    modw = P.dram("modw", [D, 4096], F32, kind="ExternalInput")
    modb2 = P.dram("modb2", [2, 4096], F32, kind="ExternalInput")
    sel = P.dram("sel", [2, 2, 128], F32, kind="ExternalInput")
    n2 = P.dram("n2", [2, D], F32, kind="ExternalInput")
    nf = P.dram("nf", [2, D], F32, kind="ExternalInput")
    pq = P.dram("pq", [D, 2048], F32, kind="ExternalInput")
    skT = P.dram("skT", [128, 8, 2, 128], F32, kind="ExternalInput")
    pu = P.dram("pu", [NEXP, D], F32, kind="ExternalInput")
    pv = P.dram("pv", [NEXP, D], F32, kind="ExternalInput")
    identd = P.dram("ident", [128, 128], F32, kind="ExternalInput")
    zseld = P.dram("zsel", [128, 255], F32, kind="ExternalInput")
    iotad = P.dram("iota16", [128, 16], F32, kind="ExternalInput")
    if "y_dst" not in hooks:
        y = P.dram("y", [N, D], F32, kind="ExternalOutput")
        hooks["y_dst"] = lambda it: y[it * 128:(it + 1) * 128, :]
    yT_dst = hooks.get("yT_dst")

    ident = P.sb("ident_sb", [128, 128])
    identb = P.sb("identb", [128, 128], BF16)
    zsel = P.sb("zsel_sb", [128, 255])
    iota16 = P.sb("iota_sb", [128, 16])
    sk_sb = P.sb("sk_sb", [128, 8, 2, 128])
    sel_sb = P.sb("sel_sb", [2, 2, 128])
    modb_sb = P.sb("modb_sb", [2, 4096])
    n2_sb = P.sb("n2_sb", [2, D])
    nf_sb = P.sb("nf_sb", [2, D])
    c_sb = P.sb("c_sb", [128, 8, 2])
    sc_sb = P.sb("sc_sb", [128, 8, 2])
    rows = P.sb("rows", [2, 4096])
    P.dma("sp", ident[:], identd[:])
    P.dma("sp", zsel[:], zseld[:])
    P.dma("sp", iota16[:], iotad[:])
    P.dma("sp", sk_sb[:], skT[:])
    P.dma("sp", sel_sb[:], sel[:])
    P.dma("sp", modb_sb[:], modb2[:])
    P.dma("sp", n2_sb[:], n2[:])
    P.dma("sp", nf_sb[:], nf[:])
    P.dma("sp", c_sb[:], cT[:])
    P.copy("dve", identb[:], ident[:])
    P.act(sc_sb[:], c_sb[:], AF.Silu)

    psA = [P.ps("psA%d" % i, [128, 512]) for i in range(2)]
    psR = [P.ps("psR%d" % i, [128, 512]) for i in range(4)]
    psO = [P.ps("psO%d" % i, [128, 512]) for i in range(2)]

    wbuf = [P.sb("wbuf%d" % i, [128, 8, 256]) for i in range(2)]
    modw_v = modw.ap.rearrange("(k p) n -> p k n", p=128)
    pq_v = pq.ap.rearrange("(k p) n -> p k n", p=128)
    for cb in range(16):
        wb = wbuf[cb % 2]
        P.dma("sp", wb[:], V(modw, modw_v[:, :, cb * 256:(cb + 1) * 256]))
        pr = psA[cb % 2]
        for k in range(8):
            P.mm(V(pr, pr.ap[0:2, 0:256]), sc_sb[:, k, :], wb[:, k, :], start=(k == 0), stop=(k == 7))
        P.tt("dve", rows[:, cb * 256:(cb + 1) * 256], V(pr, pr.ap[0:2, 0:256]), modb_sb[:, cb * 256:(cb + 1) * 256], ALU.add)
    P.stt("dve", rows[:, 2048:3072], rows[:, 2048:3072], 1.0, n2_sb[:], ALU.add, ALU.mult)

    rep = [P.sb("rep%d" % i, [128, D]) for i in range(4)]
    nf_rep = P.sb("nf_rep", [128, D])

    def load_class(cls):
        for i in range(4):
            for hf in range(2):
                pr = psA[hf]
                P.mm(pr[:], sel_sb[:, cls, :], rows[:, i * 1024 + hf * 512: i * 1024 + hf * 512 + 512])
                P.copy("act", rep[i][:, hf * 512:(hf + 1) * 512], pr[:])

    if final:
        for hf in range(2):
            pr = psA[hf]
            P.mm(pr[:], sel_sb[:, 0, :], nf_sb[:, hf * 512:(hf + 1) * 512])
            P.copy("act", nf_rep[:, hf * 512:(hf + 1) * 512], pr[:])

    UVb = P.dram("UVb", [NEXP, 2 * D], BF16)
    mk_tab = P.mark()
    tst = [P.sb("tst%d" % i, [128, 4, D]) for i in range(2)]
    tsb = [P.sb("tsb%d" % i, [128, 4, D], BF16) for i in range(2)]
    uvv = UVb.ap.rearrange("(r p) (w d) -> p r w d", p=128, w=2)
    ci = 0
    for w_, tab in enumerate((pu, pv)):
        tv_ = tab.ap.rearrange("(r p) d -> p r d", p=128)
        for r4 in range(0, NEXP // 128, 4):
            a_, b_ = tst[ci % 2], tsb[ci % 2]
            P.dma("sp", a_[:], V(tab, tv_[:, r4:r4 + 4, :]))
            if ci % 2 == 0:
                P.copy("act", b_[:], a_[:])
            else:
                P.copy("dve", b_[:], a_[:])
            P.dma("sp", V(UVb, uvv[:, r4:r4 + 4, w_, :]), b_[:])
            ci += 1
    P.barrier()
    P.release(mk_tab)

    xt = P.sb("xt", [128, D])
    pt = [P.sb("pt%d" % i, [128, D]) for i in range(n_part)]
    x1 = P.sb("x1", [128, D])
    hn = P.sb("hn", [128, D])
    hnb = P.sb("hnb", [128, D], BF16)
    hnT = P.sb("hnT", [128, 8, 128])
    qT = P.sb("qT", [128, 16, 128])
    S4 = [P.sb("S4_%d" % g, [128, 4, 128]) for g in range(4)]
    scrA = P.sb("scrA", [128, 2048])
    scrB = P.sb("scrB", [128, 2048])
    tmpj = [scrA.sub((slice(None), slice(j * 128, (j + 1) * 128)), j) for j in range(16)]
    v16 = P.sb("v16", [128, 16, 16])
    i16 = P.sb("i16", [128, 16, 16], U32)
    v16j = [v16.sub((slice(None), j, slice(None)), j) for j in range(16)]
    i16j = [i16.sub((slice(None), j, slice(None)), j) for j in range(16)]
    candh = [scrB.sub((slice(None), slice(h * 256, (h + 1) * 256)), h) for h in range(8)]
    c16 = P.sb("c16", [128, 8, 16])
    ci16 = P.sb("ci16", [128, 8, 16], U32)
    c16h = [c16.sub((slice(None), h, slice(None)), h) for h in range(8)]
    ci16h = [ci16.sub((slice(None), h, slice(None)), h) for h in range(8)]
    hi_u = P.sb("hi_u", [128, 8, 16], U32)
    lo_u = P.sb("lo_u", [128, 8, 16], U32)
    hi_f = P.sb("hi_f", [128, 8, 16])
    lo_f = P.sb("lo_f", [128, 8, 16])
    i16f = P.sb("i16f", [128, 16, 16])
    e1 = P.sb("e1", [128, 8, 16])
    e2 = P.sb("e2", [128, 8, 16])
    ef = P.sb("ef", [128, 128])
    gate = P.sb("gate", [128, 8, 16])
    gmx = P.sb("gmx", [128, 8])
    eiT = P.sb("eiT", [128, 128], I32)
    gateT = P.sb("gateT", [128, 128])
    A = P.sb("A", [128, 128])
    Ag = P.sb("Ag", [128, 128])
    ss = P.sb("ss", [128, 1])
    rstd = P.sb("rstd", [128, 1])
    NB = 6
    UVg = [P.sb("UVg%d" % i, [128, 2 * D], BF16) for i in range(NB)]
    At = [P.sb("At%d" % i, [128, 128], BF16) for i in range(NB)]
    dsum = [P.sb("dsum%d" % i, [128, 2]) for i in range(NB)]
    gcol = [P.sb("gcol%d" % i, [128, 1]) for i in range(NB)]
    xo = P.sb("xo", [128, D])
    junk = xo

    cur_cls = None
    for it in range(NT):
        cls = 0 if it < n_lat else 1
        if cls != cur_cls:
            load_class(cls)
            cur_cls = cls
        g1r, sh2r, w2r, g2r = rep
        P.dma("sp", xt[:], hooks["x_src"](it))
        for i in range(n_part):
            P.dma("sp", pt[i][:], hooks["part_src"](i, it))
        for i in range(1, n_part):
            P.tt("pool", pt[0][:], pt[0][:], pt[i][:], ALU.add)
        P.tt("dve", x1[:], pt[0][:], g1r[:], ALU.mult)
        P.tt("dve", x1[:], x1[:], xt[:], ALU.add)
        P.act(junk[:], x1[:], AF.Square, accum=ss[:])
        P.rsqrt(rstd[:], ss[:], 1.0 / D, 1e-6)
        P.stt("dve", hn[:], x1[:], rstd[:, 0:1], w2r[:], ALU.mult, ALU.mult)
        P.tt("pool", hn[:], hn[:], sh2r[:], ALU.add)
        P.copy("act", hnb[:], hn[:])
        for g in range(2):
            pr = psA[g]
            for kk in range(4):
                k = g * 4 + kk
                P.tr(pr[:, kk * 128:(kk + 1) * 128], hn[:, k * 128:(k + 1) * 128], ident[:])
            P.copy("act" if g == 0 else "dve", V(hnT, hnT.ap[:, g * 4:(g + 1) * 4, :]),
                   V(pr, pr.ap.rearrange("p (a b) -> p a b", a=4)))
        for c8 in range(8):
            wb = wbuf[c8 % 2]
            P.dma("sp", wb[:], V(pq, pq_v[:, :, c8 * 256:(c8 + 1) * 256]))
            c4 = c8 // 2
            pr = psA[c4 % 2]
            for qq in range(2):
                pos = (c8 % 2) * 2 + qq
                for k in range(8):
                    P.mm(pr[:, pos * 128:(pos + 1) * 128], wb[:, k, qq * 128:(qq + 1) * 128], hnT[:, k, :],
                         start=(k == 0), stop=(k == 7))
            if c8 % 2 == 1:
                P.copy("act" if c4 % 2 == 0 else "dve", V(qT, qT.ap[:, c4 * 4:(c4 + 1) * 4, :]),
                       V(pr, pr.ap.rearrange("p (a b) -> p a b", a=4)))
        for g in range(4):
            pr = psA[g % 2]
            for jj in range(4):
                j = g * 4 + jj
                P.mm(pr[:, jj * 128:(jj + 1) * 128], qT[:, j, :], sk_sb[:, j // 2, j % 2, :])
            P.copy("act" if g % 2 == 0 else "dve", S4[g][:], V(pr, pr.ap.rearrange("p (a b) -> p a b", a=4)))
        Sj = [S4[j // 4][:, j % 4, :] for j in range(16)]
        for j in range(16):
            P.op("dve", (lambda j=j: nc.vector.max(out=v16j[j].ap[:, 0:8], in_=Sj[j].ap)), [Sj[j]], [v16j[j][:]])
        for j in range(16):
            P.op("dve", (lambda j=j: nc.vector.match_replace(out=tmpj[j].ap, in_to_replace=v16j[j].ap[:, 0:8],
                                                             in_values=Sj[j].ap, imm_value=-1e30)),
                 [Sj[j], v16j[j][:]], [tmpj[j][:]])
        for j in range(16):
            P.op("dve", (lambda j=j: nc.vector.max(out=v16j[j].ap[:, 8:16], in_=tmpj[j].ap)), [tmpj[j][:]], [v16j[j][:]])
        for j in range(16):
            P.op("dve", (lambda j=j: nc.vector.max_index(out=i16j[j].ap[:, 0:8], in_max=v16j[j].ap[:, 0:8],
                                                         in_values=Sj[j].ap)), [Sj[j], v16j[j][:]], [i16j[j][:]])
        for j in range(16):
            P.op("dve", (lambda j=j: nc.vector.max_index(out=i16j[j].ap[:, 8:16], in_max=v16j[j].ap[:, 8:16],
                                                         in_values=Sj[j].ap)), [Sj[j], v16j[j][:]], [i16j[j][:]])
        for h in range(8):
            a0 = v16j[2 * h].ap.unsqueeze(2).broadcast_to([128, 16, 16])
            a1 = v16j[2 * h + 1].ap.unsqueeze(1).broadcast_to([128, 16, 16])
            co = candh[h].ap.rearrange("p (a b) -> p a b", a=16)
            P.op("dve", (lambda co=co, a0=a0, a1=a1: nc.vector.tensor_tensor(out=co, in0=a0, in1=a1, op=ALU.add)),
                 [v16j[2 * h][:], v16j[2 * h + 1][:]], [candh[h][:]])
        tmp2 = [scrA.sub((slice(None), slice(h * 256, (h + 1) * 256)), "c%d" % h) for h in range(8)]
        t2dep = lambda h: [tmpj[2 * h][:], tmpj[2 * h + 1][:]]
        for h in range(8):
            P.op("dve", (lambda h=h: nc.vector.max(out=c16h[h].ap[:, 0:8], in_=candh[h].ap)), [candh[h][:]], [c16h[h][:]])
        for h in range(8):
            P.op("dve", (lambda h=h: nc.vector.match_replace(out=tmp2[h].ap, in_to_replace=c16h[h].ap[:, 0:8],
                                                             in_values=candh[h].ap, imm_value=-1e30)),
                 [candh[h][:], c16h[h][:]], t2dep(h))
        for h in range(8):
            P.op("dve", (lambda h=h: nc.vector.max(out=c16h[h].ap[:, 8:16], in_=tmp2[h].ap)), t2dep(h), [c16h[h][:]])
        for h in range(8):
            P.op("dve", (lambda h=h: nc.vector.max_index(out=ci16h[h].ap[:, 0:8], in_max=c16h[h].ap[:, 0:8],
                                                         in_values=candh[h].ap)), [candh[h][:], c16h[h][:]], [ci16h[h][:]])
        for h in range(8):
            P.op("dve", (lambda h=h: nc.vector.max_index(out=ci16h[h].ap[:, 8:16], in_max=c16h[h].ap[:, 8:16],
                                                         in_values=candh[h].ap)), [candh[h][:], c16h[h][:]], [ci16h[h][:]])
        allci = [t[:] for t in ci16h]
        allc = [t[:] for t in c16h]
        alli = [t[:] for t in i16j]
        P.op("dve", lambda: nc.vector.tensor_single_scalar(out=hi_u.ap, in_=ci16.ap, scalar=4, op=ALU.logical_shift_right),
             allci, [hi_u[:]])
        P.op("dve", lambda: nc.vector.tensor_single_scalar(out=lo_u.ap, in_=ci16.ap, scalar=15, op=ALU.bitwise_and),
             allci, [lo_u[:]])
        P.copy("dve", hi_f[:], hi_u[:])
        P.copy("dve", lo_f[:], lo_u[:])
        P.op("dve", lambda: nc.vector.tensor_copy(out=i16f.ap, in_=i16.ap), alli, [i16f[:]])
        i16f_v = i16f.ap.rearrange("p (h two) k -> p h two k", two=2)
        oh = scrA.ap.rearrange("p (h k i) -> p h k i", h=8, k=16)
        pr4 = scrB.ap.rearrange("p (h k i) -> p h k i", h=8, k=16)
        scrA_all = [t[:] for t in tmpj]
        scrB_all = [t[:] for t in candh]
        iota_b = iota16.ap.unsqueeze(1).unsqueeze(1).broadcast_to([128, 8, 16, 16])
        for (src_f, half, dst) in ((hi_f, 0, e1), (lo_f, 1, e2)):
            sb_ = src_f.ap.unsqueeze(3).broadcast_to([128, 8, 16, 16])
            P.op("dve", (lambda sb_=sb_: nc.vector.tensor_tensor(out=oh, in0=sb_, in1=iota_b, op=ALU.is_equal)),
                 [src_f[:], iota16[:]], scrA_all)
            ib = i16f_v[:, :, half, :].unsqueeze(2).broadcast_to([128, 8, 16, 16])
            P.op("dve", (lambda ib=ib: nc.vector.tensor_tensor(out=pr4, in0=oh, in1=ib, op=ALU.mult)),
                 scrA_all + [i16f[:]], scrB_all)
            P.op("dve", (lambda dst=dst: nc.vector.tensor_reduce(out=dst.ap, in_=pr4, axis=AX.X, op=ALU.add)),
                 scrB_all, [dst[:]])
        ef3 = ef.ap.rearrange("p (h k) -> p h k", h=8)
        P.op("dve", lambda: nc.vector.scalar_tensor_tensor(out=ef3, in0=e1.ap, scalar=128.0, in1=e2.ap,
                                                           op0=ALU.mult, op1=ALU.add), [e1[:], e2[:]], [ef[:]])
        P.op("dve", lambda: nc.vector.tensor_copy(out=gmx.ap, in_=c16.ap[:, :, 0]), allc, [gmx[:]])
        P.op("dve", lambda: nc.vector.tensor_tensor(out=gate.ap, in0=c16.ap, in1=gmx.ap.unsqueeze(2).broadcast_to([128, 8, 16]),
                                                    op=ALU.subtract), allc + [gmx[:]], [gate[:]])
        P.act(gate[:], gate[:], AF.Exp)
        P.op("dve", lambda: nc.vector.tensor_reduce(out=gmx.ap, in_=gate.ap, axis=AX.X, op=ALU.add), [gate[:]], [gmx[:]])
        P.op("dve", lambda: nc.vector.reciprocal(out=gmx.ap, in_=gmx.ap), [gmx[:]], [gmx[:]])
        P.op("dve", lambda: nc.vector.tensor_tensor(out=gate.ap, in0=gate.ap, in1=gmx.ap.unsqueeze(2).broadcast_to([128, 8, 16]),
                                                    op=ALU.mult), [gate[:], gmx[:]], [gate[:]])
        pr = psA[0]
        P.tr(pr[:, 0:128], ef[:], ident[:])
        P.op("dve", lambda: nc.vector.tensor_copy(out=eiT.ap, in_=pr.ap[:, 0:128]), [pr[:]], [eiT[:]])
        pr2 = psA[1]
        P.tr(pr2[:, 0:128], V(gate, gate.ap.rearrange("p h k -> p (h k)")), ident[:])
        P.copy("act", gateT[:], pr2[:, 0:128])
        def s_gather(t):
            b = t % NB
            P.gather(UVg[b][:], UVb[:], eiT[:, t:t + 1])
            rb = (t % 2) * 2
            lt = V(identb, identb.ap[:, t:t + 1].broadcast_to([128, 128]))
            for hf in range(2):
                P.mm(psR[rb + hf][:], lt, hnb[:, hf * 512:(hf + 1) * 512])

        def s_dot(t):
            b = t % NB
            rb = (t % 2) * 2
            for hf in range(2):
                P.stt("dve", junk[:, hf * 512:(hf + 1) * 512], UVg[b][:, hf * 512:(hf + 1) * 512], 1.0, psR[rb + hf][:],
                      ALU.mult, ALU.mult, accum=dsum[b][:, hf:hf + 1])
            P.act(gcol[b][:], dsum[b][:, 0:1], AF.Gelu, bias=dsum[b][:, 1:2])

        def s_mask(t):
            b = t % NB
            P.ts("dve", At[b][:], zsel[:, 127 - t:255 - t], gcol[b][:, 0:1], ALU.mult, gateT[:, t:t + 1], ALU.mult)

        def s_out(t):
            b = t % NB
            for hf in range(2):
                P.mm(psO[hf][:], At[b][:], UVg[b][:, D + hf * 512:D + (hf + 1) * 512], start=(t == 0), stop=(t == 127))
        for i in range(128 + 3):
            if i < 128:
                s_gather(i)
            if 0 <= i - 1 < 128:
                s_dot(i - 1)
            if 0 <= i - 2 < 128:
                s_mask(i - 2)
            if 0 <= i - 3 < 128:
                s_out(i - 3)
        for hf in range(2):
            sl = slice(hf * 512, (hf + 1) * 512)
            P.tt("dve", xo[:, sl], psO[hf][:], g2r[:, sl], ALU.mult)
        P.tt("pool", xo[:], xo[:], x1[:], ALU.add)
        if final:
            P.act(hn[:], xo[:], AF.Square, accum=ss[:])
            P.rsqrt(rstd[:], ss[:], 1.0 / D, 1e-6)
            P.stt("dve", xo[:], xo[:], rstd[:, 0:1], nf_rep[:], ALU.mult, ALU.mult)
        P.dma("sp", hooks["y_dst"](it), xo[:])
        if yT_dst is not None:
            for g in range(2):
                pr = psA[g]
                for kk in range(4):
                    k = g * 4 + kk
                    P.tr(pr[:, kk * 128:(kk + 1) * 128], xo[:, k * 128:(k + 1) * 128], ident[:])
                P.copy("act" if g == 0 else "dve", V(hnT, hnT.ap[:, g * 4:(g + 1) * 4, :]),
                       V(pr, pr.ap.rearrange("p (a b) -> p a b", a=4)))
            P.dma("sp", yT_dst(it), hnT[:])
    if standalone:
        P.finish()
    return P


def post_consts(mod_w, mod_b, norm2, norm_f, pq, sk1, sk2, pu, pv):
    f = np.float32
    sel = np.zeros((2, 2, 128), f)
    sel[0, 0, :] = 1
    sel[1, 1, :] = 1
    zsel = np.zeros((128, 255), f)
    zsel[:, 127] = 1
    skT = np.ascontiguousarray(np.stack([sk1, sk2], 0).transpose(3, 1, 0, 2))
    return {
        "modw": np.ascontiguousarray(mod_w[:, 2048:6144]),
        "modb2": np.ascontiguousarray(np.tile(mod_b[None, 2048:6144], (2, 1))),
        "sel": sel,
        "n2": np.ascontiguousarray(np.tile(norm2[None], (2, 1))),
        "nf": np.ascontiguousarray(np.tile(norm_f[None], (2, 1))),
        "pq": pq, "skT": skT, "pu": pu, "pv": pv,
        "ident": np.eye(128, dtype=f), "zsel": zsel,
        "iota16": np.ascontiguousarray(np.tile(np.arange(16, dtype=f)[None], (128, 1))),
    }


def c_cols(c_b, c_ctx):
    cc = np.stack([c_b, c_ctx], -1).astype(np.float32)
    return np.ascontiguousarray(cc.reshape(8, 128, 2).transpose(1, 0, 2))


def build_l1(S=8192, CL=256, CONV_T=4096, upto=9, P=None, hooks=None):
    standalone = P is None
    if standalone:
        P = Prog()
    hooks = hooks or {}
    nc = P.nc
    NQB = S // 512
    NKT = (S + CL) // 128
    NCB = CONV_T // 256
    XC = CONV_T + 30
    di = lambda n, s, dt=F32: P.dram(n, s, dt, kind="ExternalInput")
    if "h_src" not in hooks:
        xT = di("xT", [D, S]); ctxT = di("ctxT", [D, CL]); xcT = di("xcT", [D, XC])
        srcs = {"lat": xT, "ctx": ctxT, "conv": xcT}
        hooks["h_src"] = lambda kind, c0, w: V(srcs[kind], srcs[kind].ap.rearrange("(k p) t -> p k t", p=128)[:, :, c0:c0 + w])
    edge = di("edge", [128, 2])
    cT = hooks["cT"] if "cT" in hooks else di("cT", [128, 8, 2])
    modw = di("modw", [D, 2048])
    modbc = di("modbc", [128, 16]); n1c = di("n1c", [128, 8])
    wq = di("wq", [D, 384]); wk = di("wk", [D, 128]); wv = di("wv", [D, 128]); wu = di("wu", [D, 512])
    gq = di("gq", [128, 1]); gk = di("gk", [128, 1])
    ropeC = di("ropeC", [128, S]); ropeS = di("ropeS", [128, S]); Rm = di("Rm", [128, 128])
    bones = di("bones", [128, 128]); onesD = di("onesD", [128, 128]); ones256 = di("ones256", [128, 128])
    dww = di("dww", [128, 2, 31]); dwb = di("dwb", [128, 2]); cng = di("cng", [128, 2]); cnb = di("cnb", [128, 2])
    woa = di("woa", [64, 6, D]); woc = di("woc", [128, 2, D]); shiftm = di("shiftm", [128, 64])
    if "pa_dst" not in hooks:
        pa = P.dram("pa", [S, D], F32, kind="ExternalOutput")
        pc = P.dram("pc", [CONV_T, D], F32, kind="ExternalOutput")
        hooks["pa_dst"] = lambda tile: pa[tile * 128:(tile + 1) * 128, :]
        hooks["pc_dst"] = lambda tile: pc[tile * 128:(tile + 1) * 128, :]

    ps = [P.ps("ps%d" % i, [128, 512]) for i in range(8)]
    def ld(name, src, shape, dt=F32):
        t = P.sb(name, shape, dt)
        P.dma("sp", t[:], src[:])
        return t
    edge_s = ld("edge_s", edge, [128, 2]); c_sb = ld("c_sb", cT, [128, 8, 2]); modb_s = ld("modb_s", modbc, [128, 16])
    n1_s = ld("n1_s", n1c, [128, 8]); gq_s = ld("gq_s", gq, [128, 1]); gk_s = ld("gk_s", gk, [128, 1])
    Rm_s = ld("Rm_s", Rm, [128, 128]); bones_s = ld("bones_s", bones, [128, 128]); onesD_s = ld("onesD_s", onesD, [128, 128])
    ones256_s = ld("ones256_s", ones256, [128, 128]); dww_s = ld("dww_s", dww, [128, 2, 31]); dwb_s = ld("dwb_s", dwb, [128, 2])
    cng_s = ld("cng_s", cng, [128, 2]); cnb_s = ld("cnb_s", cnb, [128, 2]); shift_s = ld("shift_s", shiftm, [128, 64])
    sc_sb = P.sb("sc_sb", [128, 8, 2])
    P.act(sc_sb[:], c_sb[:], AF.Silu)
    xblk = P.sb("xblk", [128, 8, 512])
    stage = xblk
    def ldw(name, src, ncol):
        t = P.sb(name, [128, 8, ncol], BF16)
        P.dma("sp", stage[:, :, 0:ncol], V(src, src.ap.rearrange("(k p) n -> p k n", p=128)))
        P.copy("dve", t[:], stage[:, :, 0:ncol])
        return t
    wq_s = ldw("wq_s", wq, 384); wk_s = ldw("wk_s", wk, 128); wv_s = ldw("wv_s", wv, 128); wu_s = ldw("wu_s", wu, 512)
    woa_s = P.sb("woa_s", [64, 6, D], BF16)
    woc_s = P.sb("woc_s", [128, 2, D], BF16)
    for hh in range(3):
        st_v = V(stage, stage.ap[0:64].rearrange("p k n -> p (k n)")[:, 0:2048].rearrange("p (a n) -> p a n", a=2))
        P.dma("sp", st_v, woa[:, hh * 2:(hh + 1) * 2, :])
        P.copy("dve", woa_s[:, hh * 2:(hh + 1) * 2, :], st_v)
    st_v = V(stage, stage.ap.rearrange("p k n -> p (k n)")[:, 0:2048].rearrange("p (a n) -> p a n", a=2))
    P.dma("sp", st_v, woc[:])
    P.copy("dve", woc_s[:], st_v)
    modc = P.sb("modc", [128, 16, 2])
    modw_v = modw.ap.rearrange("(k p) n -> p k n", p=128)
    for c4 in range(4):
        wb = xblk
        P.dma("sp", wb[:], V(modw, modw_v[:, :, c4 * 512:(c4 + 1) * 512]))
        for q4 in range(4):
            cc = c4 * 4 + q4
            pr = ps[cc % 2]
            for k in range(8):
                P.mm(pr[:, 0:2], wb[:, k, q4 * 128:(q4 + 1) * 128], sc_sb[:, k, :], start=(k == 0), stop=(k == 7))
            P.ts("dve", modc[:, cc, :], pr[:, 0:2], modb_s[:, cc:cc + 1], ALU.add)
    wmod = P.sb("wmod", [128, 8, 2])
    P.op("dve", lambda: nc.vector.scalar_tensor_tensor(out=wmod.ap, in0=modc.ap[:, 8:16, :], scalar=1.0,
                                                       in1=n1_s.ap.unsqueeze(2).broadcast_to([128, 8, 2]),
                                                       op0=ALU.add, op1=ALU.mult), [modc[:], n1_s[:]], [wmod[:]])
    if upto < 1:
        P.finish()
        return P
    qT_r = P.sb("qT_r", [128, 3, S], BF16)
    kT_r = P.sb("kT_r", [128, S + CL], BF16)
    Va = [P.sb("Va%d" % i, [128, NKT, 128], BF16) for i in range(2)]
    for i in range(2):
        P.memset("pool", Va[i][:, :, 64:128], 1.0)
    hT = P.sb("hT", [128, 8, 512], BF16)
    sqb = [P.sb("sqb%d" % i, [128, 512]) for i in range(2)]
    rstd = P.sb("rstd", [128, 512])
    tmpf = [P.sb("tmpf%d" % i, [128, 512]) for i in range(2)]
    rC = P.sb("rC", [128, 512]); rS = P.sb("rS", [128, 512])
    raw = P.sb("raw", [128, 512]); qn = P.sb("qn", [128, 512]); r2 = P.sb("r2", [128, 512])

    def hblock(src, c0, w, cls):
        pieces = hooks["h_src"](src, c0, w)
        if not isinstance(pieces, list):
            pieces = [(0, w, pieces)]
        for (d0, dw_, sv) in pieces:
            P.dma("sp", xblk[:, :, d0:d0 + dw_], sv)
        pr = ps[0]
        for k in range(8):
            sq = sqb[k % 2]
            P.act(sq[:, 0:w], xblk[:, k, 0:w], AF.Square)
            P.mm(pr[:, 0:w], onesD_s[:], sq[:, 0:w], start=(k == 0), stop=(k == 7))
        P.rsqrt(rstd[:, 0:w], pr[:, 0:w], 1.0 / D, 1e-6)
        for k in range(8):
            tf = tmpf[k % 2]
            P.tt("dve", tf[:, 0:w], xblk[:, k, 0:w], rstd[:, 0:w], ALU.mult)
            P.ts("pool", hT[:, k, 0:w], tf[:, 0:w], wmod[:, k, cls:cls + 1], ALU.mult, modc[:, k, cls:cls + 1], ALU.add)

    def headnorm(pr, w, g_s, dest, rope_c0):
        P.copy("act", raw[:, 0:w], pr[:, 0:w])
        P.act(r2[:, 0:w], raw[:, 0:w], AF.Square)
        p2 = ps[2]
        P.mm(p2[:, 0:w], bones_s[:], r2[:, 0:w])
        P.rsqrt(r2[:, 0:w], p2[:, 0:w], 1.0 / 64, 1e-6)
        if rope_c0 is None:
            P.stt("dve", dest, raw[:, 0:w], g_s[:, 0:1], r2[:, 0:w], ALU.mult, ALU.mult)
            return
        P.stt("dve", qn[:, 0:w], raw[:, 0:w], g_s[:, 0:1], r2[:, 0:w], ALU.mult, ALU.mult)
        p3 = ps[3]
        P.mm(p3[:, 0:w], Rm_s[:], qn[:, 0:w])
        P.tt("dve", r2[:, 0:w], p3[:, 0:w], rS[:, 0:w], ALU.mult)
        P.tt("pool", qn[:, 0:w], qn[:, 0:w], rC[:, 0:w], ALU.mult)
        P.tt("pool", dest, qn[:, 0:w], r2[:, 0:w], ALU.add)

    def kv_proj(w, tok0, rope_c0):
        pr = ps[1]
        for k in range(8):
            P.mm(pr[:, 0:w], wk_s[:, k, :], hT[:, k, 0:w], start=(k == 0), stop=(k == 7))
        headnorm(pr, w, gk_s, kT_r[:, tok0:tok0 + w], rope_c0)
        for tt_ in range(w // 128):
            pv_ = ps[4 + tt_ % 2]
            for k in range(8):
                P.mm(pv_[:, 0:128], hT[:, k, tt_ * 128:(tt_ + 1) * 128], wv_s[:, k, :], start=(k == 0), stop=(k == 7))
            kt = tok0 // 128 + tt_
            P.copy("act", Va[0][:, kt, 0:64], pv_[:, 0:64])
            P.copy("dve", Va[1][:, kt, 0:64], pv_[:, 64:128])

    import os
    dbg = int(os.environ.get("L1DBG", "9"))
    for qb in range(NQB):
        c0 = qb * 512
        hblock("lat", c0, 512, 0)
        if dbg < 1:
            continue
        P.dma("sp", rC[:], ropeC[:, c0:c0 + 512])
        P.dma("sp", rS[:], ropeS[:, c0:c0 + 512])
        for ti in range(3):
            pr = ps[1]
            for k in range(8):
                P.mm(pr[:], wq_s[:, k, ti * 128:(ti + 1) * 128], hT[:, k, :], start=(k == 0), stop=(k == 7))
            if dbg >= 2:
                headnorm(pr, 512, gq_s, qT_r[:, ti, c0:c0 + 512], c0)
        if dbg >= 3:
            kv_proj(512, c0, c0)
    if dbg >= 4:
        hblock("ctx", 0, CL, 1)
        kv_proj(CL, S, None)

    if upto < 2:
        P.finish()
        return P
    gl = P.sb("gl", [128, 2, 286]); sg = P.sb("sg", [128, 286])
    acc = P.sb("acc", [128, 2, 256]); dd = P.sb("dd", [128, 2, 256]); sqd = P.sb("sqd", [128, 2, 256])
    cact = P.sb("cact", [128, 2, 256], BF16)
    ostg = [P.sb("ostg%d" % i, [128, D]) for i in range(2)]
    accs = [acc.sub((slice(None), c, slice(None)), c) for c in range(2)]
    gls = [gl.sub((slice(None), c, slice(None)), c) for c in range(2)]
    for j in range(NCB):
        hblock("conv", 256 * j, 286, 0)
        for c in range(2):
            pv_ = ps[1]
            pg_ = ps[2]
            for k in range(8):
                P.mm(pv_[:, 0:286], wu_s[:, k, c * 128:(c + 1) * 128], hT[:, k, 0:286], start=(k == 0), stop=(k == 7))
            for k in range(8):
                P.mm(pg_[:, 0:286], wu_s[:, k, 256 + c * 128:256 + (c + 1) * 128], hT[:, k, 0:286], start=(k == 0), stop=(k == 7))
            P.act(sg[:], pg_[:, 0:286], AF.Sigmoid)
            P.tt("dve", gls[c][:], pv_[:, 0:286], sg[:], ALU.mult)
            if j == 0:
                P.ts("dve", gls[c][:, 0:15], gls[c][:, 0:15], edge_s[:, 0:1], ALU.mult)
            if j == NCB - 1:
                P.ts("dve", gls[c][:, 271:286], gls[c][:, 271:286], edge_s[:, 1:2], ALU.mult)
        for c in range(2):
            P.ts("dve", accs[c][:], gls[c][:, 0:256], dww_s[:, c, 0:1], ALU.mult, dwb_s[:, c:c + 1], ALU.add)
        for jj in range(1, 31):
            for c in range(2):
                P.stt("dve", accs[c][:], gls[c][:, jj:jj + 256], dww_s[:, c, jj:jj + 1], accs[c][:], ALU.mult, ALU.add)
        pm = ps[3]
        for c in range(2):
            P.mm(pm[:, 0:256], ones256_s[:], accs[c][:], start=(c == 0), stop=(c == 1))
        for c in range(2):
            P.tt("dve", dd[:, c, :], accs[c][:], pm[:, 0:256], ALU.subtract)
        P.act(sqd[:], dd[:], AF.Square)
        pvv = ps[4]
        for c in range(2):
            P.mm(pvv[:, 0:256], ones256_s[:], sqd[:, c, :], start=(c == 0), stop=(c == 1))
        P.rsqrt(rstd[:, 0:256], pvv[:, 0:256], 1.0, 1e-5)
        for c in range(2):
            P.stt("dve", dd[:, c, :], dd[:, c, :], cng_s[:, c:c + 1], rstd[:, 0:256], ALU.mult, ALU.mult)
            P.act(cact[:, c, :], dd[:, c, :], AF.Silu, bias=cnb_s[:, c:c + 1])
        for tt_ in range(2):
            og = ostg[tt_ % 2]
            for ch in range(2):
                pw = ps[5 + ch]
                for c in range(2):
                    P.mm(pw[:], cact[:, c, tt_ * 128:(tt_ + 1) * 128], woc_s[:, c, ch * 512:(ch + 1) * 512], start=(c == 0), stop=(c == 1))
                P.copy("act" if ch == 0 else "dve", og[:, ch * 512:(ch + 1) * 512], pw[:])
            P.dma("sp", hooks["pc_dst"](2 * j + tt_), og[:])

    if upto < 3:
        P.finish()
        return P
    Pt = [P.sb("Pt%d" % i, [128, 512], BF16) for i in range(3)]
    Osb = raw
    rden = r2
    oT = [P.sb("oT%d" % j, [64, 512], BF16) for j in range(6)]
    for qb in range(NQB):
        c0 = qb * 512
        for j in range(6):
            half = j // 3
            ti = j % 3
            pl = slice(half * 64, half * 64 + 64)
            pO = ps[2]
            for kt in range(NKT):
                pS = ps[kt % 2]
                P.mm(pS[:], kT_r[pl, kt * 128:(kt + 1) * 128], qT_r[pl, ti, c0:c0 + 512])
                pt_ = Pt[kt % 3]
                P.act(pt_[:], pS[:], AF.Exp, scale=0.125)
                P.mm(pO[:], Va[half][:, kt, :], pt_[:], start=(kt == 0), stop=(kt == NKT - 1))
            P.copy("dve", Osb[:], pO[:])
            pD = ps[3]
            P.mm(pD[0:64, :], shift_s[:], Osb[:])
            P.op("dve", lambda pD=pD: nc.vector.reciprocal(out=rden.ap[0:64, :], in_=pD.ap[0:64, :]), [pD[:]], [rden[:]])
            P.tt("dve", oT[j][:], Osb[0:64, :], rden[0:64, :], ALU.mult)
        for tt_ in range(4):
            og = ostg[tt_ % 2]
            for ch in range(2):
                pw = ps[5 + ch]
                for j in range(6):
                    P.mm(pw[:], oT[j][:, tt_ * 128:(tt_ + 1) * 128], woa_s[:, j, ch * 512:(ch + 1) * 512], start=(j == 0), stop=(j == 5))
                P.copy("act" if ch == 0 else "dve", og[:, ch * 512:(ch + 1) * 512], pw[:])
            P.dma("sp", hooks["pa_dst"](qb * 4 + tt_), og[:])
    if standalone:
        P.finish()
    return P


def rope_tables(S):
    f = np.float32
    t = np.arange(S)
    row = (t // 64).astype(f)
    col = (t % 64).astype(f)
    inv = (f(10000.0) ** (-np.arange(0, 32, 2, dtype=f) / f(32))).astype(f)
    ang = np.stack([row[:, None] * inv, col[:, None] * inv], 1)
    cs, sn = np.cos(ang).astype(f), np.sin(ang).astype(f)
    C = np.zeros((64, S), f)
    Sn = np.zeros((64, S), f)
    for ax in range(2):
        for hf in range(2):
            C[ax * 32 + hf * 16: ax * 32 + hf * 16 + 16] = cs[:, ax, :].T
            Sn[ax * 32 + hf * 16: ax * 32 + hf * 16 + 16] = sn[:, ax, :].T
    Rm = np.zeros((128, 128), f)
    for m in range(128):
        if (m % 32) < 16:
            Rm[m + 16, m] = -1.0
        else:
            Rm[m - 16, m] = 1.0
    return np.ascontiguousarray(np.tile(C, (2, 1))), np.ascontiguousarray(np.tile(Sn, (2, 1))), Rm


def colmat(v, nk):
    return np.ascontiguousarray(np.asarray(v, np.float32).reshape(nk, 128).T)


def l1_consts(mod_w, mod_b, norm1, w_in, q_norm, k_norm, dw_w, dw_b, cn_g, cn_b, w_out, s, S):
    f = np.float32
    C, Sn, Rm = rope_tables(S)
    bones = np.zeros((128, 128), f)
    bones[0:64, 0:64] = 1
    bones[64:128, 64:128] = 1
    shiftm = np.zeros((128, 64), f)
    for m in range(64):
        shiftm[m + 64, m] = 1
    qcols = []
    for i in range(3):
        for hh in (6 * s + i, 6 * s + 3 + i):
            qcols.append(w_in[:, hh * 64:(hh + 1) * 64])
    wq = np.ascontiguousarray(np.concatenate(qcols, 1))
    wk = np.ascontiguousarray(w_in[:, 768 + 128 * s: 768 + 128 * (s + 1)])
    wv = np.ascontiguousarray(w_in[:, 1024 + 128 * s: 1024 + 128 * (s + 1)])
    wu = np.ascontiguousarray(w_in[:, 1280:1792])
    woa = np.ascontiguousarray(w_out[384 * s:384 * (s + 1)].reshape(6, 64, D).transpose(1, 0, 2))
    woc = np.ascontiguousarray(w_out[768:1024].reshape(2, 128, D).transpose(1, 0, 2))
    dww = np.ascontiguousarray(dw_w.T.reshape(2, 128, 31).transpose(1, 0, 2))
    return {
        "modw": np.ascontiguousarray(mod_w[:, 0:2048]), "modbc": colmat(mod_b[0:2048], 16), "n1c": colmat(norm1, 8),
        "wq": wq, "wk": wk, "wv": wv, "wu": wu,
        "gq": np.ascontiguousarray(np.tile(q_norm, 2)[:, None].astype(f)),
        "gk": np.ascontiguousarray(np.tile(k_norm, 2)[:, None].astype(f)),
        "ropeC": C, "ropeS": Sn, "Rm": Rm, "bones": bones, "onesD": np.ones((128, 128), f),
        "ones256": np.full((128, 128), 1.0 / 256, f),
        "dww": dww, "dwb": colmat(dw_b, 2), "cng": colmat(cn_g, 2), "cnb": colmat(cn_b, 2),
        "woa": woa, "woc": woc, "shiftm": shiftm,
    }


def l1_core_inputs(xb, ctxb, c_b, c_ctx, s, conv_t):
    f = np.float32
    S = xb.shape[0]
    lo = conv_t * s - 15
    hi = conv_t * s + conv_t + 15
    xc = np.zeros((conv_t + 30, D), f)
    a, b_ = max(lo, 0), min(hi, S)
    xc[a - lo:b_ - lo] = xb[a:b_]
    edge = np.zeros((128, 2), f)
    edge[:, 0] = 1.0 if lo >= 0 else 0.0
    edge[:, 1] = 1.0 if hi <= S else 0.0
    return {"xT": np.ascontiguousarray(xb.T), "ctxT": np.ascontiguousarray(ctxb.T), "xcT": np.ascontiguousarray(xc.T),
            "edge": edge, "cT": c_cols(c_b, c_ctx)}


CH = 64
NEG_EXP_HALF = -0.6065306597126334


def build_l0(S=8192, CL=256, upto=9, dbg=False, P=None, hooks=None):
    standalone = P is None
    if standalone:
        P = Prog()
    hooks = hooks or {}
    nc = P.nc
    TT = CL + S
    NCH = TT // CH
    segs = [(0, CL, 1), (CL, S, 0)]
    di = lambda n, s, dt=F32: P.dram(n, s, dt, kind="ExternalInput")
    xT = di("xT", [D, S]); ctxT = di("ctxT", [D, CL])
    cT = hooks["cT"] if "cT" in hooks else di("cT", [128, 8, 2])
    modw = di("modw", [D, 2048]); modbc = di("modbc", [128, 16]); n1c = di("n1c", [128, 8])
    wsel = di("wsel", [D, 1664]); mup = di("mup", [1, 1536]); mun = di("mun", [1, 1536])
    vecs = di("vecs", [128, 3, 5])
    w0a0 = di("w0a0", [128, 2, 2, 3])
    w2d = di("w2", [128, 384]); a2d = di("a2", [128, 384]); g2d = di("g2", [128, 384])
    wod = di("wo", [128, 4, D])
    mAT = di("mAT", [2, 128, 128]); mN = di("mN", [2, 64, 64]); id64 = di("id64", [64, 64]); mAK = di("mAK", [2, 128, 64])
    rmaskd = di("rmask", [128, 512]); bonesd = di("bones", [128, 128]); onesDd = di("onesD", [128, 128])
    W64d = di("W64", [2, 128, 256])
    tabA = di("tabA", [S, 128]); tabB = di("tabB", [S, S // 128]); ctxCS = di("ctxCS", [2, CL, CL])
    if "out_dst" not in hooks:
        pa_out = P.dram("pa", [TT, D], F32, kind="ExternalOutput")
        hooks["out_dst"] = lambda tile: pa_out[tile * 128:(tile + 1) * 128, :]
    sk = "ExternalOutput" if dbg else "Internal"
    hT_s = [P.dram("hT_s%d" % i, [128, 8, ln + 2], BF16, kind=sk) for i, (_, ln, _) in enumerate(segs)]
    A_s = [P.dram("A_s%d" % d, [384, TT], F32, kind=sk) for d in range(2)]
    R_s = [P.dram("R_s%d" % d, [384, TT], F32, kind=sk) for d in range(2)]
    B_s = [P.dram("B_s%d" % d, [384, TT], F32, kind=sk) for d in range(2)]
    K_s = [P.dram("K_s%d" % d, [384, TT], F32, kind=sk) for d in range(2)]
    eL_s = [P.dram("eL_s%d" % d, [384, NCH], F32, kind=sk) for d in range(2)]
    V_s = P.dram("V_s", [TT, 384], F32, kind=sk)
    g_s = P.dram("g_s", [384, TT], F32, kind=sk)
    bon_s = P.dram("bon_s", [384, TT], F32, kind=sk)
    Y_s = [P.dram("Y_s%d" % d, [TT, 384], F32, kind=sk) for d in range(2)]

    ps = [P.ps("ps%d" % i, [128, 512]) for i in range(8)]

    def ld(name, src, shape, dt=F32, view=None):
        t = P.sb(name, shape, dt)
        P.dma("sp", t[:], src[:] if view is None else view)
        return t
    c_sb = ld("c_sb", cT, [128, 8, 2]); modb_s = ld("modb_s", modbc, [128, 16]); n1_s = ld("n1_s", n1c, [128, 8])
    vec_s = ld("vec_s", vecs, [128, 3, 5]); wa_s = ld("wa_s", w0a0, [128, 2, 2, 3])
    w2_s = ld("w2_s", w2d, [128, 384]); a2_s = ld("a2_s", a2d, [128, 384]); g2_s = ld("g2_s", g2d, [128, 384])
    bones_s = ld("bones_s", bonesd, [128, 128]); onesD_s = ld("onesD_s", onesDd, [128, 128])
    rmask_s = ld("rmask_s", rmaskd, [128, 512])
    omk = P.sb("omk", [128, 3])
    P.ts("dve", omk[:], vec_s[:, :, 1], -1.0, ALU.mult, 1.0, ALU.add)
    sc_sb = P.sb("sc_sb", [128, 8, 2])
    P.act(sc_sb[:], c_sb[:], AF.Silu)
    W64f = P.sb("W64f", [128, 2, 256])
    P.dma("sp", W64f[:], V(W64d, W64d.ap.rearrange("s p n -> p s n")))
    W64b = P.sb("W64b", [128, 2, 256], BF16)
    P.copy("dve", W64b[:], W64f[:])
    Zs = [P.sb("Zs%d" % i, [128, ln // 128, 256], BF16) for i, (_, ln, _) in enumerate(segs)]
    zero_b = P.sb("zero_b", [128, 8, 1], BF16)
    P.memset("dve", zero_b[:], 0.0)
    FT = P.sb("FT", [128, TT], BF16)

    m_w = P.mark()
    modc = P.sb("modc", [128, 16, 2])
    wmod = P.sb("wmod", [128, 8, 2])
    Wj = [P.sb("Wj%d" % i, [128, 8, 1536], BF16) for i in range(3)]
    Wf = P.sb("Wf", [128, 8, 128], BF16)
    m_phase = P.mark()
    xblk = P.sb("xblk", [128, 8, 512])
    hT = P.sb("hT", [128, 8, 512], BF16)
    sqb = [P.sb("sqb%d" % i, [128, 512]) for i in range(2)]
    rstd = P.sb("rstd", [128, 512])
    tmpf = [P.sb("tmpf%d" % i, [128, 512]) for i in range(2)]
    modw_v = modw.ap.rearrange("(k p) n -> p k n", p=128)
    for c4 in range(4):
        P.dma("sp", xblk[:], V(modw, modw_v[:, :, c4 * 512:(c4 + 1) * 512]))
        for q4 in range(4):
            cc = c4 * 4 + q4
            pr = ps[cc % 2]
            for k in range(8):
                P.mm(pr[:, 0:2], xblk[:, k, q4 * 128:(q4 + 1) * 128], sc_sb[:, k, :], start=(k == 0), stop=(k == 7))
            P.ts("dve", modc[:, cc, :], pr[:, 0:2], modb_s[:, cc:cc + 1], ALU.add)
    P.op("dve", lambda: nc.vector.scalar_tensor_tensor(out=wmod.ap, in0=modc.ap[:, 8:16, :], scalar=1.0,
                                                       in1=n1_s.ap.unsqueeze(2).broadcast_to([128, 8, 2]),
                                                       op0=ALU.add, op1=ALU.mult), [modc[:], n1_s[:]], [wmod[:]])
    mk_mu = P.mark()
    mu_r = [P.sb("mu_r%d" % i, [128, 1536]) for i in range(3)]
    P.dma("sp", mu_r[1][:], V(mup, mup.ap.partition_broadcast(128)))
    P.dma("sp", mu_r[2][:], V(mun, mun.ap.partition_broadcast(128)))
    P.tt("dve", mu_r[0][:], mu_r[1][:], mu_r[2][:], ALU.add)
    P.ts("dve", mu_r[0][:], mu_r[0][:], -1.0, ALU.mult, 1.0, ALU.add)
    wsel_v = wsel.ap.rearrange("(k p) n -> p k n", p=128)
    for c3 in range(3):
        P.dma("sp", xblk[:], V(wsel, wsel_v[:, :, c3 * 512:(c3 + 1) * 512]))
        for j in range(3):
            mb = mu_r[j].ap[:, c3 * 512:(c3 + 1) * 512].unsqueeze(1).broadcast_to([128, 8, 512])
            P.op("dve" if j != 1 else "pool",
                 (lambda j=j, mb=mb: P.e["dve" if j != 1 else "pool"].tensor_tensor(
                     out=Wj[j].ap[:, :, c3 * 512:(c3 + 1) * 512], in0=xblk.ap, in1=mb, op=ALU.mult)),
                 [xblk[:], mu_r[j][:]], [Wj[j][:]])
    P.dma("sp", xblk[:, :, 0:128], V(wsel, wsel_v[:, :, 1536:1664]))
    P.copy("dve", Wf[:], xblk[:, :, 0:128])
    P.barrier()
    P.release(mk_mu)

    for si, (tok0, ln, cls) in enumerate(segs):
        src = ctxT if si == 0 else xT
        P.dma("sp", hT_s[si][:, :, 0:1], zero_b[:], allow_slow_non_contiguous=True)
        P.dma("sp", hT_s[si][:, :, ln + 1:ln + 2], zero_b[:], allow_slow_non_contiguous=True)
        for c0 in range(0, ln, 512):
            w = min(512, ln - c0)
            P.dma("sp", xblk[:, :, 0:w], V(src, src.ap.rearrange("(k p) t -> p k t", p=128)[:, :, c0:c0 + w]))
            pr = ps[0]
            for k in range(8):
                sq = sqb[k % 2]
                P.act(sq[:, 0:w], xblk[:, k, 0:w], AF.Square)
                P.mm(pr[:, 0:w], onesD_s[:], sq[:, 0:w], start=(k == 0), stop=(k == 7))
            P.rsqrt(rstd[:, 0:w], pr[:, 0:w], 1.0 / D, 1e-6)
            for k in range(8):
                tf = tmpf[k % 2]
                P.tt("dve", tf[:, 0:w], xblk[:, k, 0:w], rstd[:, 0:w], ALU.mult)
                P.ts("pool", hT[:, k, 0:w], tf[:, 0:w], wmod[:, k, cls:cls + 1], ALU.mult, modc[:, k, cls:cls + 1], ALU.add)
            P.dma("sp", hT_s[si][:, :, 1 + c0:1 + c0 + w], hT[:, :, 0:w])
    if upto < 1:
        P.finish()
        return P
    P.barrier()
    P.release(m_phase)

    hwin = P.sb("hwin", [128, 8, 514], BF16)
    zt_ = {nm: [P.sb("z%s%d" % (nm, hp), [128, 512]) for hp in range(3)] for nm in ("r", "k", "v")}
    tw = P.sb("tw", [128, 512]); xa_s = P.sb("xa_s", [128, 512]); sg = P.sb("sg", [128, 512])
    fTb = P.sb("fTb", [128, 512], BF16)
    NTMP = 14
    tp = [P.sb("tp%d" % i, [128, 512]) for i in range(NTMP)]
    eLt = [P.sb("eLt%d" % i, [128, 8]) for i in range(2)]
    vst = [P.sb("vst%d" % i, [128, 128]) for i in range(2)]
    SHIFTS = ((0, 0), (1, -1), (2, 1))
    for si, (tok0, ln, cls) in enumerate(segs):
        for c0 in range(0, ln, 512):
            n = min(512, ln - c0)
            ncb = n // CH
            g0 = tok0 + c0
            P.dma("sp", hwin[:, :, 0:n + 2], hT_s[si][:, :, c0:c0 + n + 2])
            pi = [0]

            def nps():
                pi[0] = (pi[0] + 1) % 8
                return ps[pi[0]]

            def proj(col0, shifted=True):
                pr = nps()
                sh = SHIFTS if shifted else ((None, 0),)
                nmm = len(sh) * 8
                i = 0
                for (j, dj) in sh:
                    for k in range(8):
                        wt = Wj[j][:, k, col0:col0 + 128] if shifted else Wf[:, k, :]
                        P.mm(pr[:, 0:n], wt, hwin[:, k, 1 + dj:1 + dj + n], start=(i == 0), stop=(i == nmm - 1))
                        i += 1
                return pr
            for hp in range(3):
                P.copy("act", zt_["r"][hp][:, 0:n], proj(hp * 128)[:, 0:n])
                P.copy("dve", zt_["k"][hp][:, 0:n], proj(384 + hp * 128)[:, 0:n])
                P.copy("act", zt_["v"][hp][:, 0:n], proj(768 + hp * 128)[:, 0:n])
            P.act(tw[:, 0:n], proj(1152)[:, 0:n], AF.Tanh)
            P.copy("dve", xa_s[:, 0:n], proj(1280)[:, 0:n])
            P.act(sg[:, 0:n], proj(1408)[:, 0:n], AF.Sigmoid)
            P.copy("act", fTb[:, 0:n], proj(0, shifted=False)[:, 0:n])
            for tt_ in range(n // 128):
                pr = nps()
                P.mm(pr[:, 0:256], fTb[:, tt_ * 128:(tt_ + 1) * 128], W64b[:, si, :])
                P.copy("dve", Zs[si][:, c0 // 128 + tt_, :], pr[:, 0:256])
            for hp in range(3):
                zr, zk, zv = zt_["r"][hp], zt_["k"][hp], zt_["v"][hp]
                rows = slice(hp * 128, (hp + 1) * 128)
                cols = slice(g0, g0 + n)
                kkc, kac, rkc = vec_s[:, hp, 0:1], vec_s[:, hp, 1:2], vec_s[:, hp, 2:3]
                pr = nps()
                P.mm(pr[:, 0:n], g2_s[:, hp * 128:(hp + 1) * 128], sg[:, 0:n])
                P.copy("act", tp[0][:, 0:n], pr[:, 0:n])
                P.dma("sp", g_s[rows, cols], tp[0][:, 0:n])
                kk, kkn, aneg = tp[1], tp[2], tp[3]
                P.ts("dve", kk[:, 0:n], zk[:, 0:n], kkc, ALU.mult)
                P.act(tp[4][:, 0:n], kk[:, 0:n], AF.Square)
                pr = nps()
                P.mm(pr[:, 0:n], bones_s[:], tp[4][:, 0:n])
                P.act(tp[4][:, 0:n], pr[:, 0:n], AF.Sqrt)
                P.ts("dve", tp[4][:, 0:n], tp[4][:, 0:n], 1e-12, ALU.max)
                P.op("dve", lambda: nc.vector.reciprocal(out=tp[4].ap[:, 0:n], in_=tp[4].ap[:, 0:n]), [tp[4][:]], [tp[4][:]])
                P.tt("dve", kkn[:, 0:n], kk[:, 0:n], tp[4][:, 0:n], ALU.mult)
                P.ts("pool", aneg[:, 0:n], kkn[:, 0:n], -1.0, ALU.mult)
                bon = tp[5]
                for d in range(2):
                    dl = slice(d * 64, (d + 1) * 64)
                    lw, ag, kd, bb, F_, L_, Lx = tp[6], tp[7], tp[8], tp[9], tp[10], tp[11], tp[12]
                    pr = nps()
                    P.mm(pr[:, 0:n], w2_s[dl, hp * 128:(hp + 1) * 128], tw[dl, 0:n])
                    P.act(lw[:, 0:n], pr[:, 0:n], AF.Sigmoid, bias=wa_s[:, 0, d, hp:hp + 1])
                    P.ts("dve", lw[:, 0:n], lw[:, 0:n], NEG_EXP_HALF, ALU.mult)
                    pr = nps()
                    P.mm(pr[:, 0:n], a2_s[dl, hp * 128:(hp + 1) * 128], xa_s[dl, 0:n])
                    P.act(ag[:, 0:n], pr[:, 0:n], AF.Sigmoid, bias=wa_s[:, 1, d, hp:hp + 1])
                    P.ts("dve", kd[:, 0:n], ag[:, 0:n], kac, ALU.mult, omk[:, hp:hp + 1], ALU.add)
                    P.tt("dve", kd[:, 0:n], kd[:, 0:n], zk[:, 0:n], ALU.mult)
                    P.tt("pool", bb[:, 0:n], kkn[:, 0:n], ag[:, 0:n], ALU.mult)
                    P.stt("dve", tp[13][:, 0:n], zr[:, 0:n], rkc, kd[:, 0:n], ALU.mult, ALU.mult)
                    pr = nps()
                    P.mm(pr[:, 0:n], bones_s[:], tp[13][:, 0:n])
                    if d == 0:
                        P.tt("dve", bon[:, 0:n], pr[:, 0:n], zv[:, 0:n], ALU.mult)
                    else:
                        P.tt("dve", tp[13][:, 0:n], pr[:, 0:n], zv[:, 0:n], ALU.mult)
                        P.tt("pool", bon[:, 0:n], bon[:, 0:n], tp[13][:, 0:n], ALU.add)
                        P.dma("sp", bon_s[rows, cols], bon[:, 0:n])
                    P.op("dve", lambda: nc.vector.tensor_tensor_scan(out=F_.ap[:, 0:n], data0=rmask_s.ap[:, 0:n], data1=lw.ap[:, 0:n],
                                                                     initial=0.0, op0=ALU.mult, op1=ALU.add),
                         [rmask_s[:], lw[:]], [F_[:]])
                    if d == 0:
                        P.tt("pool", Lx[:, 0:n], F_[:, 0:n], lw[:, 0:n], ALU.subtract)
                        Lsrc = F_
                    else:
                        P.tt("dve", Lx[:, 0:n], lw[:, 0:n], F_[:, 0:n], ALU.subtract)
                        f3 = F_.ap[:, 0:n].rearrange("p (c t) -> p c t", t=CH)
                        l3 = L_.ap[:, 0:n].rearrange("p (c t) -> p c t", t=CH)
                        x3 = Lx.ap[:, 0:n].rearrange("p (c t) -> p c t", t=CH)
                        tot = f3[:, :, CH - 1:CH].broadcast_to([128, ncb, CH])
                        P.op("dve", (lambda l3=l3, x3=x3, tot=tot: nc.vector.tensor_tensor(out=l3, in0=x3, in1=tot, op=ALU.add)),
                             [Lx[:], F_[:]], [L_[:]])
                        P.tt("pool", Lx[:, 0:n], L_[:, 0:n], lw[:, 0:n], ALU.subtract)
                        Lsrc = L_
                    e1, e2, e3 = tp[13], tp[6], tp[10] if d == 1 else tp[11]
                    P.act(e1[:, 0:n], Lx[:, 0:n], AF.Exp)
                    P.act(e3[:, 0:n], Lsrc[:, 0:n], AF.Exp, scale=-1.0)
                    P.act(e2[:, 0:n], Lsrc[:, 0:n], AF.Exp)
                    elt = eLt[d]
                    e23 = e2.ap[:, 0:n].rearrange("p (c t) -> p c t", t=CH)
                    pos = CH - 1 if d == 0 else 0
                    P.op("pool", (lambda elt=elt, e23=e23, pos=pos: nc.gpsimd.tensor_copy(out=elt.ap[:, 0:ncb], in_=e23[:, :, pos])),
                         [e2[:]], [elt[:]])
                    P.dma("sp", eL_s[d][rows, g0 // CH:g0 // CH + ncb], elt[:, 0:ncb])
                    P.tt("dve", e1[:, 0:n], e1[:, 0:n], aneg[:, 0:n], ALU.mult)
                    P.dma("sp", A_s[d][rows, cols], e1[:, 0:n])
                    P.tt("pool", e2[:, 0:n], e2[:, 0:n], zr[:, 0:n], ALU.mult)
                    P.dma("sp", R_s[d][rows, cols], e2[:, 0:n])
                    P.tt("dve", bb[:, 0:n], bb[:, 0:n], e3[:, 0:n], ALU.mult)
                    P.dma("sp", B_s[d][rows, cols], bb[:, 0:n])
                    P.tt("pool", kd[:, 0:n], kd[:, 0:n], e3[:, 0:n], ALU.mult)
                    P.dma("sp", K_s[d][rows, cols], kd[:, 0:n])
                for tt_ in range(n // 128):
                    pr = nps()
                    i = 0
                    for (j, dj) in SHIFTS:
                        for k in range(8):
                            P.mm(pr[:, 0:128], hwin[:, k, 1 + dj + tt_ * 128:1 + dj + tt_ * 128 + 128],
                                 Wj[j][:, k, 768 + hp * 128:768 + (hp + 1) * 128], start=(i == 0), stop=(i == 23))
                            i += 1
                    vs_ = vst[tt_ % 2]
                    P.copy("act", vs_[:], pr[:, 0:128])
                    P.dma("sp", V_s[g0 + tt_ * 128:g0 + (tt_ + 1) * 128, hp * 128:(hp + 1) * 128], vs_[:])
    if upto < 2:
        P.finish()
        return P
    P.barrier()
    P.release(m_w)

    m_s = P.mark()
    ncc = CL // CH
    order = [list(range(NCH)), list(range(ncc - 1, -1, -1)) + list(range(NCH - 1, ncc - 1, -1))]
    mAT_s = P.sb("mAT_s", [128, 2, 128]); mN_s = P.sb("mN_s", [64, 2, 64]); id_s = P.sb("id_s", [64, 64])
    P.dma("sp", mAT_s[:], V(mAT, mAT.ap.rearrange("d p n -> p d n")))
    P.dma("sp", mN_s[:], V(mN, mN.ap.rearrange("d p n -> p d n")))
    P.dma("sp", id_s[:], id64[:])
    mAK_s = P.sb("mAK_s", [128, 2, 64])
    P.dma("sp", mAK_s[:], V(mAK, mAK.ap.rearrange("d p n -> p d n")))
    kj = lambda t_: t_.ap.rearrange("(j k) t -> k j t", k=64)
    sbd = lambda nm, shp: [[P.sb("%s_%d_%d" % (nm, d, i), shp) for i in range(2)] for d in range(2)]
    AR = sbd("AR", [128, 6, 2, 64]); BK = sbd("BK", [64, 6, 2, 64]); UV = sbd("UV", [128, 6, 64])
    for d in range(2):
        for i in range(2):
            P.memset("pool", AR[d][i][:], 0.0)
            P.memset("pool", UV[d][i][:], 0.0)
    eLa = [P.sb("eLa%d" % d, [64, 6, NCH]) for d in range(2)]
    for d in range(2):
        P.dma("sp", eLa[d][:], V(eL_s[d], kj(eL_s[d])))
    Xb = sbd("Xb", [64, 6, 64]); Zb = sbd("Zb", [64, 6, 64])
    one = lambda nm, shp: [P.sb("%s_%d" % (nm, d), shp) for d in range(2)]
    ATs = one("ATs", [128, 6, 128]); Rm = one("Rm", [64, 6, 64]); BKt = one("BKt", [128, 6, 64])
    W0s = one("W0s", [64, 6, 64]); Ys = one("Ys", [64, 6, 64]); tS = one("tS", [64, 6, 64]); ST = one("ST", [128, 6, 64])
    AakP = one("AakP", [128, 6, 64])
    for d in range(2):
        P.memset("pool", ST[d][:], 0.0)
    ring = [0, 0]

    def nb(d):
        ring[d] = (ring[d] + 1) % 4
        return ps[d * 4 + ring[d]]

    def loads(n):
        for d in range(2):
            c = order[d][n]
            cs = slice(c * CH, (c + 1) * CH)
            pb = n % 2
            P.dma("sp", AR[d][pb][0:64, :, 0, :], V(A_s[d], kj(A_s[d])[:, :, cs]))
            P.dma("sp", AR[d][pb][0:64, :, 1, :], V(R_s[d], kj(R_s[d])[:, :, cs]))
            P.dma("sp", BK[d][pb][:, :, 0, :], V(B_s[d], kj(B_s[d])[:, :, cs]))
            P.dma("sp", BK[d][pb][:, :, 1, :], V(K_s[d], kj(K_s[d])[:, :, cs]))
            P.dma("sp", UV[d][pb][64:128, :, :], V(V_s, V_s.ap[cs, :].rearrange("t (j v) -> t j v", v=64)))

    def v3(t_, rows=slice(0, 64), width=384):
        return V(t_, t_.ap[rows, 0:width].rearrange("p (j v) -> p j v", j=6))

    loads(0)
    for n in range(NCH):
        pb = n % 2
        if n + 1 < NCH:
            loads(n + 1)
        ar = [AR[d][pb] for d in range(2)]; bk = [BK[d][pb] for d in range(2)]; uv = [UV[d][pb] for d in range(2)]
        for d in range(2):
            for g in range(2):
                pa = nb(d)
                for jj in range(3):
                    j = g * 3 + jj
                    P.mm(pa[:, jj * 128:(jj + 1) * 128], V(bk[d], bk[d].ap[:, j].rearrange("p a i -> p (a i)")),
                         V(ar[d], ar[d].ap[0:64, j].rearrange("p a i -> p (a i)")))
                mk_ = mAK_s.ap[:, d, :].unsqueeze(1).broadcast_to([128, 3, 64])
                P.op("dve", (lambda pa=pa, d=d, g=g, mk_=mk_: nc.vector.tensor_tensor(
                    out=AakP[d].ap[:, g * 3:(g + 1) * 3, :], in0=pa.ap[:, 0:384].rearrange("p (j n) -> p j n", j=3)[:, :, 0:64], in1=mk_, op=ALU.mult)),
                    [pa[:], mAK_s[:]], [AakP[d][:]])
                mb = mAT_s.ap[:, d, :].unsqueeze(1).broadcast_to([128, 3, 128])
                P.op("dve", (lambda pa=pa, d=d, g=g, mb=mb: nc.vector.tensor_tensor(
                    out=ATs[d].ap[:, g * 3:(g + 1) * 3, :], in0=pa.ap[:, 0:384].rearrange("p (j n) -> p j n", j=3), in1=mb, op=ALU.mult)),
                    [pa[:], mAT_s[:]], [ATs[d][:]])
        pn = [nb(d) for d in range(2)]
        for d in range(2):
            for j in range(6):
                P.mm(pn[d][0:64, j * 64:(j + 1) * 64], ar[d][0:64, j, 0, :], bk[d][:, j, 0, :])
            mb = mN_s.ap[:, d, :].unsqueeze(1).broadcast_to([64, 6, 64])
            P.op("dve", (lambda d=d, mb=mb: nc.vector.tensor_tensor(out=Xb[d][0].ap, in0=pn[d].ap[0:64, 0:384].rearrange("p (j n) -> p j n", j=6),
                                                                    in1=mb, op=ALU.mult)), [pn[d][:], mN_s[:]], [Xb[d][0][:]])
            ib = id_s.ap.unsqueeze(1).broadcast_to([64, 6, 64])
            P.op("pool", (lambda d=d, ib=ib: nc.gpsimd.tensor_tensor(out=Rm[d].ap, in0=ATs[d].ap[0:64, :, 0:64], in1=ib, op=ALU.add)),
                 [ATs[d][:], id_s[:]], [Rm[d][:]])
        pk = [nb(d) for d in range(2)]
        for d in range(2):
            for j in range(6):
                P.mm(pk[d][:, j * 64:(j + 1) * 64], V(bk[d], bk[d].ap[:, j].rearrange("p a i -> p (a i)")), id_s[:])
            P.copy("act", BKt[d][:], v3(pk[d], slice(0, 128)))
        Xp = [Xb[d][0] for d in range(2)]
        Zp = [V(ATs[d], ATs[d].ap[0:64, :, 0:64]) for d in range(2)]
        for lv in range(1, 6):
            px = [nb(d) for d in range(2)]
            for d in range(2):
                for j in range(6):
                    P.mm(px[d][0:64, j * 64:(j + 1) * 64], V(Zp[d].t, Zp[d].ap[:, j, :]), Xp[d][:, j, :])
            Xn = [Xb[d][lv % 2] for d in range(2)]
            for d in range(2):
                P.copy("act", Xn[d][:], v3(px[d]))
            if lv < 5:
                pz = [nb(d) for d in range(2)]
                for d in range(2):
                    for j in range(6):
                        P.mm(pz[d][0:64, j * 64:(j + 1) * 64], Xp[d][:, j, :], V(Zp[d].t, Zp[d].ap[:, j, :]))
                Zn = [Zb[d][lv % 2] for d in range(2)]
                for d in range(2):
                    P.copy("dve", Zn[d][:], v3(pz[d]))
            prr = [nb(d) for d in range(2)]
            for d in range(2):
                for j in range(6):
                    P.mm(prr[d][0:64, j * 64:(j + 1) * 64], Xn[d][:, j, :], Rm[d][:, j, :])
            for d in range(2):
                P.tt("dve", Rm[d][:], v3(prr[d]), Rm[d][:], ALU.add)
            Xp = Xn
            if lv < 5:
                Zp = [Zn[d][:] for d in range(2)]
        pw = [nb(d) for d in range(2)]
        for d in range(2):
            for j in range(6):
                o_ = pw[d][0:64, j * 64:(j + 1) * 64]
                P.mm(o_, ar[d][:, j, 0, :], ST[d][:, j, :], start=True, stop=False)
                P.mm(o_, AakP[d][:, j, :], uv[d][:, j, :], start=False, stop=True)
            P.copy("act", W0s[d][:], v3(pw[d]))
        pu_ = [nb(d) for d in range(2)]
        for d in range(2):
            for j in range(6):
                P.mm(pu_[d][0:64, j * 64:(j + 1) * 64], Rm[d][:, j, :], W0s[d][:, j, :])
            P.copy("dve", uv[d][0:64, :, :], v3(pu_[d]))
        py = [nb(d) for d in range(2)]
        for d in range(2):
            for j in range(6):
                o_ = py[d][0:64, j * 64:(j + 1) * 64]
                P.mm(o_, ar[d][:, j, 1, :], ST[d][:, j, :], start=True, stop=False)
                P.mm(o_, ATs[d][:, j, 64:128], uv[d][:, j, :], start=False, stop=True)
            P.copy("act", Ys[d][:], v3(py[d]))
            c = order[d][n]
            P.dma("sp", V(Y_s[d], Y_s[d].ap[c * CH:(c + 1) * CH, :].rearrange("t (j v) -> t j v", v=64)), Ys[d][:])
        pst = [nb(d) for d in range(2)]
        for d in range(2):
            for j in range(6):
                P.mm(pst[d][0:64, j * 64:(j + 1) * 64], BKt[d][:, j, :], uv[d][:, j, :])
            P.tt("dve", tS[d][:], v3(pst[d]), ST[d][0:64, :, :], ALU.add)
            eb = eLa[d].ap[:, :, order[d][n]].unsqueeze(2).broadcast_to([64, 6, 64])
            P.op("pool", (lambda d=d, eb=eb: nc.gpsimd.tensor_tensor(out=ST[d].ap[0:64], in0=tS[d].ap, in1=eb, op=ALU.mult)),
                 [tS[d][:], eLa[d][:]], [ST[d][:]])
    if upto < 3:
        P.finish()
        return P
    P.barrier()
    P.release(m_s)

    m_n = P.mark()
    import math
    ccs = P.sb("ccs", [128, CL // 128, 2, CL], BF16)
    cstage = P.sb("cstage", [128, CL // 128, 2, CL])
    for c_ in range(2):
        P.dma("sp", cstage[:, :, c_, :], V(ctxCS, ctxCS.ap[c_].rearrange("(a p) n -> p a n", p=128)))
    P.copy("dve", ccs[:], cstage[:])
    pr = ps[0]
    ntc = CL // 128
    for ti in range(ntc):
        P.mm(pr[:, 0:CL], Zs[0][:, ti, 0:128], ccs[:, ti, 0, :], start=(ti == 0), stop=False)
        P.mm(pr[:, 0:CL], Zs[0][:, ti, 128:256], ccs[:, ti, 1, :], start=False, stop=(ti == ntc - 1))
    P.copy("act", FT[:, 0:CL], pr[:, 0:CL])
    NTT = S // 128
    PW = min(S, 2048)
    HP = PW // 128
    NBLK = PW // 512
    tA = P.sb("tA", [128, NTT, 128]); tB = P.sb("tB", [128, NTT, S // 128])
    P.dma("sp", tA[:], V(tabA, tabA.ap.rearrange("(a p) n -> p a n", p=128)))
    P.dma("sp", tB[:], V(tabB, tabB.ap.rearrange("(a p) n -> p a n", p=128)))
    negpi = P.sb("negpi", [128, 2])
    SHR = 1.0 - 1e-6
    P.memset("dve", negpi[:, 0:1], -math.pi * SHR)
    P.memset("dve", negpi[:, 1:2], -0.5 * math.pi * SHR)
    kang = 2.0 * math.pi / S * SHR
    mm_ = [P.sb("mm_%d" % i, [128, PW]) for i in range(2)]
    mc_ = [P.sb("mc_%d" % i, [128, PW]) for i in range(2)]
    Ct = [P.sb("Ct%d" % i, [128, PW], BF16) for i in range(2)]
    St = [P.sb("St%d" % i, [128, PW], BF16) for i in range(2)]
    for pz_ in range(S // PW):
        for ti in range(NTT):
            b2 = ti % 2
            m3 = mm_[b2].ap.rearrange("p (h l) -> p h l", l=128)
            i0 = tB.ap[:, ti, pz_ * HP:(pz_ + 1) * HP].unsqueeze(2).broadcast_to([128, HP, 128])
            i1 = tA.ap[:, ti, :].unsqueeze(1).broadcast_to([128, HP, 128])
            P.op("dve", (lambda m3=m3, i0=i0, i1=i1: nc.vector.tensor_tensor(out=m3, in0=i0, in1=i1, op=ALU.add)),
                 [tA[:], tB[:]], [mm_[b2][:]])
            P.ts("dve", mc_[b2][:], mm_[b2][:], float(S), ALU.is_ge, -float(S), ALU.mult)
            P.tt("pool", mm_[b2][:], mm_[b2][:], mc_[b2][:], ALU.add)
            P.act(St[b2][:], mm_[b2][:], AF.Sin, bias=negpi[:, 0:1], scale=kang)
            P.ts("dve", mc_[b2][:], mm_[b2][:], 0.75 * S, ALU.is_ge, -float(S), ALU.mult)
            P.tt("pool", mc_[b2][:], mc_[b2][:], mm_[b2][:], ALU.add)
            P.act(Ct[b2][:], mc_[b2][:], AF.Sin, bias=negpi[:, 1:2], scale=kang)
            for blk in range(NBLK):
                P.mm(ps[blk][:], Zs[1][:, ti, 0:128], Ct[b2][:, blk * 512:(blk + 1) * 512], start=(ti == 0), stop=False)
                P.mm(ps[blk][:], Zs[1][:, ti, 128:256], St[b2][:, blk * 512:(blk + 1) * 512], start=False, stop=(ti == NTT - 1))
        for blk in range(NBLK):
            c0 = CL + pz_ * PW + blk * 512
            P.copy("act" if blk % 2 == 0 else "dve", FT[:, c0:c0 + 512], ps[blk][:])
    if upto < 4:
        P.finish()
        return P
    P.barrier()
    P.release(m_n)

    wo_s = P.sb("wo_s", [128, 4, D], BF16)
    wstage = P.sb("wstage", [128, 4, D])
    P.dma("sp", wstage[:], wod[:])
    P.copy("dve", wo_s[:], wstage[:])
    yf = [P.sb("yf%d" % i, [128, 384]) for i in range(2)]
    yb = [P.sb("yb%d" % i, [128, 384]) for i in range(2)]
    dd = P.sb("dd", [128, 384]); sq = P.sb("sq", [128, 384]); st6 = P.sb("st6", [128, 6]); st6b = P.sb("st6b", [128, 6])
    idf = P.sb("idf", [128, 128])
    P.dma("sp", idf[0:64, 0:64], id64[:])
    bonT = [P.sb("bonT%d" % i, [128, 512]) for i in range(3)]
    gT = [P.sb("gT%d" % i, [128, 512]) for i in range(3)]
    oT = [P.sb("oT%d" % i, [128, 512], BF16) for i in range(3)]
    ot32 = P.sb("ot32", [128, 512])
    ostg = [P.sb("ostg%d" % i, [128, D]) for i in range(2)]
    P.memset("dve", idf[0:64, 64:128], 0.0)
    P.memset("dve", idf[64:128, 0:64], 0.0)
    P.dma("sp", idf[64:128, 64:128], id64[:])
    for (tok0, ln, cls) in segs:
        for c0 in range(0, ln, 512):
            n = min(512, ln - c0)
            g0 = tok0 + c0
            for hp in range(3):
                P.dma("sp", bonT[hp][:, 0:n], bon_s[hp * 128:(hp + 1) * 128, g0:g0 + n])
                P.dma("sp", gT[hp][:, 0:n], g_s[hp * 128:(hp + 1) * 128, g0:g0 + n])
            for tt_ in range(n // 128):
                r0 = g0 + tt_ * 128
                a_, b_ = yf[tt_ % 2], yb[tt_ % 2]
                P.dma("sp", a_[:], Y_s[0][r0:r0 + 128, :])
                P.dma("sp", b_[:], Y_s[1][r0:r0 + 128, :])
                P.tt("pool", a_[:], a_[:], b_[:], ALU.add)
                a3 = a_.ap.rearrange("p (j v) -> p j v", v=64)
                d3 = dd.ap.rearrange("p (j v) -> p j v", v=64)
                P.op("dve", (lambda a3=a3: nc.vector.tensor_reduce(out=st6.ap, in_=a3, axis=AX.X, op=ALU.add)), [a_[:]], [st6[:]])
                P.ts("dve", st6[:], st6[:], 1.0 / 64, ALU.mult)
                P.op("dve", (lambda a3=a3, d3=d3: nc.vector.tensor_tensor(out=d3, in0=a3, in1=st6.ap.unsqueeze(2).broadcast_to([128, 6, 64]),
                                                                          op=ALU.subtract)), [a_[:], st6[:]], [dd[:]])
                P.act(sq[:], dd[:], AF.Square)
                P.op("dve", lambda: nc.vector.tensor_reduce(out=st6b.ap, in_=sq.ap.rearrange("p (j v) -> p j v", v=64), axis=AX.X, op=ALU.add),
                     [sq[:]], [st6b[:]])
                P.rsqrt(st6b[:], st6b[:], 1.0 / 64, 64e-5)
                P.op("dve", (lambda d3=d3: nc.vector.tensor_tensor(out=d3, in0=d3, in1=st6b.ap.unsqueeze(2).broadcast_to([128, 6, 64]),
                                                                   op=ALU.mult)), [dd[:], st6b[:]], [dd[:]])
                for hp in range(3):
                    P.tr(ps[hp][:, tt_ * 128:(tt_ + 1) * 128], dd[:, hp * 128:(hp + 1) * 128], idf[:])
            for hp in range(3):
                P.ts("dve", ot32[:, 0:n], ps[hp][:, 0:n], vec_s[:, hp, 3:4], ALU.mult, vec_s[:, hp, 4:5], ALU.add)
                P.tt("pool", ot32[:, 0:n], ot32[:, 0:n], bonT[hp][:, 0:n], ALU.add)
                P.tt("dve", oT[hp][:, 0:n], ot32[:, 0:n], gT[hp][:, 0:n], ALU.mult)
            for tt_ in range(n // 128):
                tsl = slice(tt_ * 128, (tt_ + 1) * 128)
                og = ostg[tt_ % 2]
                for ch in range(2):
                    pw_ = ps[4 + ch]
                    for hp in range(3):
                        P.mm(pw_[:], oT[hp][:, tsl], wo_s[:, hp, ch * 512:(ch + 1) * 512], start=(hp == 0), stop=False)
                    P.mm(pw_[:], FT[:, g0 + tt_ * 128:g0 + (tt_ + 1) * 128], wo_s[:, 3, ch * 512:(ch + 1) * 512], start=False, stop=True)
                    P.copy("act" if ch == 0 else "dve", og[:, ch * 512:(ch + 1) * 512], pw_[:])
                P.dma("sp", hooks["out_dst"](g0 // 128 + tt_), og[:])
    if standalone:
        P.finish()
    return P


def l0_consts(mod_w, mod_b, norm1, w_in, shift_prev, shift_next, w0, w2, a0, a2, g2, k_k, k_a, r_k, lnx_g, lnx_b,
              w_out, s, S, CL):
    f = np.float32
    c384 = slice(384 * s, 384 * (s + 1))
    sel = np.concatenate([np.arange(384 * s, 384 * (s + 1)), 768 + np.arange(384 * s, 384 * (s + 1)),
                          1536 + np.arange(384 * s, 384 * (s + 1)), np.arange(2304, 2688)])
    fsel = 2688 + np.arange(128 * s, 128 * (s + 1))
    wsel = np.ascontiguousarray(np.concatenate([w_in[:, sel], w_in[:, fsel]], 1))
    ch = lambda v: np.asarray(v, f).reshape(-1)[c384].reshape(3, 128).T
    vecs = np.ascontiguousarray(np.stack([ch(k_k), ch(k_a), ch(r_k), ch(lnx_g), ch(lnx_b)], -1))
    w0a0 = np.zeros((128, 2, 2, 3), f)
    for d in range(2):
        w0a0[:, 0, d, :] = ch(w0[d])
        w0a0[:, 1, d, :] = ch(a0[d])
    wo = np.zeros((128, 4, D), f)
    wo[:, 0:3, :] = w_out[c384].reshape(3, 128, D).transpose(1, 0, 2)
    wo[:, 3, :] = w_out[768 + 128 * s:768 + 128 * (s + 1)]
    ii = np.arange(64)
    mAT = np.zeros((2, 128, 128), f)
    mN = np.zeros((2, 64, 64), f)
    mAK = np.zeros((2, 128, 64), f)
    for d in range(2):
        before = (ii[:, None] < ii[None, :]) if d == 0 else (ii[:, None] > ii[None, :])
        beq = before | (ii[:, None] == ii[None, :])
        for r0 in (0, 64):
            mAT[d, r0:r0 + 64, 0:64] = before
            mAT[d, r0:r0 + 64, 64:128] = beq
        mN[d] = before.T
        mAK[d, 64:128, :] = before
    rmask = np.ones((128, 512), f)
    rmask[:, ::64] = 0
    bones = np.zeros((128, 128), f)
    bones[0:64, 0:64] = 1
    bones[64:, 64:] = 1
    cc = np.arange(64)
    ang = 2 * np.pi * ((cc[:, None] * cc[None, :]) % 64) / 64.0
    C64 = np.zeros((128, 128)); S64 = np.zeros((128, 128))
    for g in range(2):
        C64[g * 64:(g + 1) * 64, g * 64:(g + 1) * 64] = np.cos(ang)
        S64[g * 64:(g + 1) * 64, g * 64:(g + 1) * 64] = np.sin(ang)
    al_c = 1.0 / np.sqrt(CL * 64.0)
    al_l = 1.0 / np.sqrt(S * 64.0)
    W64 = np.stack([np.concatenate([al_c * C64, -al_c * S64], 1), np.concatenate([-al_l * C64, al_l * S64], 1)], 0).astype(f)
    t = np.arange(S, dtype=np.int64)
    tabA = ((t[:, None] * np.arange(128)[None, :]) % S).astype(f)
    tabB = ((128 * t[:, None] * np.arange(S // 128)[None, :]) % S).astype(f)
    tc = np.arange(CL, dtype=np.int64)
    angc = 2 * np.pi * ((tc[:, None] * tc[None, :]) % CL) / float(CL)
    ctxCS = np.stack([np.cos(angc), np.sin(angc)], 0).astype(f)
    return {
        "modw": np.ascontiguousarray(mod_w[:, 0:2048]), "modbc": colmat(mod_b[0:2048], 16), "n1c": colmat(norm1, 8),
        "wsel": wsel, "mup": np.ascontiguousarray(shift_prev[sel][None].astype(f)),
        "mun": np.ascontiguousarray(shift_next[sel][None].astype(f)),
        "vecs": vecs, "w0a0": w0a0,
        "w2": np.ascontiguousarray(w2[:, :, c384].reshape(128, 384)), "a2": np.ascontiguousarray(a2[:, :, c384].reshape(128, 384)),
        "g2": np.ascontiguousarray(g2[:, c384]), "wo": wo, "mAT": mAT, "mN": mN, "mAK": mAK, "id64": np.eye(64, dtype=f),
        "rmask": rmask, "bones": bones, "onesD": np.ones((128, 128), f), "W64": W64,
        "tabA": tabA, "tabB": tabB, "ctxCS": ctxCS,
    }


def l0_core_inputs(xb, ctxb, c_b, c_ctx):
    return {"xT": np.ascontiguousarray(xb.T), "ctxT": np.ascontiguousarray(ctxb.T), "cT": c_cols(c_b, c_ctx)}


def build_fused(S=8192, CL=256, n_cores=8, stop_after=None, part=None):
    P = Prog(num_devices=n_cores)
    nc = P.nc
    TT = CL + S
    H = S // 2
    HC = CL // 2
    NTL = H // 128
    NTC = HC // 128
    NT0 = NTL + NTC
    CT = CL // 128
    TILES = TT // 128
    cT = P.dram("cT", [128, 8, 2], F32, kind="ExternalInput")
    xh = P.dram("xh", [NT0 * 128, D], F32, kind="ExternalInput") if part != 2 else None
    y = P.dram("y", [H, D], F32, kind="ExternalOutput") if part != 1 else None
    mixS = P.dram("mixS", [2 * TT, D], F32, shared=True, persist=True)
    if part == 2:
        x1T = P.dram("x1T", [TILES + 1, D, 128], F32, kind="ExternalInput")
    else:
        x1T = P.dram("x1T", [TILES + 1, D, 128], F32, shared=True, persist=True)
    paS = P.dram("paS", [2 * S, D], F32, shared=True, persist=True)
    mixL = P.dram("mixL", [TT, D], F32, persist=True)
    partL = [P.dram("partL%d" % i, [NT0 * 128, D], F32, persist=True) for i in range(2)]
    io_kind = {1: "ExternalOutput", 2: "ExternalInput"}.get(part, "Internal")
    x1L = P.dram("x1L", [NT0 * 128, D], F32, kind=io_kind, persist=True)
    x1TL = P.dram("x1TL", [NT0, D, 128], F32, kind="ExternalOutput" if part == 1 else "Internal", persist=True)
    xcL = P.dram("xcL", [NTL + 2, D, 128], F32, persist=True)
    paL = P.dram("paL", [S, D], F32, persist=True)
    paP = [P.dram("paP%d" % i, [H, D], F32, persist=True) for i in range(2)]
    pcL = P.dram("pcL", [H, D], F32, persist=True)

    def parity(q):
        e = P.e[q]
        return e.snap(e.partition_id() % 2, min_val=0, max_val=1)

    def tview(t, tile, lo=0, hi=128):
        return V(t, t.ap[tile:tile + 1].rearrange("o (k p) t -> p (o k) t", p=128)[:, :, lo:hi])

    r_sp = r_act = None
    if part == 2:
        r_sp = parity("sp")
        r_act = parity("act")
    if part != 2:
      P.stage_begin("A_")
      if part is None:
          zpad = P.sb("zpad", [128, 8, 128])
          P.memset("dve", zpad[:], 0.0)
          P.dma("sp", tview(x1T, TILES), zpad[:])
      build_l0(S, CL, P=P, hooks={"cT": cT, "out_dst": lambda tile: mixL[tile * 128:(tile + 1) * 128, :]})
      r_sp = parity("sp")
      P.dma("sp", V(mixS, mixS.ap[bass.ds(r_sp * TT, TT), :]), mixL[:])
      P.stage_end()
      P.stage_begin("B_")
      r_act = parity("act")
      for i in range(2):
          P.dma("act", partL[i][0:NTL * 128, :], V(mixS, mixS.ap[bass.ds(r_act * (NTL * 128) + (i * TT + CT * 128), NTL * 128), :]))
          P.dma("act", partL[i][NTL * 128:NT0 * 128, :], V(mixS, mixS.ap[bass.ds(r_act * (NTC * 128) + i * TT, NTC * 128), :]))
      build_post(NT0, NTL, 2, False, P=P, hooks={
          "cT": cT,
          "x_src": lambda it: xh[it * 128:(it + 1) * 128, :],
          "part_src": lambda i, it: partL[i][it * 128:(it + 1) * 128, :],
          "y_dst": lambda it: x1L[it * 128:(it + 1) * 128, :],
          "yT_dst": lambda it: tview(x1TL, it),
      })
      if part == 1:
          P.stage_end(core_sync=False)
          P.finish()
          return P
      r_pool = parity("pool")
      P.dma("pool", V(x1T, x1T.ap[bass.ds(r_pool * NTL + CT, NTL)]), x1TL[0:NTL])
      P.dma("pool", V(x1T, x1T.ap[bass.ds(r_pool * NTC, NTC)]), x1TL[NTL:NT0])
      P.stage_end()
      if stop_after == "B":
          P.finish()
          return P
    P.stage_begin("C_")
    P.dma("sp", xcL[:], V(x1T, x1T.ap[bass.ds(r_sp * NTL + (CT - 1), NTL + 2)]))

    def h_src(kind, c0, w):
        if kind == "lat":
            return [(i * 128, 128, tview(x1T, CT + c0 // 128 + i)) for i in range(w // 128)]
        if kind == "ctx":
            return [(i * 128, 128, tview(x1T, c0 // 128 + i)) for i in range(w // 128)]
        t0 = 1 + c0 // 128
        return [(0, 15, tview(xcL, t0 - 1, 113, 128)), (15, 128, tview(xcL, t0)), (143, 128, tview(xcL, t0 + 1)),
                (271, 15, tview(xcL, t0 + 2, 0, 15))]
    build_l1(S, CL, H, P=P, hooks={
        "cT": cT, "h_src": h_src,
        "pa_dst": lambda tile: paL[tile * 128:(tile + 1) * 128, :],
        "pc_dst": lambda tile: pcL[tile * 128:(tile + 1) * 128, :],
    })
    P.dma("sp", V(paS, paS.ap[bass.ds(r_sp * S, S), :]), paL[:])
    P.stage_end()
    if stop_after == "C":
        P.finish()
        return P
    P.stage_begin("D_")
    for i in range(2):
        P.dma("act", paP[i][:], V(paS, paS.ap[bass.ds(r_act * H + i * S, H), :]))
    build_post(NTL, NTL, 3, True, P=P, hooks={
        "cT": cT,
        "x_src": lambda it: x1L[it * 128:(it + 1) * 128, :],
        "part_src": lambda i, it: (paP[i] if i < 2 else pcL)[it * 128:(it + 1) * 128, :],
        "y_dst": lambda it: y[it * 128:(it + 1) * 128, :],
    })
    P.stage_end(core_sync=False)
    P.finish()
    return P


_L0N = ["mod_w", "mod_b", "norm1", "w_in", "shift_prev", "shift_next", "w0", "w2", "a0", "a2", "g2", "k_k", "k_a", "r_k",
        "lnx_g", "lnx_b", "w_out"]
_L1N = ["mod_w", "mod_b", "norm1", "w_in", "q_norm", "k_norm", "dw_w", "dw_b", "cn_g", "cn_b", "w_out"]
_PROGS = {}
FUSED = False


def kernel(**inp):
    f = np.float32
    g = lambda k: np.ascontiguousarray(np.asarray(inp[k], dtype=f))
    x = g("x"); ctx = g("ctx"); c = g("c"); c_ctx = g("c_ctx")
    B, S, _ = x.shape
    CL = ctx.shape[1]
    H = S // 2
    HC = CL // 2
    NTL, NTC, CT = H // 128, HC // 128, CL // 128
    NT0 = NTL + NTC
    TILES = (S + CL) // 128
    n_cores = 2 * B
    pre = lambda p, d: {p + k: v for k, v in d.items()}

    def progs(part):
        key = (S, CL, n_cores, part)
        if key not in _PROGS:
            _PROGS[key] = build_fused(S, CL, n_cores, part=part)
        return _PROGS[key]

    def edge_of(s):
        e = np.zeros((128, 2), f)
        e[:, 0] = 1.0 if s == 1 else 0.0
        e[:, 1] = 1.0 if s == 0 else 0.0
        return e

    def in_l0():
        cons0 = [pre("A_", l0_consts(*[g("l0_" + n) for n in _L0N], s, S, CL)) for s in range(2)]
        consB = pre("B_", post_consts(g("l0_mod_w"), g("l0_mod_b"), g("l0_norm2"), g("norm_f"), g("l0_pq"), g("l0_sk1"),
                                      g("l0_sk2"), g("l0_pu"), g("l0_pv")))
        ims = []
        for b in range(B):
            xT = np.ascontiguousarray(x[b].T)
            ctxT = np.ascontiguousarray(ctx[b].T)
            for s in range(2):
                im = {}
                im.update(cons0[s]); im.update(consB)
                im["A_xT"] = xT
                im["A_ctxT"] = ctxT
                im["cT"] = c_cols(c[b], c_ctx)
                im["xh"] = np.ascontiguousarray(np.concatenate([x[b, H * s:H * (s + 1)], ctx[b, HC * s:HC * (s + 1)]], 0))
                ims.append(im)
        return ims

    def in_l1():
        cons1 = [pre("C_", l1_consts(*[g("l1_" + n) for n in _L1N], s, S)) for s in range(2)]
        consD = pre("D_", post_consts(g("l1_mod_w"), g("l1_mod_b"), g("l1_norm2"), g("norm_f"), g("l1_pq"), g("l1_sk1"),
                                      g("l1_sk2"), g("l1_pu"), g("l1_pv")))
        ims = []
        for b in range(B):
            for s in range(2):
                im = {}
                im.update(cons1[s]); im.update(consD)
                im["cT"] = c_cols(c[b], c_ctx)
                im["C_edge"] = edge_of(s)
                ims.append(im)
        return ims

    if FUSED:
        P = progs(None)
        ims = [dict(a, **b_) for a, b_ in zip(in_l0(), in_l1())]
        res = run_bass_kernel_spmd(P.nc, ims, core_ids=list(range(n_cores))).results
    else:
        r1 = run_bass_kernel_spmd(progs(1).nc, in_l0(), core_ids=list(range(n_cores))).results
        x1 = np.zeros_like(x)
        ctx1 = np.zeros_like(ctx)
        for b in range(B):
            for s in range(2):
                yb = r1[2 * b + s]["x1L"]
                x1[b, H * s:H * (s + 1)] = yb[0:H]
                ctx1[b, HC * s:HC * (s + 1)] = yb[H:H + HC]
        del r1
        key = ("l1", S, CL)
        if key not in _PROGS:
            _PROGS[key] = build_l1(S, CL, H)
        cons1 = [l1_consts(*[g("l1_" + n) for n in _L1N], s, S) for s in range(2)]
        ims = []
        for b in range(B):
            for s in range(2):
                im = dict(cons1[s])
                im.update(l1_core_inputs(x1[b], ctx1[b], c[b], c_ctx, s, H))
                ims.append(im)
        r2 = run_bass_kernel_spmd(_PROGS[key].nc, ims, core_ids=list(range(n_cores))).results
        key = ("post1", H)
        if key not in _PROGS:
            _PROGS[key] = build_post(NTL, NTL, 3, True)
        consD = post_consts(g("l1_mod_w"), g("l1_mod_b"), g("l1_norm2"), g("norm_f"), g("l1_pq"), g("l1_sk1"),
                            g("l1_sk2"), g("l1_pu"), g("l1_pv"))
        ims = []
        for b in range(B):
            for s in range(2):
                im = dict(consD)
                im["x"] = np.ascontiguousarray(x1[b, H * s:H * (s + 1)])
                im["p0"] = np.ascontiguousarray(r2[2 * b]["pa"][H * s:H * (s + 1)])
                im["p1"] = np.ascontiguousarray(r2[2 * b + 1]["pa"][H * s:H * (s + 1)])
                im["p2"] = r2[2 * b + s]["pc"]
                im["cT"] = c_cols(c[b], c_ctx)
                ims.append(im)
        res = run_bass_kernel_spmd(_PROGS[key].nc, ims, core_ids=list(range(n_cores))).results
    out = np.zeros_like(x)
    for b in range(B):
        for s in range(2):
            out[b, H * s:H * (s + 1)] = res[2 * b + s]["y"]
    return out
```

```python
import numpy as np
import concourse.bass as bass
import concourse.mybir as mybir
from concourse.bass_utils import run_bass_kernel_spmd

F32 = mybir.dt.float32
BF16 = mybir.dt.bfloat16
I32 = mybir.dt.int32
U32 = mybir.dt.uint32
AF = mybir.ActivationFunctionType
ALU = mybir.AluOpType
AX = mybir.AxisListType


class T:
    def __init__(self, P, ap, name):
        self.P = P
        self.ap = ap
        self.name = name
        self.w = None
        self.r = {}
        self.dsem = None
        self.dcnt = 0
        self.psum = False

    def __getitem__(self, idx):
        return V(self, self.ap[idx])

    def sub(self, idx, tag):
        t = T(self.P, self.ap[idx], self.name + "_" + str(tag))
        t.psum = self.psum
        return t


class V:
    def __init__(self, t, ap):
        self.t = t
        self.ap = ap

    def __getitem__(self, idx):
        return V(self.t, self.ap[idx])


class Prog:
    ENG = ("pe", "dve", "act", "pool", "sp")

    def __init__(self, strict=True, num_devices=None):
        if num_devices is None:
            self.nc = bass.Bass("TRN2", target_bir_lowering=False)
        else:
            self.nc = bass.Bass("TRN2", target_bir_lowering=False, num_devices=num_devices)
        self.pfx = ""
        self.fused = num_devices is not None
        self._banks = None
        self._ps_i = 0
        self._sem_pool = []
        self._stage_tiles = None
        self._nsem = 0
        nc = self.nc
        self.e = {"pe": nc.tensor, "dve": nc.vector, "act": nc.scalar, "pool": nc.gpsimd, "sp": nc.sync}
        self.sem = {}
        self.cnt = {}
        self.seen = {k: {} for k in self.ENG}
        self.strict = strict
        self._ctx = []
        self._sctx = []
        for k in ("pe", "dve", "act", "pool"):
            self.sem[k] = self._senter(nc.semaphore("sem_" + k))
            self.cnt[k] = 0
        self.n_ins = 0
        self.out_tiles = []
        self._dcnt = {}

    def _enter(self, cm):
        v = cm.__enter__()
        self._ctx.append(cm)
        return v

    def _senter(self, cm):
        v = cm.__enter__()
        self._sctx.append(cm)
        return v

    def sb(self, name, shape, dt=F32):
        name = self.pfx + name
        h = self._enter(self.nc.sbuf_tensor(name, list(shape), dt))
        return T(self, h[:], name)

    def ps(self, name, shape, dt=F32):
        if self.fused:
            if self._banks is None:
                self._banks = []
                for i in range(8):
                    h = self._senter(self.nc.psum_tensor("bank%d" % i, [128, 512], F32))
                    t = T(self, h[:], "bank%d" % i)
                    t.psum = True
                    self._banks.append(t)
            t = self._banks[self._ps_i % 8]
            self._ps_i += 1
            return t
        h = self._enter(self.nc.psum_tensor(name, list(shape), dt))
        t = T(self, h[:], name)
        t.psum = True
        return t

    def dram(self, name, shape, dt=F32, kind="Internal", shared=False, persist=False):
        name = self.pfx + name
        if shared:
            h = self.nc.dram_tensor(name, list(shape), dt, kind=kind, addr_space="Shared")
        else:
            h = self.nc.dram_tensor(name, list(shape), dt, kind=kind)
        t = T(self, h.ap(), name)
        t.persist = persist or kind == "ExternalOutput"
        if kind == "ExternalOutput":
            self.out_tiles.append(t)
        return t

    def _dsem(self, t):
        if t.dsem is None:
            if self._sem_pool and not getattr(t, "persist", False):
                t.dsem, t.dcnt, t.dkey = self._sem_pool.pop()
            else:
                self._nsem += 1
                t.dkey = ("d", self._nsem)
                t.dsem = self._senter(self.nc.semaphore("ds%d" % self._nsem))
                t.dcnt = 0
                self.sem[t.dkey] = t.dsem
            if self._stage_tiles is not None and not getattr(t, "persist", False):
                self._stage_tiles.append(t)
        return t.dsem

    def stage_begin(self, pfx):
        self.pfx = pfx
        self._ps_i = 0
        self._stage_tiles = []
        self._stage_mark = self.mark()

    def stage_end(self, core_sync=True):
        self.barrier()
        self.release(self._stage_mark)
        for t in self._stage_tiles:
            self._sem_pool.append((t.dsem, t.dcnt, t.dkey))
            t.dsem = None
        self._stage_tiles = None
        if core_sync:
            self.nc.all_core_barrier()

    def _wait(self, eng, key, val, skip_self=False):
        if key == eng and (skip_self or not self.strict):
            return
        if self.seen[eng].get(key, 0) >= val:
            return
        self.seen[eng][key] = val
        self.e[eng].wait_ge(self.sem[key], val)

    def _deps(self, eng, reads, writes, acc=False):
        for v in reads:
            t = v.t
            if t.w is not None:
                self._wait(eng, *t.w)
            if t.psum:
                for k, c in t.r.items():
                    if k != eng:
                        self._wait(eng, k, c)
        for v in writes:
            t = v.t
            if t.w is not None:
                self._wait(eng, *t.w, skip_self=acc)
            for k, c in t.r.items():
                self._wait(eng, k, c)

    def _mark(self, key, val, reads, writes):
        for v in reads:
            t = v.t
            t.r[key] = max(t.r.get(key, 0), val)
        for v in writes:
            t = v.t
            t.w = (key, val)
            t.r = {}

    def op(self, eng, fn, reads, writes, acc=False):
        self._deps(eng, reads, writes, acc)
        ins = fn()
        self.cnt[eng] += 1
        ins.then_inc(self.sem[eng], 1)
        self._mark(eng, self.cnt[eng], reads, writes)
        self.n_ins += 1
        return ins

    def dma(self, q, out, in_, **kw):
        owner = out.t
        sem = self._dsem(owner)
        self._deps(q, [in_], [out])
        ins = self.e[q].dma_start(out=out.ap, in_=in_.ap, **kw)
        owner.dcnt += 16
        ins.then_inc(sem, 16)
        self._dcnt[owner.dkey] = owner.dcnt
        self._mark(owner.dkey, owner.dcnt, [in_], [out])
        self.n_ins += 1
        return ins

    def gather(self, out, table, idx):
        owner = out.t
        sem = self._dsem(owner)
        self._deps("pool", [table, idx], [out])
        ins = self.nc.gpsimd.indirect_dma_start(
            out=out.ap, out_offset=None, in_=table.ap,
            in_offset=bass.IndirectOffsetOnAxis(ap=idx.ap, axis=0))
        owner.dcnt += 16
        ins.then_inc(sem, 16)
        self._dcnt[owner.dkey] = owner.dcnt
        self._mark(owner.dkey, owner.dcnt, [table, idx], [out])
        self.n_ins += 1
        return ins

    def mm(self, out, lhsT, rhs, start=True, stop=True):
        nc = self.nc
        return self.op("pe", lambda: nc.tensor.matmul(out.ap, lhsT.ap, rhs.ap, start=start, stop=stop),
                       [lhsT, rhs], [out], acc=True)

    def tr(self, out, in_, ident):
        nc = self.nc
        return self.op("pe", lambda: nc.tensor.transpose(out.ap, in_.ap, ident.ap), [in_, ident], [out], acc=True)

    def act(self, out, in_, func, bias=None, scale=1.0, accum=None, eng="act"):
        nc = self.nc
        kw = {}
        rd = [in_]
        wr = [out]
        if bias is not None:
            if isinstance(bias, V):
                kw["bias"] = bias.ap
                rd.append(bias)
            else:
                kw["bias"] = bias
        if isinstance(scale, V):
            kw["scale"] = scale.ap
            rd.append(scale)
        else:
            kw["scale"] = scale
        if accum is not None:
            kw["accum_out"] = accum.ap
            wr.append(accum)
        return self.op("act", lambda: nc.scalar.activation(out=out.ap, in_=in_.ap, func=func, **kw), rd, wr)

    def tt(self, eng, out, a, b, op):
        e = self.e[eng]
        return self.op(eng, lambda: e.tensor_tensor(out=out.ap, in0=a.ap, in1=b.ap, op=op), [a, b], [out])

    def ts(self, eng, out, a, s1, op0, s2=None, op1=None):
        e = self.e[eng]
        rd = [a]
        a1 = s1.ap if isinstance(s1, V) else s1
        a2 = s2.ap if isinstance(s2, V) else s2
        if isinstance(s1, V):
            rd.append(s1)
        if isinstance(s2, V):
            rd.append(s2)
        if op1 is None:
            return self.op(eng, lambda: e.tensor_scalar(out=out.ap, in0=a.ap, scalar1=a1, scalar2=None, op0=op0), rd, [out])
        return self.op(eng, lambda: e.tensor_scalar(out=out.ap, in0=a.ap, scalar1=a1, scalar2=a2, op0=op0, op1=op1), rd, [out])

    def stt(self, eng, out, a, s, b, op0, op1, accum=None):
        e = self.e[eng]
        rd = [a, b]
        sa = s.ap if isinstance(s, V) else s
        if isinstance(s, V):
            rd.append(s)
        wr = [out]
        kw = {}
        if accum is not None:
            kw["accum_out"] = accum.ap
            wr.append(accum)
        return self.op(eng, lambda: e.scalar_tensor_tensor(out=out.ap, in0=a.ap, scalar=sa, in1=b.ap, op0=op0, op1=op1, **kw), rd, wr)

    def copy(self, eng, out, in_):
        if eng == "act":
            nc = self.nc
            return self.op("act", lambda: nc.scalar.copy(out=out.ap, in_=in_.ap), [in_], [out])
        e = self.e[eng]
        return self.op(eng, lambda: e.tensor_copy(out=out.ap, in_=in_.ap), [in_], [out])

    def memset(self, eng, out, val):
        e = self.e[eng]
        return self.op(eng, lambda: e.memset(out.ap, val), [], [out])

    def rsqrt(self, out, in_, scale, eps):
        nc = self.nc
        self.ts("dve", out, in_, scale, ALU.mult, eps, ALU.add)
        self.act(out, out, AF.Sqrt)
        self.op("dve", lambda: nc.vector.reciprocal(out=out.ap, in_=out.ap), [out], [out])

    def mark(self):
        return len(self._ctx)

    def barrier(self):
        for eng in self.ENG:
            for k in list(self.sem.keys()):
                if isinstance(k, tuple):
                    c = self._dcnt.get(k, 0)
                else:
                    c = self.cnt[k]
                if c > 0:
                    self._wait(eng, k, c)

    def release(self, mark):
        while len(self._ctx) > mark:
            cm = self._ctx.pop()
            cm.__exit__(None, None, None)

    def finish(self):
        for t in self.out_tiles:
            if t.w is not None:
                self._wait("sp", *t.w)
        for cm in reversed(self._ctx):
            cm.__exit__(None, None, None)
        for cm in reversed(self._sctx):
            cm.__exit__(None, None, None)
        return self.nc


D = 1024
NEXP = 16384


def build_post(NT, n_lat, n_part, final, P=None, hooks=None):
    standalone = P is None
    if standalone:
        P = Prog()
    nc = P.nc
    N = NT * 128
    hooks = hooks or {}
    if "x_src" not in hooks:
        x = P.dram("x", [N, D], F32, kind="ExternalInput")
        hooks["x_src"] = lambda it: x[it * 128:(it + 1) * 128, :]
    if "part_src" not in hooks:
        parts = [P.dram("p%d" % i, [N, D], F32, kind="ExternalInput") for i in range(n_part)]
        hooks["part_src"] = lambda i, it: parts[i][it * 128:(it + 1) * 128, :]
    if "cT" in hooks:
        cT = hooks["cT"]
    else:
        cT = P.dram("cT", [128, 8, 2], F32, kind="ExternalInput")
    modw = P.dram("modw", [D, 4096], F32, kind="ExternalInput")
    modb2 = P.dram("modb2", [2, 4096], F32, kind="ExternalInput")
    sel = P.dram("sel", [2, 2, 128], F32, kind="ExternalInput")
    n2 = P.dram("n2", [2, D], F32, kind="ExternalInput")
    nf = P.dram("nf", [2, D], F32, kind="ExternalInput")
    pq = P.dram("pq", [D, 2048], F32, kind="ExternalInput")
    skT = P.dram("skT", [128, 8, 2, 128], F32, kind="ExternalInput")
    pu = P.dram("pu", [NEXP, D], F32, kind="ExternalInput")
    pv = P.dram("pv", [NEXP, D], F32, kind="ExternalInput")
    identd = P.dram("ident", [128, 128], F32, kind="ExternalInput")
    zseld = P.dram("zsel", [128, 255], F32, kind="ExternalInput")
    iotad = P.dram("iota16", [128, 16], F32, kind="ExternalInput")
    if "y_dst" not in hooks:
        y = P.dram("y", [N, D], F32, kind="ExternalOutput")
        hooks["y_dst"] = lambda it: y[it * 128:(it + 1) * 128, :]
    yT_dst = hooks.get("yT_dst")

    ident = P.sb("ident_sb", [128, 128])
    identb = P.sb("identb", [128, 128], BF16)
    zsel = P.sb("zsel_sb", [128, 255])
    iota16 = P.sb("iota_sb", [128, 16])
    sk_sb = P.sb("sk_sb", [128, 8, 2, 128])
    sel_sb = P.sb("sel_sb", [2, 2, 128])
    modb_sb = P.sb("modb_sb", [2, 4096])
    n2_sb = P.sb("n2_sb", [2, D])
    nf_sb = P.sb("nf_sb", [2, D])
    c_sb = P.sb("c_sb", [128, 8, 2])
    sc_sb = P.sb("sc_sb", [128, 8, 2])
    rows = P.sb("rows", [2, 4096])
    P.dma("sp", ident[:], identd[:])
    P.dma("sp", zsel[:], zseld[:])
    P.dma("sp", iota16[:], iotad[:])
    P.dma("sp", sk_sb[:], skT[:])
    P.dma("sp", sel_sb[:], sel[:])
    P.dma("sp", modb_sb[:], modb2[:])
    P.dma("sp", n2_sb[:], n2[:])
    P.dma("sp", nf_sb[:], nf[:])
    P.dma("sp", c_sb[:], cT[:])
    P.copy("dve", identb[:], ident[:])
    P.act(sc_sb[:], c_sb[:], AF.Silu)

    psA = [P.ps("psA%d" % i, [128, 512]) for i in range(2)]
    psR = [P.ps("psR%d" % i, [128, 512]) for i in range(4)]
    psO = [P.ps("psO%d" % i, [128, 512]) for i in range(2)]

    wbuf = [P.sb("wbuf%d" % i, [128, 8, 256]) for i in range(2)]
    modw_v = modw.ap.rearrange("(k p) n -> p k n", p=128)
    pq_v = pq.ap.rearrange("(k p) n -> p k n", p=128)
    for cb in range(16):
        wb = wbuf[cb % 2]
        P.dma("sp", wb[:], V(modw, modw_v[:, :, cb * 256:(cb + 1) * 256]))
        pr = psA[cb % 2]
        for k in range(8):
            P.mm(V(pr, pr.ap[0:2, 0:256]), sc_sb[:, k, :], wb[:, k, :], start=(k == 0), stop=(k == 7))
        P.tt("dve", rows[:, cb * 256:(cb + 1) * 256], V(pr, pr.ap[0:2, 0:256]), modb_sb[:, cb * 256:(cb + 1) * 256], ALU.add)
    P.stt("dve", rows[:, 2048:3072], rows[:, 2048:3072], 1.0, n2_sb[:], ALU.add, ALU.mult)

    rep = [P.sb("rep%d" % i, [128, D]) for i in range(4)]
    nf_rep = P.sb("nf_rep", [128, D])

    def load_class(cls):
        for i in range(4):
            for hf in range(2):
                pr = psA[hf]
                P.mm(pr[:], sel_sb[:, cls, :], rows[:, i * 1024 + hf * 512: i * 1024 + hf * 512 + 512])
                P.copy("act", rep[i][:, hf * 512:(hf + 1) * 512], pr[:])

    if final:
        for hf in range(2):
            pr = psA[hf]
            P.mm(pr[:], sel_sb[:, 0, :], nf_sb[:, hf * 512:(hf + 1) * 512])
            P.copy("act", nf_rep[:, hf * 512:(hf + 1) * 512], pr[:])

    UVb = P.dram("UVb", [NEXP, 2 * D], BF16)
    mk_tab = P.mark()
    tst = [P.sb("tst%d" % i, [128, 4, D]) for i in range(2)]
    tsb = [P.sb("tsb%d" % i, [128, 4, D], BF16) for i in range(2)]
    uvv = UVb.ap.rearrange("(r p) (w d) -> p r w d", p=128, w=2)
    ci = 0
    for w_, tab in enumerate((pu, pv)):
        tv_ = tab.ap.rearrange("(r p) d -> p r d", p=128)
        for r4 in range(0, NEXP // 128, 4):
            a_, b_ = tst[ci % 2], tsb[ci % 2]
            P.dma("sp", a_[:], V(tab, tv_[:, r4:r4 + 4, :]))
            if ci % 2 == 0:
                P.copy("act", b_[:], a_[:])
            else:
                P.copy("dve", b_[:], a_[:])
            P.dma("sp", V(UVb, uvv[:, r4:r4 + 4, w_, :]), b_[:])
            ci += 1
    P.barrier()
    P.release(mk_tab)

    xt = P.sb("xt", [128, D])
    pt = [P.sb("pt%d" % i, [128, D]) for i in range(n_part)]
    x1 = P.sb("x1", [128, D])
    hn = P.sb("hn", [128, D])
    hnb = P.sb("hnb", [128, D], BF16)
    hnT = P.sb("hnT", [128, 8, 128])
    qT = P.sb("qT", [128, 16, 128])
    S4 = [P.sb("S4_%d" % g, [128, 4, 128]) for g in range(4)]
    scrA = P.sb("scrA", [128, 2048])
    scrB = P.sb("scrB", [128, 2048])
    tmpj = [scrA.sub((slice(None), slice(j * 128, (j + 1) * 128)), j) for j in range(16)]
    v16 = P.sb("v16", [128, 16, 16])
    i16 = P.sb("i16", [128, 16, 16], U32)
    v16j = [v16.sub((slice(None), j, slice(None)), j) for j in range(16)]
    i16j = [i16.sub((slice(None), j, slice(None)), j) for j in range(16)]
    candh = [scrB.sub((slice(None), slice(h * 256, (h + 1) * 256)), h) for h in range(8)]
    c16 = P.sb("c16", [128, 8, 16])
    ci16 = P.sb("ci16", [128, 8, 16], U32)
    c16h = [c16.sub((slice(None), h, slice(None)), h) for h in range(8)]
    ci16h = [ci16.sub((slice(None), h, slice(None)), h) for h in range(8)]
    hi_u = P.sb("hi_u", [128, 8, 16], U32)
    lo_u = P.sb("lo_u", [128, 8, 16], U32)
    hi_f = P.sb("hi_f", [128, 8, 16])
    lo_f = P.sb("lo_f", [128, 8, 16])
    i16f = P.sb("i16f", [128, 16, 16])
    e1 = P.sb("e1", [128, 8, 16])
    e2 = P.sb("e2", [128, 8, 16])
    ef = P.sb("ef", [128, 128])
    gate = P.sb("gate", [128, 8, 16])
    gmx = P.sb("gmx", [128, 8])
    eiT = P.sb("eiT", [128, 128], I32)
    gateT = P.sb("gateT", [128, 128])
    A = P.sb("A", [128, 128])
    Ag = P.sb("Ag", [128, 128])
    ss = P.sb("ss", [128, 1])
    rstd = P.sb("rstd", [128, 1])
    NB = 6
    UVg = [P.sb("UVg%d" % i, [128, 2 * D], BF16) for i in range(NB)]
    At = [P.sb("At%d" % i, [128, 128], BF16) for i in range(NB)]
    dsum = [P.sb("dsum%d" % i, [128, 2]) for i in range(NB)]
    gcol = [P.sb("gcol%d" % i, [128, 1]) for i in range(NB)]
    xo = P.sb("xo", [128, D])
    junk = xo

    cur_cls = None
    for it in range(NT):
        cls = 0 if it < n_lat else 1
        if cls != cur_cls:
            load_class(cls)
            cur_cls = cls
        g1r, sh2r, w2r, g2r = rep
        P.dma("sp", xt[:], hooks["x_src"](it))
        for i in range(n_part):
            P.dma("sp", pt[i][:], hooks["part_src"](i, it))
        for i in range(1, n_part):
            P.tt("pool", pt[0][:], pt[0][:], pt[i][:], ALU.add)
        P.tt("dve", x1[:], pt[0][:], g1r[:], ALU.mult)
        P.tt("dve", x1[:], x1[:], xt[:], ALU.add)
        P.act(junk[:], x1[:], AF.Square, accum=ss[:])
        P.rsqrt(rstd[:], ss[:], 1.0 / D, 1e-6)
        P.stt("dve", hn[:], x1[:], rstd[:, 0:1], w2r[:], ALU.mult, ALU.mult)
        P.tt("pool", hn[:], hn[:], sh2r[:], ALU.add)
        P.copy("act", hnb[:], hn[:])
        for g in range(2):
            pr = psA[g]
            for kk in range(4):
                k = g * 4 + kk
                P.tr(pr[:, kk * 128:(kk + 1) * 128], hn[:, k * 128:(k + 1) * 128], ident[:])
            P.copy("act" if g == 0 else "dve", V(hnT, hnT.ap[:, g * 4:(g + 1) * 4, :]),
                   V(pr, pr.ap.rearrange("p (a b) -> p a b", a=4)))
        for c8 in range(8):
            wb = wbuf[c8 % 2]
            P.dma("sp", wb[:], V(pq, pq_v[:, :, c8 * 256:(c8 + 1) * 256]))
            c4 = c8 // 2
            pr = psA[c4 % 2]
            for qq in range(2):
                pos = (c8 % 2) * 2 + qq
                for k in range(8):
                    P.mm(pr[:, pos * 128:(pos + 1) * 128], wb[:, k, qq * 128:(qq + 1) * 128], hnT[:, k, :],
                         start=(k == 0), stop=(k == 7))
            if c8 % 2 == 1:
                P.copy("act" if c4 % 2 == 0 else "dve", V(qT, qT.ap[:, c4 * 4:(c4 + 1) * 4, :]),
                       V(pr, pr.ap.rearrange("p (a b) -> p a b", a=4)))
        for g in range(4):
            pr = psA[g % 2]
            for jj in range(4):
                j = g * 4 + jj
                P.mm(pr[:, jj * 128:(jj + 1) * 128], qT[:, j, :], sk_sb[:, j // 2, j % 2, :])
            P.copy("act" if g % 2 == 0 else "dve", S4[g][:], V(pr, pr.ap.rearrange("p (a b) -> p a b", a=4)))
        Sj = [S4[j // 4][:, j % 4, :] for j in range(16)]
        for j in range(16):
            P.op("dve", (lambda j=j: nc.vector.max(out=v16j[j].ap[:, 0:8], in_=Sj[j].ap)), [Sj[j]], [v16j[j][:]])
        for j in range(16):
            P.op("dve", (lambda j=j: nc.vector.match_replace(out=tmpj[j].ap, in_to_replace=v16j[j].ap[:, 0:8],
                                                             in_values=Sj[j].ap, imm_value=-1e30)),
                 [Sj[j], v16j[j][:]], [tmpj[j][:]])
        for j in range(16):
            P.op("dve", (lambda j=j: nc.vector.max(out=v16j[j].ap[:, 8:16], in_=tmpj[j].ap)), [tmpj[j][:]], [v16j[j][:]])
        for j in range(16):
            P.op("dve", (lambda j=j: nc.vector.max_index(out=i16j[j].ap[:, 0:8], in_max=v16j[j].ap[:, 0:8],
                                                         in_values=Sj[j].ap)), [Sj[j], v16j[j][:]], [i16j[j][:]])
        for j in range(16):
            P.op("dve", (lambda j=j: nc.vector.max_index(out=i16j[j].ap[:, 8:16], in_max=v16j[j].ap[:, 8:16],
                                                         in_values=Sj[j].ap)), [Sj[j], v16j[j][:]], [i16j[j][:]])
        for h in range(8):
            a0 = v16j[2 * h].ap.unsqueeze(2).broadcast_to([128, 16, 16])
            a1 = v16j[2 * h + 1].ap.unsqueeze(1).broadcast_to([128, 16, 16])
            co = candh[h].ap.rearrange("p (a b) -> p a b", a=16)
            P.op("dve", (lambda co=co, a0=a0, a1=a1: nc.vector.tensor_tensor(out=co, in0=a0, in1=a1, op=ALU.add)),
                 [v16j[2 * h][:], v16j[2 * h + 1][:]], [candh[h][:]])
        tmp2 = [scrA.sub((slice(None), slice(h * 256, (h + 1) * 256)), "c%d" % h) for h in range(8)]
        t2dep = lambda h: [tmpj[2 * h][:], tmpj[2 * h + 1][:]]
        for h in range(8):
            P.op("dve", (lambda h=h: nc.vector.max(out=c16h[h].ap[:, 0:8], in_=candh[h].ap)), [candh[h][:]], [c16h[h][:]])
        for h in range(8):
            P.op("dve", (lambda h=h: nc.vector.match_replace(out=tmp2[h].ap, in_to_replace=c16h[h].ap[:, 0:8],
                                                             in_values=candh[h].ap, imm_value=-1e30)),
                 [candh[h][:], c16h[h][:]], t2dep(h))
        for h in range(8):
            P.op("dve", (lambda h=h: nc.vector.max(out=c16h[h].ap[:, 8:16], in_=tmp2[h].ap)), t2dep(h), [c16h[h][:]])
        for h in range(8):
            P.op("dve", (lambda h=h: nc.vector.max_index(out=ci16h[h].ap[:, 0:8], in_max=c16h[h].ap[:, 0:8],
                                                         in_values=candh[h].ap)), [candh[h][:], c16h[h][:]], [ci16h[h][:]])
        for h in range(8):
            P.op("dve", (lambda h=h: nc.vector.max_index(out=ci16h[h].ap[:, 8:16], in_max=c16h[h].ap[:, 8:16],
                                                         in_values=candh[h].ap)), [candh[h][:], c16h[h][:]], [ci16h[h][:]])
        allci = [t[:] for t in ci16h]
        allc = [t[:] for t in c16h]
        alli = [t[:] for t in i16j]
        P.op("dve", lambda: nc.vector.tensor_single_scalar(out=hi_u.ap, in_=ci16.ap, scalar=4, op=ALU.logical_shift_right),
             allci, [hi_u[:]])
        P.op("dve", lambda: nc.vector.tensor_single_scalar(out=lo_u.ap, in_=ci16.ap, scalar=15, op=ALU.bitwise_and),
             allci, [lo_u[:]])
        P.copy("dve", hi_f[:], hi_u[:])
        P.copy("dve", lo_f[:], lo_u[:])
        P.op("dve", lambda: nc.vector.tensor_copy(out=i16f.ap, in_=i16.ap), alli, [i16f[:]])
        i16f_v = i16f.ap.rearrange("p (h two) k -> p h two k", two=2)
        oh = scrA.ap.rearrange("p (h k i) -> p h k i", h=8, k=16)
        pr4 = scrB.ap.rearrange("p (h k i) -> p h k i", h=8, k=16)
        scrA_all = [t[:] for t in tmpj]
        scrB_all = [t[:] for t in candh]
        iota_b = iota16.ap.unsqueeze(1).unsqueeze(1).broadcast_to([128, 8, 16, 16])
        for (src_f, half, dst) in ((hi_f, 0, e1), (lo_f, 1, e2)):
            sb_ = src_f.ap.unsqueeze(3).broadcast_to([128, 8, 16, 16])
            P.op("dve", (lambda sb_=sb_: nc.vector.tensor_tensor(out=oh, in0=sb_, in1=iota_b, op=ALU.is_equal)),
                 [src_f[:], iota16[:]], scrA_all)
            ib = i16f_v[:, :, half, :].unsqueeze(2).broadcast_to([128, 8, 16, 16])
            P.op("dve", (lambda ib=ib: nc.vector.tensor_tensor(out=pr4, in0=oh, in1=ib, op=ALU.mult)),
                 scrA_all + [i16f[:]], scrB_all)
            P.op("dve", (lambda dst=dst: nc.vector.tensor_reduce(out=dst.ap, in_=pr4, axis=AX.X, op=ALU.add)),
                 scrB_all, [dst[:]])
        ef3 = ef.ap.rearrange("p (h k) -> p h k", h=8)
        P.op("dve", lambda: nc.vector.scalar_tensor_tensor(out=ef3, in0=e1.ap, scalar=128.0, in1=e2.ap,
                                                           op0=ALU.mult, op1=ALU.add), [e1[:], e2[:]], [ef[:]])
        P.op("dve", lambda: nc.vector.tensor_copy(out=gmx.ap, in_=c16.ap[:, :, 0]), allc, [gmx[:]])
        P.op("dve", lambda: nc.vector.tensor_tensor(out=gate.ap, in0=c16.ap, in1=gmx.ap.unsqueeze(2).broadcast_to([128, 8, 16]),
                                                    op=ALU.subtract), allc + [gmx[:]], [gate[:]])
        P.act(gate[:], gate[:], AF.Exp)
        P.op("dve", lambda: nc.vector.tensor_reduce(out=gmx.ap, in_=gate.ap, axis=AX.X, op=ALU.add), [gate[:]], [gmx[:]])
        P.op("dve", lambda: nc.vector.reciprocal(out=gmx.ap, in_=gmx.ap), [gmx[:]], [gmx[:]])
        P.op("dve", lambda: nc.vector.tensor_tensor(out=gate.ap, in0=gate.ap, in1=gmx.ap.unsqueeze(2).broadcast_to([128, 8, 16]),
                                                    op=ALU.mult), [gate[:], gmx[:]], [gate[:]])
        pr = psA[0]
        P.tr(pr[:, 0:128], ef[:], ident[:])
        P.op("dve", lambda: nc.vector.tensor_copy(out=eiT.ap, in_=pr.ap[:, 0:128]), [pr[:]], [eiT[:]])
        pr2 = psA[1]
        P.tr(pr2[:, 0:128], V(gate, gate.ap.rearrange("p h k -> p (h k)")), ident[:])
        P.copy("act", gateT[:], pr2[:, 0:128])
        def s_gather(t):
            b = t % NB
            P.gather(UVg[b][:], UVb[:], eiT[:, t:t + 1])
            rb = (t % 2) * 2
            lt = V(identb, identb.ap[:, t:t + 1].broadcast_to([128, 128]))
            for hf in range(2):
                P.mm(psR[rb + hf][:], lt, hnb[:, hf * 512:(hf + 1) * 512])

        def s_dot(t):
            b = t % NB
            rb = (t % 2) * 2
            for hf in range(2):
                P.stt("dve", junk[:, hf * 512:(hf + 1) * 512], UVg[b][:, hf * 512:(hf + 1) * 512], 1.0, psR[rb + hf][:],
                      ALU.mult, ALU.mult, accum=dsum[b][:, hf:hf + 1])
            P.act(gcol[b][:], dsum[b][:, 0:1], AF.Gelu, bias=dsum[b][:, 1:2])

        def s_mask(t):
            b = t % NB
            P.ts("dve", At[b][:], zsel[:, 127 - t:255 - t], gcol[b][:, 0:1], ALU.mult, gateT[:, t:t + 1], ALU.mult)

        def s_out(t):
            b = t % NB
            for hf in range(2):
                P.mm(psO[hf][:], At[b][:], UVg[b][:, D + hf * 512:D + (hf + 1) * 512], start=(t == 0), stop=(t == 127))
        for i in range(128 + 3):
            if i < 128:
                s_gather(i)
            if 0 <= i - 1 < 128:
                s_dot(i - 1)
            if 0 <= i - 2 < 128:
                s_mask(i - 2)
            if 0 <= i - 3 < 128:
                s_out(i - 3)
        for hf in range(2):
            sl = slice(hf * 512, (hf + 1) * 512)
            P.tt("dve", xo[:, sl], psO[hf][:], g2r[:, sl], ALU.mult)
        P.tt("pool", xo[:], xo[:], x1[:], ALU.add)
        if final:
            P.act(hn[:], xo[:], AF.Square, accum=ss[:])
            P.rsqrt(rstd[:], ss[:], 1.0 / D, 1e-6)
            P.stt("dve", xo[:], xo[:], rstd[:, 0:1], nf_rep[:], ALU.mult, ALU.mult)
        P.dma("sp", hooks["y_dst"](it), xo[:])
        if yT_dst is not None:
            for g in range(2):
                pr = psA[g]
                for kk in range(4):
                    k = g * 4 + kk
                    P.tr(pr[:, kk * 128:(kk + 1) * 128], xo[:, k * 128:(k + 1) * 128], ident[:])
                P.copy("act" if g == 0 else "dve", V(hnT, hnT.ap[:, g * 4:(g + 1) * 4, :]),
                       V(pr, pr.ap.rearrange("p (a b) -> p a b", a=4)))
            P.dma("sp", yT_dst(it), hnT[:])
    if standalone:
        P.finish()
    return P


def post_consts(mod_w, mod_b, norm2, norm_f, pq, sk1, sk2, pu, pv):
    f = np.float32
    sel = np.zeros((2, 2, 128), f)
    sel[0, 0, :] = 1
    sel[1, 1, :] = 1
    zsel = np.zeros((128, 255), f)
    zsel[:, 127] = 1
    skT = np.ascontiguousarray(np.stack([sk1, sk2], 0).transpose(3, 1, 0, 2))
    return {
        "modw": np.ascontiguousarray(mod_w[:, 2048:6144]),
        "modb2": np.ascontiguousarray(np.tile(mod_b[None, 2048:6144], (2, 1))),
        "sel": sel,
        "n2": np.ascontiguousarray(np.tile(norm2[None], (2, 1))),
        "nf": np.ascontiguousarray(np.tile(norm_f[None], (2, 1))),
        "pq": pq, "skT": skT, "pu": pu, "pv": pv,
        "ident": np.eye(128, dtype=f), "zsel": zsel,
        "iota16": np.ascontiguousarray(np.tile(np.arange(16, dtype=f)[None], (128, 1))),
    }


def c_cols(c_b, c_ctx):
    cc = np.stack([c_b, c_ctx], -1).astype(np.float32)
    return np.ascontiguousarray(cc.reshape(8, 128, 2).transpose(1, 0, 2))


def build_l1(S=8192, CL=256, CONV_T=4096, upto=9, P=None, hooks=None):
    standalone = P is None
    if standalone:
        P = Prog()
    hooks = hooks or {}
    nc = P.nc
    NQB = S // 512
    NKT = (S + CL) // 128
    NCB = CONV_T // 256
    XC = CONV_T + 30
    di = lambda n, s, dt=F32: P.dram(n, s, dt, kind="ExternalInput")
    if "h_src" not in hooks:
        xT = di("xT", [D, S]); ctxT = di("ctxT", [D, CL]); xcT = di("xcT", [D, XC])
        srcs = {"lat": xT, "ctx": ctxT, "conv": xcT}
        hooks["h_src"] = lambda kind, c0, w: V(srcs[kind], srcs[kind].ap.rearrange("(k p) t -> p k t", p=128)[:, :, c0:c0 + w])
    edge = di("edge", [128, 2])
    cT = hooks["cT"] if "cT" in hooks else di("cT", [128, 8, 2])
    modw = di("modw", [D, 2048])
    modbc = di("modbc", [128, 16]); n1c = di("n1c", [128, 8])
    wq = di("wq", [D, 384]); wk = di("wk", [D, 128]); wv = di("wv", [D, 128]); wu = di("wu", [D, 512])
    gq = di("gq", [128, 1]); gk = di("gk", [128, 1])
    ropeC = di("ropeC", [128, S]); ropeS = di("ropeS", [128, S]); Rm = di("Rm", [128, 128])
    bones = di("bones", [128, 128]); onesD = di("onesD", [128, 128]); ones256 = di("ones256", [128, 128])
    dww = di("dww", [128, 2, 31]); dwb = di("dwb", [128, 2]); cng = di("cng", [128, 2]); cnb = di("cnb", [128, 2])
    woa = di("woa", [64, 6, D]); woc = di("woc", [128, 2, D]); shiftm = di("shiftm", [128, 64])
    if "pa_dst" not in hooks:
        pa = P.dram("pa", [S, D], F32, kind="ExternalOutput")
        pc = P.dram("pc", [CONV_T, D], F32, kind="ExternalOutput")
        hooks["pa_dst"] = lambda tile: pa[tile * 128:(tile + 1) * 128, :]
        hooks["pc_dst"] = lambda tile: pc[tile * 128:(tile + 1) * 128, :]

    ps = [P.ps("ps%d" % i, [128, 512]) for i in range(8)]
    def ld(name, src, shape, dt=F32):
        t = P.sb(name, shape, dt)
        P.dma("sp", t[:], src[:])
        return t
    edge_s = ld("edge_s", edge, [128, 2]); c_sb = ld("c_sb", cT, [128, 8, 2]); modb_s = ld("modb_s", modbc, [128, 16])
    n1_s = ld("n1_s", n1c, [128, 8]); gq_s = ld("gq_s", gq, [128, 1]); gk_s = ld("gk_s", gk, [128, 1])
    Rm_s = ld("Rm_s", Rm, [128, 128]); bones_s = ld("bones_s", bones, [128, 128]); onesD_s = ld("onesD_s", onesD, [128, 128])
    ones256_s = ld("ones256_s", ones256, [128, 128]); dww_s = ld("dww_s", dww, [128, 2, 31]); dwb_s = ld("dwb_s", dwb, [128, 2])
    cng_s = ld("cng_s", cng, [128, 2]); cnb_s = ld("cnb_s", cnb, [128, 2]); shift_s = ld("shift_s", shiftm, [128, 64])
    sc_sb = P.sb("sc_sb", [128, 8, 2])
    P.act(sc_sb[:], c_sb[:], AF.Silu)
    xblk = P.sb("xblk", [128, 8, 512])
    stage = xblk
    def ldw(name, src, ncol):
        t = P.sb(name, [128, 8, ncol], BF16)
        P.dma("sp", stage[:, :, 0:ncol], V(src, src.ap.rearrange("(k p) n -> p k n", p=128)))
        P.copy("dve", t[:], stage[:, :, 0:ncol])
        return t
    wq_s = ldw("wq_s", wq, 384); wk_s = ldw("wk_s", wk, 128); wv_s = ldw("wv_s", wv, 128); wu_s = ldw("wu_s", wu, 512)
    woa_s = P.sb("woa_s", [64, 6, D], BF16)
    woc_s = P.sb("woc_s", [128, 2, D], BF16)
    for hh in range(3):
        st_v = V(stage, stage.ap[0:64].rearrange("p k n -> p (k n)")[:, 0:2048].rearrange("p (a n) -> p a n", a=2))
        P.dma("sp", st_v, woa[:, hh * 2:(hh + 1) * 2, :])
        P.copy("dve", woa_s[:, hh * 2:(hh + 1) * 2, :], st_v)
    st_v = V(stage, stage.ap.rearrange("p k n -> p (k n)")[:, 0:2048].rearrange("p (a n) -> p a n", a=2))
    P.dma("sp", st_v, woc[:])
    P.copy("dve", woc_s[:], st_v)
    modc = P.sb("modc", [128, 16, 2])
    modw_v = modw.ap.rearrange("(k p) n -> p k n", p=128)
    for c4 in range(4):
        wb = xblk
        P.dma("sp", wb[:], V(modw, modw_v[:, :, c4 * 512:(c4 + 1) * 512]))
        for q4 in range(4):
            cc = c4 * 4 + q4
            pr = ps[cc % 2]
            for k in range(8):
                P.mm(pr[:, 0:2], wb[:, k, q4 * 128:(q4 + 1) * 128], sc_sb[:, k, :], start=(k == 0), stop=(k == 7))
            P.ts("dve", modc[:, cc, :], pr[:, 0:2], modb_s[:, cc:cc + 1], ALU.add)
    wmod = P.sb("wmod", [128, 8, 2])
    P.op("dve", lambda: nc.vector.scalar_tensor_tensor(out=wmod.ap, in0=modc.ap[:, 8:16, :], scalar=1.0,
                                                       in1=n1_s.ap.unsqueeze(2).broadcast_to([128, 8, 2]),
                                                       op0=ALU.add, op1=ALU.mult), [modc[:], n1_s[:]], [wmod[:]])
    if upto < 1:
        P.finish()
        return P
    qT_r = P.sb("qT_r", [128, 3, S], BF16)
    kT_r = P.sb("kT_r", [128, S + CL], BF16)
    Va = [P.sb("Va%d" % i, [128, NKT, 128], BF16) for i in range(2)]
    for i in range(2):
        P.memset("pool", Va[i][:, :, 64:128], 1.0)
    hT = P.sb("hT", [128, 8, 512], BF16)
    sqb = [P.sb("sqb%d" % i, [128, 512]) for i in range(2)]
    rstd = P.sb("rstd", [128, 512])
    tmpf = [P.sb("tmpf%d" % i, [128, 512]) for i in range(2)]
    rC = P.sb("rC", [128, 512]); rS = P.sb("rS", [128, 512])
    raw = P.sb("raw", [128, 512]); qn = P.sb("qn", [128, 512]); r2 = P.sb("r2", [128, 512])

    def hblock(src, c0, w, cls):
        pieces = hooks["h_src"](src, c0, w)
        if not isinstance(pieces, list):
            pieces = [(0, w, pieces)]
        for (d0, dw_, sv) in pieces:
            P.dma("sp", xblk[:, :, d0:d0 + dw_], sv)
        pr = ps[0]
        for k in range(8):
            sq = sqb[k % 2]
            P.act(sq[:, 0:w], xblk[:, k, 0:w], AF.Square)
            P.mm(pr[:, 0:w], onesD_s[:], sq[:, 0:w], start=(k == 0), stop=(k == 7))
        P.rsqrt(rstd[:, 0:w], pr[:, 0:w], 1.0 / D, 1e-6)
        for k in range(8):
            tf = tmpf[k % 2]
            P.tt("dve", tf[:, 0:w], xblk[:, k, 0:w], rstd[:, 0:w], ALU.mult)
            P.ts("pool", hT[:, k, 0:w], tf[:, 0:w], wmod[:, k, cls:cls + 1], ALU.mult, modc[:, k, cls:cls + 1], ALU.add)

    def headnorm(pr, w, g_s, dest, rope_c0):
        P.copy("act", raw[:, 0:w], pr[:, 0:w])
        P.act(r2[:, 0:w], raw[:, 0:w], AF.Square)
        p2 = ps[2]
        P.mm(p2[:, 0:w], bones_s[:], r2[:, 0:w])
        P.rsqrt(r2[:, 0:w], p2[:, 0:w], 1.0 / 64, 1e-6)
        if rope_c0 is None:
            P.stt("dve", dest, raw[:, 0:w], g_s[:, 0:1], r2[:, 0:w], ALU.mult, ALU.mult)
            return
        P.stt("dve", qn[:, 0:w], raw[:, 0:w], g_s[:, 0:1], r2[:, 0:w], ALU.mult, ALU.mult)
        p3 = ps[3]
        P.mm(p3[:, 0:w], Rm_s[:], qn[:, 0:w])
        P.tt("dve", r2[:, 0:w], p3[:, 0:w], rS[:, 0:w], ALU.mult)
        P.tt("pool", qn[:, 0:w], qn[:, 0:w], rC[:, 0:w], ALU.mult)
        P.tt("pool", dest, qn[:, 0:w], r2[:, 0:w], ALU.add)

    def kv_proj(w, tok0, rope_c0):
        pr = ps[1]
        for k in range(8):
            P.mm(pr[:, 0:w], wk_s[:, k, :], hT[:, k, 0:w], start=(k == 0), stop=(k == 7))
        headnorm(pr, w, gk_s, kT_r[:, tok0:tok0 + w], rope_c0)
        for tt_ in range(w // 128):
            pv_ = ps[4 + tt_ % 2]
            for k in range(8):
                P.mm(pv_[:, 0:128], hT[:, k, tt_ * 128:(tt_ + 1) * 128], wv_s[:, k, :], start=(k == 0), stop=(k == 7))
            kt = tok0 // 128 + tt_
            P.copy("act", Va[0][:, kt, 0:64], pv_[:, 0:64])
            P.copy("dve", Va[1][:, kt, 0:64], pv_[:, 64:128])

    import os
    dbg = int(os.environ.get("L1DBG", "9"))
    for qb in range(NQB):
        c0 = qb * 512
        hblock("lat", c0, 512, 0)
        if dbg < 1:
            continue
        P.dma("sp", rC[:], ropeC[:, c0:c0 + 512])
        P.dma("sp", rS[:], ropeS[:, c0:c0 + 512])
        for ti in range(3):
            pr = ps[1]
            for k in range(8):
                P.mm(pr[:], wq_s[:, k, ti * 128:(ti + 1) * 128], hT[:, k, :], start=(k == 0), stop=(k == 7))
            if dbg >= 2:
                headnorm(pr, 512, gq_s, qT_r[:, ti, c0:c0 + 512], c0)
        if dbg >= 3:
            kv_proj(512, c0, c0)
    if dbg >= 4:
        hblock("ctx", 0, CL, 1)
        kv_proj(CL, S, None)

    if upto < 2:
        P.finish()
        return P
    gl = P.sb("gl", [128, 2, 286]); sg = P.sb("sg", [128, 286])
    acc = P.sb("acc", [128, 2, 256]); dd = P.sb("dd", [128, 2, 256]); sqd = P.sb("sqd", [128, 2, 256])
    cact = P.sb("cact", [128, 2, 256], BF16)
    ostg = [P.sb("ostg%d" % i, [128, D]) for i in range(2)]
    accs = [acc.sub((slice(None), c, slice(None)), c) for c in range(2)]
    gls = [gl.sub((slice(None), c, slice(None)), c) for c in range(2)]
    for j in range(NCB):
        hblock("conv", 256 * j, 286, 0)
        for c in range(2):
            pv_ = ps[1]
            pg_ = ps[2]
            for k in range(8):
                P.mm(pv_[:, 0:286], wu_s[:, k, c * 128:(c + 1) * 128], hT[:, k, 0:286], start=(k == 0), stop=(k == 7))
            for k in range(8):
                P.mm(pg_[:, 0:286], wu_s[:, k, 256 + c * 128:256 + (c + 1) * 128], hT[:, k, 0:286], start=(k == 0), stop=(k == 7))
            P.act(sg[:], pg_[:, 0:286], AF.Sigmoid)
            P.tt("dve", gls[c][:], pv_[:, 0:286], sg[:], ALU.mult)
            if j == 0:
                P.ts("dve", gls[c][:, 0:15], gls[c][:, 0:15], edge_s[:, 0:1], ALU.mult)
            if j == NCB - 1:
                P.ts("dve", gls[c][:, 271:286], gls[c][:, 271:286], edge_s[:, 1:2], ALU.mult)
        for c in range(2):
            P.ts("dve", accs[c][:], gls[c][:, 0:256], dww_s[:, c, 0:1], ALU.mult, dwb_s[:, c:c + 1], ALU.add)
        for jj in range(1, 31):
            for c in range(2):
                P.stt("dve", accs[c][:], gls[c][:, jj:jj + 256], dww_s[:, c, jj:jj + 1], accs[c][:], ALU.mult, ALU.add)
        pm = ps[3]
        for c in range(2):
            P.mm(pm[:, 0:256], ones256_s[:], accs[c][:], start=(c == 0), stop=(c == 1))
        for c in range(2):
            P.tt("dve", dd[:, c, :], accs[c][:], pm[:, 0:256], ALU.subtract)
        P.act(sqd[:], dd[:], AF.Square)
        pvv = ps[4]
        for c in range(2):
            P.mm(pvv[:, 0:256], ones256_s[:], sqd[:, c, :], start=(c == 0), stop=(c == 1))
        P.rsqrt(rstd[:, 0:256], pvv[:, 0:256], 1.0, 1e-5)
        for c in range(2):
            P.stt("dve", dd[:, c, :], dd[:, c, :], cng_s[:, c:c + 1], rstd[:, 0:256], ALU.mult, ALU.mult)
            P.act(cact[:, c, :], dd[:, c, :], AF.Silu, bias=cnb_s[:, c:c + 1])
        for tt_ in range(2):
            og = ostg[tt_ % 2]
            for ch in range(2):
                pw = ps[5 + ch]
                for c in range(2):
                    P.mm(pw[:], cact[:, c, tt_ * 128:(tt_ + 1) * 128], woc_s[:, c, ch * 512:(ch + 1) * 512], start=(c == 0), stop=(c == 1))
                P.copy("act" if ch == 0 else "dve", og[:, ch * 512:(ch + 1) * 512], pw[:])
            P.dma("sp", hooks["pc_dst"](2 * j + tt_), og[:])

    if upto < 3:
        P.finish()
        return P
    Pt = [P.sb("Pt%d" % i, [128, 512], BF16) for i in range(3)]
    Osb = raw
    rden = r2
    oT = [P.sb("oT%d" % j, [64, 512], BF16) for j in range(6)]
    for qb in range(NQB):
        c0 = qb * 512
        for j in range(6):
            half = j // 3
            ti = j % 3
            pl = slice(half * 64, half * 64 + 64)
            pO = ps[2]

            def s_mm(kt):
                P.mm(ps[kt % 2][:], kT_r[pl, kt * 128:(kt + 1) * 128], qT_r[pl, ti, c0:c0 + 512])

            def s_exp_pv(kt):
                pt_ = Pt[kt % 3]
                P.act(pt_[:], ps[kt % 2][:], AF.Exp, scale=0.125)
                P.mm(pO[:], Va[half][:, kt, :], pt_[:], start=(kt == 0), stop=(kt == NKT - 1))
            s_mm(0)
            for kt in range(NKT):
                if kt + 1 < NKT:
                    s_mm(kt + 1)
                s_exp_pv(kt)
            P.copy("dve", Osb[:], pO[:])
            pD = ps[3]
            P.mm(pD[0:64, :], shift_s[:], Osb[:])
            P.op("dve", lambda pD=pD: nc.vector.reciprocal(out=rden.ap[0:64, :], in_=pD.ap[0:64, :]), [pD[:]], [rden[:]])
            P.tt("dve", oT[j][:], Osb[0:64, :], rden[0:64, :], ALU.mult)
        for tt_ in range(4):
            og = ostg[tt_ % 2]
            for ch in range(2):
                pw = ps[5 + ch]
                for j in range(6):
                    P.mm(pw[:], oT[j][:, tt_ * 128:(tt_ + 1) * 128], woa_s[:, j, ch * 512:(ch + 1) * 512], start=(j == 0), stop=(j == 5))
                P.copy("act" if ch == 0 else "dve", og[:, ch * 512:(ch + 1) * 512], pw[:])
            P.dma("sp", hooks["pa_dst"](qb * 4 + tt_), og[:])
    if standalone:
        P.finish()
    return P


def rope_tables(S):
    f = np.float32
    t = np.arange(S)
    row = (t // 64).astype(f)
    col = (t % 64).astype(f)
    inv = (f(10000.0) ** (-np.arange(0, 32, 2, dtype=f) / f(32))).astype(f)
    ang = np.stack([row[:, None] * inv, col[:, None] * inv], 1)
    cs, sn = np.cos(ang).astype(f), np.sin(ang).astype(f)
    C = np.zeros((64, S), f)
    Sn = np.zeros((64, S), f)
    for ax in range(2):
        for hf in range(2):
            C[ax * 32 + hf * 16: ax * 32 + hf * 16 + 16] = cs[:, ax, :].T
            Sn[ax * 32 + hf * 16: ax * 32 + hf * 16 + 16] = sn[:, ax, :].T
    Rm = np.zeros((128, 128), f)
    for m in range(128):
        if (m % 32) < 16:
            Rm[m + 16, m] = -1.0
        else:
            Rm[m - 16, m] = 1.0
    return np.ascontiguousarray(np.tile(C, (2, 1))), np.ascontiguousarray(np.tile(Sn, (2, 1))), Rm


def colmat(v, nk):
    return np.ascontiguousarray(np.asarray(v, np.float32).reshape(nk, 128).T)


def l1_consts(mod_w, mod_b, norm1, w_in, q_norm, k_norm, dw_w, dw_b, cn_g, cn_b, w_out, s, S):
    f = np.float32
    C, Sn, Rm = rope_tables(S)
    bones = np.zeros((128, 128), f)
    bones[0:64, 0:64] = 1
    bones[64:128, 64:128] = 1
    shiftm = np.zeros((128, 64), f)
    for m in range(64):
        shiftm[m + 64, m] = 1
    qcols = []
    for i in range(3):
        for hh in (6 * s + i, 6 * s + 3 + i):
            qcols.append(w_in[:, hh * 64:(hh + 1) * 64])
    wq = np.ascontiguousarray(np.concatenate(qcols, 1))
    wk = np.ascontiguousarray(w_in[:, 768 + 128 * s: 768 + 128 * (s + 1)])
    wv = np.ascontiguousarray(w_in[:, 1024 + 128 * s: 1024 + 128 * (s + 1)])
    wu = np.ascontiguousarray(w_in[:, 1280:1792])
    woa = np.ascontiguousarray(w_out[384 * s:384 * (s + 1)].reshape(6, 64, D).transpose(1, 0, 2))
    woc = np.ascontiguousarray(w_out[768:1024].reshape(2, 128, D).transpose(1, 0, 2))
    dww = np.ascontiguousarray(dw_w.T.reshape(2, 128, 31).transpose(1, 0, 2))
    return {
        "modw": np.ascontiguousarray(mod_w[:, 0:2048]), "modbc": colmat(mod_b[0:2048], 16), "n1c": colmat(norm1, 8),
        "wq": wq, "wk": wk, "wv": wv, "wu": wu,
        "gq": np.ascontiguousarray(np.tile(q_norm, 2)[:, None].astype(f)),
        "gk": np.ascontiguousarray(np.tile(k_norm, 2)[:, None].astype(f)),
        "ropeC": C, "ropeS": Sn, "Rm": Rm, "bones": bones, "onesD": np.ones((128, 128), f),
        "ones256": np.full((128, 128), 1.0 / 256, f),
        "dww": dww, "dwb": colmat(dw_b, 2), "cng": colmat(cn_g, 2), "cnb": colmat(cn_b, 2),
        "woa": woa, "woc": woc, "shiftm": shiftm,
    }


def l1_core_inputs(xb, ctxb, c_b, c_ctx, s, conv_t):
    f = np.float32
    S = xb.shape[0]
    lo = conv_t * s - 15
    hi = conv_t * s + conv_t + 15
    xc = np.zeros((conv_t + 30, D), f)
    a, b_ = max(lo, 0), min(hi, S)
    xc[a - lo:b_ - lo] = xb[a:b_]
    edge = np.zeros((128, 2), f)
    edge[:, 0] = 1.0 if lo >= 0 else 0.0
    edge[:, 1] = 1.0 if hi <= S else 0.0
    return {"xT": np.ascontiguousarray(xb.T), "ctxT": np.ascontiguousarray(ctxb.T), "xcT": np.ascontiguousarray(xc.T),
            "edge": edge, "cT": c_cols(c_b, c_ctx)}


CH = 64
NEG_EXP_HALF = -0.6065306597126334


def build_l0(S=8192, CL=256, upto=9, dbg=False, P=None, hooks=None):
    standalone = P is None
    if standalone:
        P = Prog()
    hooks = hooks or {}
    nc = P.nc
    TT = CL + S
    NCH = TT // CH
    segs = [(0, CL, 1), (CL, S, 0)]
    di = lambda n, s, dt=F32: P.dram(n, s, dt, kind="ExternalInput")
    xT = di("xT", [D, S]); ctxT = di("ctxT", [D, CL])
    cT = hooks["cT"] if "cT" in hooks else di("cT", [128, 8, 2])
    modw = di("modw", [D, 2048]); modbc = di("modbc", [128, 16]); n1c = di("n1c", [128, 8])
    wsel = di("wsel", [D, 1664]); mup = di("mup", [1, 1536]); mun = di("mun", [1, 1536])
    vecs = di("vecs", [128, 3, 5])
    w0a0 = di("w0a0", [128, 2, 2, 3])
    w2d = di("w2", [128, 384]); a2d = di("a2", [128, 384]); g2d = di("g2", [128, 384])
    wod = di("wo", [128, 4, D])
    mAT = di("mAT", [2, 128, 128]); mN = di("mN", [2, 64, 64]); id64 = di("id64", [64, 64]); mAK = di("mAK", [2, 128, 64])
    rmaskd = di("rmask", [128, 512]); bonesd = di("bones", [128, 128]); onesDd = di("onesD", [128, 128])
    W64d = di("W64", [2, 128, 256])
    tabA = di("tabA", [S, 128]); tabB = di("tabB", [S, S // 128]); ctxCS = di("ctxCS", [2, CL, CL])
    if "out_dst" not in hooks:
        pa_out = P.dram("pa", [TT, D], F32, kind="ExternalOutput")
        hooks["out_dst"] = lambda tile: pa_out[tile * 128:(tile + 1) * 128, :]
    sk = "ExternalOutput" if dbg else "Internal"
    hT_s = [P.dram("hT_s%d" % i, [128, 8, ln + 2], BF16, kind=sk) for i, (_, ln, _) in enumerate(segs)]
    A_s = [P.dram("A_s%d" % d, [384, TT], F32, kind=sk) for d in range(2)]
    R_s = [P.dram("R_s%d" % d, [384, TT], F32, kind=sk) for d in range(2)]
    B_s = [P.dram("B_s%d" % d, [384, TT], F32, kind=sk) for d in range(2)]
    K_s = [P.dram("K_s%d" % d, [384, TT], F32, kind=sk) for d in range(2)]
    eL_s = [P.dram("eL_s%d" % d, [384, NCH], F32, kind=sk) for d in range(2)]
    V_s = P.dram("V_s", [TT, 384], F32, kind=sk)
    g_s = P.dram("g_s", [384, TT], F32, kind=sk)
    bon_s = P.dram("bon_s", [384, TT], F32, kind=sk)
    Y_s = [P.dram("Y_s%d" % d, [TT, 384], F32, kind=sk) for d in range(2)]

    ps = [P.ps("ps%d" % i, [128, 512]) for i in range(8)]

    def ld(name, src, shape, dt=F32, view=None):
        t = P.sb(name, shape, dt)
        P.dma("sp", t[:], src[:] if view is None else view)
        return t
    c_sb = ld("c_sb", cT, [128, 8, 2]); modb_s = ld("modb_s", modbc, [128, 16]); n1_s = ld("n1_s", n1c, [128, 8])
    vec_s = ld("vec_s", vecs, [128, 3, 5]); wa_s = ld("wa_s", w0a0, [128, 2, 2, 3])
    w2_s = ld("w2_s", w2d, [128, 384]); a2_s = ld("a2_s", a2d, [128, 384]); g2_s = ld("g2_s", g2d, [128, 384])
    bones_s = ld("bones_s", bonesd, [128, 128]); onesD_s = ld("onesD_s", onesDd, [128, 128])
    rmask_s = ld("rmask_s", rmaskd, [128, 512])
    omk = P.sb("omk", [128, 3])
    P.ts("dve", omk[:], vec_s[:, :, 1], -1.0, ALU.mult, 1.0, ALU.add)
    sc_sb = P.sb("sc_sb", [128, 8, 2])
    P.act(sc_sb[:], c_sb[:], AF.Silu)
    W64f = P.sb("W64f", [128, 2, 256])
    P.dma("sp", W64f[:], V(W64d, W64d.ap.rearrange("s p n -> p s n")))
    W64b = P.sb("W64b", [128, 2, 256], BF16)
    P.copy("dve", W64b[:], W64f[:])
    Zs = [P.sb("Zs%d" % i, [128, ln // 128, 256], BF16) for i, (_, ln, _) in enumerate(segs)]
    zero_b = P.sb("zero_b", [128, 8, 1], BF16)
    P.memset("dve", zero_b[:], 0.0)
    FT = P.sb("FT", [128, TT], BF16)

    m_w = P.mark()
    modc = P.sb("modc", [128, 16, 2])
    wmod = P.sb("wmod", [128, 8, 2])
    Wj = [P.sb("Wj%d" % i, [128, 8, 1536], BF16) for i in range(3)]
    Wf = P.sb("Wf", [128, 8, 128], BF16)
    m_phase = P.mark()
    xblk = P.sb("xblk", [128, 8, 512])
    hT = P.sb("hT", [128, 8, 512], BF16)
    sqb = [P.sb("sqb%d" % i, [128, 512]) for i in range(2)]
    rstd = P.sb("rstd", [128, 512])
    tmpf = [P.sb("tmpf%d" % i, [128, 512]) for i in range(2)]
    modw_v = modw.ap.rearrange("(k p) n -> p k n", p=128)
    for c4 in range(4):
        P.dma("sp", xblk[:], V(modw, modw_v[:, :, c4 * 512:(c4 + 1) * 512]))
        for q4 in range(4):
            cc = c4 * 4 + q4
            pr = ps[cc % 2]
            for k in range(8):
                P.mm(pr[:, 0:2], xblk[:, k, q4 * 128:(q4 + 1) * 128], sc_sb[:, k, :], start=(k == 0), stop=(k == 7))
            P.ts("dve", modc[:, cc, :], pr[:, 0:2], modb_s[:, cc:cc + 1], ALU.add)
    P.op("dve", lambda: nc.vector.scalar_tensor_tensor(out=wmod.ap, in0=modc.ap[:, 8:16, :], scalar=1.0,
                                                       in1=n1_s.ap.unsqueeze(2).broadcast_to([128, 8, 2]),
                                                       op0=ALU.add, op1=ALU.mult), [modc[:], n1_s[:]], [wmod[:]])
    mk_mu = P.mark()
    mu_r = [P.sb("mu_r%d" % i, [128, 1536]) for i in range(3)]
    P.dma("sp", mu_r[1][:], V(mup, mup.ap.partition_broadcast(128)))
    P.dma("sp", mu_r[2][:], V(mun, mun.ap.partition_broadcast(128)))
    P.tt("dve", mu_r[0][:], mu_r[1][:], mu_r[2][:], ALU.add)
    P.ts("dve", mu_r[0][:], mu_r[0][:], -1.0, ALU.mult, 1.0, ALU.add)
    wsel_v = wsel.ap.rearrange("(k p) n -> p k n", p=128)
    for c3 in range(3):
        P.dma("sp", xblk[:], V(wsel, wsel_v[:, :, c3 * 512:(c3 + 1) * 512]))
        for j in range(3):
            mb = mu_r[j].ap[:, c3 * 512:(c3 + 1) * 512].unsqueeze(1).broadcast_to([128, 8, 512])
            P.op("dve" if j != 1 else "pool",
                 (lambda j=j, mb=mb: P.e["dve" if j != 1 else "pool"].tensor_tensor(
                     out=Wj[j].ap[:, :, c3 * 512:(c3 + 1) * 512], in0=xblk.ap, in1=mb, op=ALU.mult)),
                 [xblk[:], mu_r[j][:]], [Wj[j][:]])
    P.dma("sp", xblk[:, :, 0:128], V(wsel, wsel_v[:, :, 1536:1664]))
    P.copy("dve", Wf[:], xblk[:, :, 0:128])
    P.barrier()
    P.release(mk_mu)

    for si, (tok0, ln, cls) in enumerate(segs):
        src = ctxT if si == 0 else xT
        P.dma("sp", hT_s[si][:, :, 0:1], zero_b[:], allow_slow_non_contiguous=True)
        P.dma("sp", hT_s[si][:, :, ln + 1:ln + 2], zero_b[:], allow_slow_non_contiguous=True)
        for c0 in range(0, ln, 512):
            w = min(512, ln - c0)
            P.dma("sp", xblk[:, :, 0:w], V(src, src.ap.rearrange("(k p) t -> p k t", p=128)[:, :, c0:c0 + w]))
            pr = ps[0]
            for k in range(8):
                sq = sqb[k % 2]
                P.act(sq[:, 0:w], xblk[:, k, 0:w], AF.Square)
                P.mm(pr[:, 0:w], onesD_s[:], sq[:, 0:w], start=(k == 0), stop=(k == 7))
            P.rsqrt(rstd[:, 0:w], pr[:, 0:w], 1.0 / D, 1e-6)
            for k in range(8):
                tf = tmpf[k % 2]
                P.tt("dve", tf[:, 0:w], xblk[:, k, 0:w], rstd[:, 0:w], ALU.mult)
                P.ts("pool", hT[:, k, 0:w], tf[:, 0:w], wmod[:, k, cls:cls + 1], ALU.mult, modc[:, k, cls:cls + 1], ALU.add)
            P.dma("sp", hT_s[si][:, :, 1 + c0:1 + c0 + w], hT[:, :, 0:w])
    if upto < 1:
        P.finish()
        return P
    P.barrier()
    P.release(m_phase)

    hwin = P.sb("hwin", [128, 8, 514], BF16)
    zt_ = {nm: [P.sb("z%s%d" % (nm, hp), [128, 512]) for hp in range(3)] for nm in ("r", "k", "v")}
    tw = P.sb("tw", [128, 512]); xa_s = P.sb("xa_s", [128, 512]); sg = P.sb("sg", [128, 512])
    fTb = P.sb("fTb", [128, 512], BF16)
    NTMP = 14
    tp = [P.sb("tp%d" % i, [128, 512]) for i in range(NTMP)]
    eLt = [P.sb("eLt%d" % i, [128, 8]) for i in range(2)]
    vst = [P.sb("vst%d" % i, [128, 128]) for i in range(2)]
    SHIFTS = ((0, 0), (1, -1), (2, 1))
    for si, (tok0, ln, cls) in enumerate(segs):
        for c0 in range(0, ln, 512):
            n = min(512, ln - c0)
            ncb = n // CH
            g0 = tok0 + c0
            P.dma("sp", hwin[:, :, 0:n + 2], hT_s[si][:, :, c0:c0 + n + 2])
            pi = [0]

            def nps():
                pi[0] = (pi[0] + 1) % 8
                return ps[pi[0]]

            def proj(col0, shifted=True):
                pr = nps()
                sh = SHIFTS if shifted else ((None, 0),)
                nmm = len(sh) * 8
                i = 0
                for (j, dj) in sh:
                    for k in range(8):
                        wt = Wj[j][:, k, col0:col0 + 128] if shifted else Wf[:, k, :]
                        P.mm(pr[:, 0:n], wt, hwin[:, k, 1 + dj:1 + dj + n], start=(i == 0), stop=(i == nmm - 1))
                        i += 1
                return pr
            for hp in range(3):
                P.copy("act", zt_["r"][hp][:, 0:n], proj(hp * 128)[:, 0:n])
                P.copy("dve", zt_["k"][hp][:, 0:n], proj(384 + hp * 128)[:, 0:n])
                P.copy("act", zt_["v"][hp][:, 0:n], proj(768 + hp * 128)[:, 0:n])
            P.act(tw[:, 0:n], proj(1152)[:, 0:n], AF.Tanh)
            P.copy("dve", xa_s[:, 0:n], proj(1280)[:, 0:n])
            P.act(sg[:, 0:n], proj(1408)[:, 0:n], AF.Sigmoid)
            P.copy("act", fTb[:, 0:n], proj(0, shifted=False)[:, 0:n])
            for tt_ in range(n // 128):
                pr = nps()
                P.mm(pr[:, 0:256], fTb[:, tt_ * 128:(tt_ + 1) * 128], W64b[:, si, :])
                P.copy("dve", Zs[si][:, c0 // 128 + tt_, :], pr[:, 0:256])
            for hp in range(3):
                zr, zk, zv = zt_["r"][hp], zt_["k"][hp], zt_["v"][hp]
                rows = slice(hp * 128, (hp + 1) * 128)
                cols = slice(g0, g0 + n)
                kkc, kac, rkc = vec_s[:, hp, 0:1], vec_s[:, hp, 1:2], vec_s[:, hp, 2:3]
                pr = nps()
                P.mm(pr[:, 0:n], g2_s[:, hp * 128:(hp + 1) * 128], sg[:, 0:n])
                P.copy("act", tp[0][:, 0:n], pr[:, 0:n])
                P.dma("sp", g_s[rows, cols], tp[0][:, 0:n])
                kk, kkn, aneg = tp[1], tp[2], tp[3]
                P.ts("dve", kk[:, 0:n], zk[:, 0:n], kkc, ALU.mult)
                P.act(tp[4][:, 0:n], kk[:, 0:n], AF.Square)
                pr = nps()
                P.mm(pr[:, 0:n], bones_s[:], tp[4][:, 0:n])
                P.act(tp[4][:, 0:n], pr[:, 0:n], AF.Sqrt)
                P.ts("dve", tp[4][:, 0:n], tp[4][:, 0:n], 1e-12, ALU.max)
                P.op("dve", lambda: nc.vector.reciprocal(out=tp[4].ap[:, 0:n], in_=tp[4].ap[:, 0:n]), [tp[4][:]], [tp[4][:]])
                P.tt("dve", kkn[:, 0:n], kk[:, 0:n], tp[4][:, 0:n], ALU.mult)
                P.ts("pool", aneg[:, 0:n], kkn[:, 0:n], -1.0, ALU.mult)
                bon = tp[5]
                for d in range(2):
                    dl = slice(d * 64, (d + 1) * 64)
                    lw, ag, kd, bb, F_, L_, Lx = tp[6], tp[7], tp[8], tp[9], tp[10], tp[11], tp[12]
                    pr = nps()
                    P.mm(pr[:, 0:n], w2_s[dl, hp * 128:(hp + 1) * 128], tw[dl, 0:n])
                    P.act(lw[:, 0:n], pr[:, 0:n], AF.Sigmoid, bias=wa_s[:, 0, d, hp:hp + 1])
                    P.ts("dve", lw[:, 0:n], lw[:, 0:n], NEG_EXP_HALF, ALU.mult)
                    pr = nps()
                    P.mm(pr[:, 0:n], a2_s[dl, hp * 128:(hp + 1) * 128], xa_s[dl, 0:n])
                    P.act(ag[:, 0:n], pr[:, 0:n], AF.Sigmoid, bias=wa_s[:, 1, d, hp:hp + 1])
                    P.ts("dve", kd[:, 0:n], ag[:, 0:n], kac, ALU.mult, omk[:, hp:hp + 1], ALU.add)
                    P.tt("dve", kd[:, 0:n], kd[:, 0:n], zk[:, 0:n], ALU.mult)
                    P.tt("pool", bb[:, 0:n], kkn[:, 0:n], ag[:, 0:n], ALU.mult)
                    P.stt("dve", tp[13][:, 0:n], zr[:, 0:n], rkc, kd[:, 0:n], ALU.mult, ALU.mult)
                    pr = nps()
                    P.mm(pr[:, 0:n], bones_s[:], tp[13][:, 0:n])
                    if d == 0:
                        P.tt("dve", bon[:, 0:n], pr[:, 0:n], zv[:, 0:n], ALU.mult)
                    else:
                        P.tt("dve", tp[13][:, 0:n], pr[:, 0:n], zv[:, 0:n], ALU.mult)
                        P.tt("pool", bon[:, 0:n], bon[:, 0:n], tp[13][:, 0:n], ALU.add)
                        P.dma("sp", bon_s[rows, cols], bon[:, 0:n])
                    P.op("dve", lambda: nc.vector.tensor_tensor_scan(out=F_.ap[:, 0:n], data0=rmask_s.ap[:, 0:n], data1=lw.ap[:, 0:n],
                                                                     initial=0.0, op0=ALU.mult, op1=ALU.add),
                         [rmask_s[:], lw[:]], [F_[:]])
                    if d == 0:
                        P.tt("pool", Lx[:, 0:n], F_[:, 0:n], lw[:, 0:n], ALU.subtract)
                        Lsrc = F_
                    else:
                        P.tt("dve", Lx[:, 0:n], lw[:, 0:n], F_[:, 0:n], ALU.subtract)
                        f3 = F_.ap[:, 0:n].rearrange("p (c t) -> p c t", t=CH)
                        l3 = L_.ap[:, 0:n].rearrange("p (c t) -> p c t", t=CH)
                        x3 = Lx.ap[:, 0:n].rearrange("p (c t) -> p c t", t=CH)
                        tot = f3[:, :, CH - 1:CH].broadcast_to([128, ncb, CH])
                        P.op("dve", (lambda l3=l3, x3=x3, tot=tot: nc.vector.tensor_tensor(out=l3, in0=x3, in1=tot, op=ALU.add)),
                             [Lx[:], F_[:]], [L_[:]])
                        P.tt("pool", Lx[:, 0:n], L_[:, 0:n], lw[:, 0:n], ALU.subtract)
                        Lsrc = L_
                    e1, e2, e3 = tp[13], tp[6], tp[10] if d == 1 else tp[11]
                    P.act(e1[:, 0:n], Lx[:, 0:n], AF.Exp)
                    P.act(e3[:, 0:n], Lsrc[:, 0:n], AF.Exp, scale=-1.0)
                    P.act(e2[:, 0:n], Lsrc[:, 0:n], AF.Exp)
                    elt = eLt[d]
                    e23 = e2.ap[:, 0:n].rearrange("p (c t) -> p c t", t=CH)
                    pos = CH - 1 if d == 0 else 0
                    P.op("pool", (lambda elt=elt, e23=e23, pos=pos: nc.gpsimd.tensor_copy(out=elt.ap[:, 0:ncb], in_=e23[:, :, pos])),
                         [e2[:]], [elt[:]])
                    P.dma("sp", eL_s[d][rows, g0 // CH:g0 // CH + ncb], elt[:, 0:ncb])
                    P.tt("dve", e1[:, 0:n], e1[:, 0:n], aneg[:, 0:n], ALU.mult)
                    P.dma("sp", A_s[d][rows, cols], e1[:, 0:n])
                    P.tt("pool", e2[:, 0:n], e2[:, 0:n], zr[:, 0:n], ALU.mult)
                    P.dma("sp", R_s[d][rows, cols], e2[:, 0:n])
                    P.tt("dve", bb[:, 0:n], bb[:, 0:n], e3[:, 0:n], ALU.mult)
                    P.dma("sp", B_s[d][rows, cols], bb[:, 0:n])
                    P.tt("pool", kd[:, 0:n], kd[:, 0:n], e3[:, 0:n], ALU.mult)
                    P.dma("sp", K_s[d][rows, cols], kd[:, 0:n])
                for tt_ in range(n // 128):
                    pr = nps()
                    i = 0
                    for (j, dj) in SHIFTS:
                        for k in range(8):
                            P.mm(pr[:, 0:128], hwin[:, k, 1 + dj + tt_ * 128:1 + dj + tt_ * 128 + 128],
                                 Wj[j][:, k, 768 + hp * 128:768 + (hp + 1) * 128], start=(i == 0), stop=(i == 23))
                            i += 1
                    vs_ = vst[tt_ % 2]
                    P.copy("act", vs_[:], pr[:, 0:128])
                    P.dma("sp", V_s[g0 + tt_ * 128:g0 + (tt_ + 1) * 128, hp * 128:(hp + 1) * 128], vs_[:])
    if upto < 2:
        P.finish()
        return P
    P.barrier()
    P.release(m_w)

    m_s = P.mark()
    ncc = CL // CH
    order = [list(range(NCH)), list(range(ncc - 1, -1, -1)) + list(range(NCH - 1, ncc - 1, -1))]
    mAT_s = P.sb("mAT_s", [128, 2, 128]); mN_s = P.sb("mN_s", [64, 2, 64]); id_s = P.sb("id_s", [64, 64])
    P.dma("sp", mAT_s[:], V(mAT, mAT.ap.rearrange("d p n -> p d n")))
    P.dma("sp", mN_s[:], V(mN, mN.ap.rearrange("d p n -> p d n")))
    P.dma("sp", id_s[:], id64[:])
    mAK_s = P.sb("mAK_s", [128, 2, 64])
    P.dma("sp", mAK_s[:], V(mAK, mAK.ap.rearrange("d p n -> p d n")))
    kj = lambda t_: t_.ap.rearrange("(j k) t -> k j t", k=64)
    sbd = lambda nm, shp: [[P.sb("%s_%d_%d" % (nm, d, i), shp) for i in range(2)] for d in range(2)]
    AR = sbd("AR", [128, 6, 2, 64]); BK = sbd("BK", [64, 6, 2, 64]); UV = sbd("UV", [128, 6, 64])
    for d in range(2):
        for i in range(2):
            P.memset("pool", AR[d][i][:], 0.0)
            P.memset("pool", UV[d][i][:], 0.0)
    eLa = [P.sb("eLa%d" % d, [64, 6, NCH]) for d in range(2)]
    for d in range(2):
        P.dma("sp", eLa[d][:], V(eL_s[d], kj(eL_s[d])))
    Xb = sbd("Xb", [64, 6, 64]); Zb = sbd("Zb", [64, 6, 64])
    one = lambda nm, shp: [P.sb("%s_%d" % (nm, d), shp) for d in range(2)]
    ATs = one("ATs", [128, 6, 128]); Rm = one("Rm", [64, 6, 64]); BKt = one("BKt", [128, 6, 64])
    W0s = one("W0s", [64, 6, 64]); Ys = one("Ys", [64, 6, 64]); tS = one("tS", [64, 6, 64]); ST = one("ST", [128, 6, 64])
    AakP = one("AakP", [128, 6, 64])
    for d in range(2):
        P.memset("pool", ST[d][:], 0.0)
    ring = [0, 0]

    def nb(d):
        ring[d] = (ring[d] + 1) % 4
        return ps[d * 4 + ring[d]]

    def loads(n):
        for d in range(2):
            c = order[d][n]
            cs = slice(c * CH, (c + 1) * CH)
            pb = n % 2
            P.dma("sp", AR[d][pb][0:64, :, 0, :], V(A_s[d], kj(A_s[d])[:, :, cs]))
            P.dma("sp", AR[d][pb][0:64, :, 1, :], V(R_s[d], kj(R_s[d])[:, :, cs]))
            P.dma("sp", BK[d][pb][:, :, 0, :], V(B_s[d], kj(B_s[d])[:, :, cs]))
            P.dma("sp", BK[d][pb][:, :, 1, :], V(K_s[d], kj(K_s[d])[:, :, cs]))
            P.dma("sp", UV[d][pb][64:128, :, :], V(V_s, V_s.ap[cs, :].rearrange("t (j v) -> t j v", v=64)))

    def v3(t_, rows=slice(0, 64), width=384):
        return V(t_, t_.ap[rows, 0:width].rearrange("p (j v) -> p j v", j=6))

    loads(0)
    for n in range(NCH):
        pb = n % 2
        if n + 1 < NCH:
            loads(n + 1)
        ar = [AR[d][pb] for d in range(2)]; bk = [BK[d][pb] for d in range(2)]; uv = [UV[d][pb] for d in range(2)]
        for d in range(2):
            for g in range(2):
                pa = nb(d)
                for jj in range(3):
                    j = g * 3 + jj
                    P.mm(pa[:, jj * 128:(jj + 1) * 128], V(bk[d], bk[d].ap[:, j].rearrange("p a i -> p (a i)")),
                         V(ar[d], ar[d].ap[0:64, j].rearrange("p a i -> p (a i)")))
                mk_ = mAK_s.ap[:, d, :].unsqueeze(1).broadcast_to([128, 3, 64])
                P.op("dve", (lambda pa=pa, d=d, g=g, mk_=mk_: nc.vector.tensor_tensor(
                    out=AakP[d].ap[:, g * 3:(g + 1) * 3, :], in0=pa.ap[:, 0:384].rearrange("p (j n) -> p j n", j=3)[:, :, 0:64], in1=mk_, op=ALU.mult)),
                    [pa[:], mAK_s[:]], [AakP[d][:]])
                mb = mAT_s.ap[:, d, :].unsqueeze(1).broadcast_to([128, 3, 128])
                P.op("dve", (lambda pa=pa, d=d, g=g, mb=mb: nc.vector.tensor_tensor(
                    out=ATs[d].ap[:, g * 3:(g + 1) * 3, :], in0=pa.ap[:, 0:384].rearrange("p (j n) -> p j n", j=3), in1=mb, op=ALU.mult)),
                    [pa[:], mAT_s[:]], [ATs[d][:]])
        pn = [nb(d) for d in range(2)]
        for d in range(2):
            for j in range(6):
                P.mm(pn[d][0:64, j * 64:(j + 1) * 64], ar[d][0:64, j, 0, :], bk[d][:, j, 0, :])
            mb = mN_s.ap[:, d, :].unsqueeze(1).broadcast_to([64, 6, 64])
            P.op("dve", (lambda d=d, mb=mb: nc.vector.tensor_tensor(out=Xb[d][0].ap, in0=pn[d].ap[0:64, 0:384].rearrange("p (j n) -> p j n", j=6),
                                                                    in1=mb, op=ALU.mult)), [pn[d][:], mN_s[:]], [Xb[d][0][:]])
            ib = id_s.ap.unsqueeze(1).broadcast_to([64, 6, 64])
            P.op("pool", (lambda d=d, ib=ib: nc.gpsimd.tensor_tensor(out=Rm[d].ap, in0=ATs[d].ap[0:64, :, 0:64], in1=ib, op=ALU.add)),
                 [ATs[d][:], id_s[:]], [Rm[d][:]])
        pk = [nb(d) for d in range(2)]
        for d in range(2):
            for j in range(6):
                P.mm(pk[d][:, j * 64:(j + 1) * 64], V(bk[d], bk[d].ap[:, j].rearrange("p a i -> p (a i)")), id_s[:])
            P.copy("act", BKt[d][:], v3(pk[d], slice(0, 128)))
        Xp = [Xb[d][0] for d in range(2)]
        Zp = [V(ATs[d], ATs[d].ap[0:64, :, 0:64]) for d in range(2)]
        for lv in range(1, 6):
            px = [nb(d) for d in range(2)]
            for d in range(2):
                for j in range(6):
                    P.mm(px[d][0:64, j * 64:(j + 1) * 64], V(Zp[d].t, Zp[d].ap[:, j, :]), Xp[d][:, j, :])
            Xn = [Xb[d][lv % 2] for d in range(2)]
            for d in range(2):
                P.copy("act", Xn[d][:], v3(px[d]))
            if lv < 5:
                pz = [nb(d) for d in range(2)]
                for d in range(2):
                    for j in range(6):
                        P.mm(pz[d][0:64, j * 64:(j + 1) * 64], Xp[d][:, j, :], V(Zp[d].t, Zp[d].ap[:, j, :]))
                Zn = [Zb[d][lv % 2] for d in range(2)]
                for d in range(2):
                    P.copy("dve", Zn[d][:], v3(pz[d]))
            prr = [nb(d) for d in range(2)]
            for d in range(2):
                for j in range(6):
                    P.mm(prr[d][0:64, j * 64:(j + 1) * 64], Xn[d][:, j, :], Rm[d][:, j, :])
            for d in range(2):
                P.tt("dve", Rm[d][:], v3(prr[d]), Rm[d][:], ALU.add)
            Xp = Xn
            if lv < 5:
                Zp = [Zn[d][:] for d in range(2)]
        pw = [nb(d) for d in range(2)]
        for d in range(2):
            for j in range(6):
                o_ = pw[d][0:64, j * 64:(j + 1) * 64]
                P.mm(o_, ar[d][:, j, 0, :], ST[d][:, j, :], start=True, stop=False)
                P.mm(o_, AakP[d][:, j, :], uv[d][:, j, :], start=False, stop=True)
            P.copy("act", W0s[d][:], v3(pw[d]))
        pu_ = [nb(d) for d in range(2)]
        for d in range(2):
            for j in range(6):
                P.mm(pu_[d][0:64, j * 64:(j + 1) * 64], Rm[d][:, j, :], W0s[d][:, j, :])
            P.copy("dve", uv[d][0:64, :, :], v3(pu_[d]))
        py = [nb(d) for d in range(2)]
        for d in range(2):
            for j in range(6):
                o_ = py[d][0:64, j * 64:(j + 1) * 64]
                P.mm(o_, ar[d][:, j, 1, :], ST[d][:, j, :], start=True, stop=False)
                P.mm(o_, ATs[d][:, j, 64:128], uv[d][:, j, :], start=False, stop=True)
            P.copy("act", Ys[d][:], v3(py[d]))
            c = order[d][n]
            P.dma("sp", V(Y_s[d], Y_s[d].ap[c * CH:(c + 1) * CH, :].rearrange("t (j v) -> t j v", v=64)), Ys[d][:])
        pst = [nb(d) for d in range(2)]
        for d in range(2):
            for j in range(6):
                P.mm(pst[d][0:64, j * 64:(j + 1) * 64], BKt[d][:, j, :], uv[d][:, j, :])
            P.tt("dve", tS[d][:], v3(pst[d]), ST[d][0:64, :, :], ALU.add)
            eb = eLa[d].ap[:, :, order[d][n]].unsqueeze(2).broadcast_to([64, 6, 64])
            P.op("pool", (lambda d=d, eb=eb: nc.gpsimd.tensor_tensor(out=ST[d].ap[0:64], in0=tS[d].ap, in1=eb, op=ALU.mult)),
                 [tS[d][:], eLa[d][:]], [ST[d][:]])
    if upto < 3:
        P.finish()
        return P
    P.barrier()
    P.release(m_s)

    m_n = P.mark()
    import math
    ccs = P.sb("ccs", [128, CL // 128, 2, CL], BF16)
    cstage = P.sb("cstage", [128, CL // 128, 2, CL])
    for c_ in range(2):
        P.dma("sp", cstage[:, :, c_, :], V(ctxCS, ctxCS.ap[c_].rearrange("(a p) n -> p a n", p=128)))
    P.copy("dve", ccs[:], cstage[:])
    pr = ps[0]
    ntc = CL // 128
    for ti in range(ntc):
        P.mm(pr[:, 0:CL], Zs[0][:, ti, 0:128], ccs[:, ti, 0, :], start=(ti == 0), stop=False)
        P.mm(pr[:, 0:CL], Zs[0][:, ti, 128:256], ccs[:, ti, 1, :], start=False, stop=(ti == ntc - 1))
    P.copy("act", FT[:, 0:CL], pr[:, 0:CL])
    NTT = S // 128
    PW = min(S, 2048)
    HP = PW // 128
    NBLK = PW // 512
    tA = P.sb("tA", [128, NTT, 128]); tB = P.sb("tB", [128, NTT, S // 128])
    P.dma("sp", tA[:], V(tabA, tabA.ap.rearrange("(a p) n -> p a n", p=128)))
    P.dma("sp", tB[:], V(tabB, tabB.ap.rearrange("(a p) n -> p a n", p=128)))
    negpi = P.sb("negpi", [128, 2])
    SHR = 1.0 - 1e-6
    P.memset("dve", negpi[:, 0:1], -math.pi * SHR)
    P.memset("dve", negpi[:, 1:2], -0.5 * math.pi * SHR)
    kang = 2.0 * math.pi / S * SHR
    mm_ = [P.sb("mm_%d" % i, [128, PW]) for i in range(2)]
    mc_ = [P.sb("mc_%d" % i, [128, PW]) for i in range(2)]
    Ct = [P.sb("Ct%d" % i, [128, PW], BF16) for i in range(2)]
    St = [P.sb("St%d" % i, [128, PW], BF16) for i in range(2)]
    for pz_ in range(S // PW):
        for ti in range(NTT):
            b2 = ti % 2
            m3 = mm_[b2].ap.rearrange("p (h l) -> p h l", l=128)
            i0 = tB.ap[:, ti, pz_ * HP:(pz_ + 1) * HP].unsqueeze(2).broadcast_to([128, HP, 128])
            i1 = tA.ap[:, ti, :].unsqueeze(1).broadcast_to([128, HP, 128])
            P.op("dve", (lambda m3=m3, i0=i0, i1=i1: nc.vector.tensor_tensor(out=m3, in0=i0, in1=i1, op=ALU.add)),
                 [tA[:], tB[:]], [mm_[b2][:]])
            P.ts("dve", mc_[b2][:], mm_[b2][:], float(S), ALU.is_ge, -float(S), ALU.mult)
            P.tt("pool", mm_[b2][:], mm_[b2][:], mc_[b2][:], ALU.add)
            P.act(St[b2][:], mm_[b2][:], AF.Sin, bias=negpi[:, 0:1], scale=kang)
            P.ts("dve", mc_[b2][:], mm_[b2][:], 0.75 * S, ALU.is_ge, -float(S), ALU.mult)
            P.tt("pool", mc_[b2][:], mc_[b2][:], mm_[b2][:], ALU.add)
            P.act(Ct[b2][:], mc_[b2][:], AF.Sin, bias=negpi[:, 1:2], scale=kang)
            for blk in range(NBLK):
                P.mm(ps[blk][:], Zs[1][:, ti, 0:128], Ct[b2][:, blk * 512:(blk + 1) * 512], start=(ti == 0), stop=False)
                P.mm(ps[blk][:], Zs[1][:, ti, 128:256], St[b2][:, blk * 512:(blk + 1) * 512], start=False, stop=(ti == NTT - 1))
        for blk in range(NBLK):
            c0 = CL + pz_ * PW + blk * 512
            P.copy("act" if blk % 2 == 0 else "dve", FT[:, c0:c0 + 512], ps[blk][:])
    if upto < 4:
        P.finish()
        return P
    P.barrier()
    P.release(m_n)

    wo_s = P.sb("wo_s", [128, 4, D], BF16)
    wstage = P.sb("wstage", [128, 4, D])
    P.dma("sp", wstage[:], wod[:])
    P.copy("dve", wo_s[:], wstage[:])
    yf = [P.sb("yf%d" % i, [128, 384]) for i in range(2)]
    yb = [P.sb("yb%d" % i, [128, 384]) for i in range(2)]
    dd = P.sb("dd", [128, 384]); sq = P.sb("sq", [128, 384]); st6 = P.sb("st6", [128, 6]); st6b = P.sb("st6b", [128, 6])
    idf = P.sb("idf", [128, 128])
    P.dma("sp", idf[0:64, 0:64], id64[:])
    bonT = [P.sb("bonT%d" % i, [128, 512]) for i in range(3)]
    gT = [P.sb("gT%d" % i, [128, 512]) for i in range(3)]
    oT = [P.sb("oT%d" % i, [128, 512], BF16) for i in range(3)]
    ot32 = P.sb("ot32", [128, 512])
    ostg = [P.sb("ostg%d" % i, [128, D]) for i in range(2)]
    P.memset("dve", idf[0:64, 64:128], 0.0)
    P.memset("dve", idf[64:128, 0:64], 0.0)
    P.dma("sp", idf[64:128, 64:128], id64[:])
    for (tok0, ln, cls) in segs:
        for c0 in range(0, ln, 512):
            n = min(512, ln - c0)
            g0 = tok0 + c0
            for hp in range(3):
                P.dma("sp", bonT[hp][:, 0:n], bon_s[hp * 128:(hp + 1) * 128, g0:g0 + n])
                P.dma("sp", gT[hp][:, 0:n], g_s[hp * 128:(hp + 1) * 128, g0:g0 + n])
            for tt_ in range(n // 128):
                r0 = g0 + tt_ * 128
                a_, b_ = yf[tt_ % 2], yb[tt_ % 2]
                P.dma("sp", a_[:], Y_s[0][r0:r0 + 128, :])
                P.dma("sp", b_[:], Y_s[1][r0:r0 + 128, :])
                P.tt("pool", a_[:], a_[:], b_[:], ALU.add)
                a3 = a_.ap.rearrange("p (j v) -> p j v", v=64)
                d3 = dd.ap.rearrange("p (j v) -> p j v", v=64)
                P.op("dve", (lambda a3=a3: nc.vector.tensor_reduce(out=st6.ap, in_=a3, axis=AX.X, op=ALU.add)), [a_[:]], [st6[:]])
                P.ts("dve", st6[:], st6[:], 1.0 / 64, ALU.mult)
                P.op("dve", (lambda a3=a3, d3=d3: nc.vector.tensor_tensor(out=d3, in0=a3, in1=st6.ap.unsqueeze(2).broadcast_to([128, 6, 64]),
                                                                          op=ALU.subtract)), [a_[:], st6[:]], [dd[:]])
                P.act(sq[:], dd[:], AF.Square)
                P.op("dve", lambda: nc.vector.tensor_reduce(out=st6b.ap, in_=sq.ap.rearrange("p (j v) -> p j v", v=64), axis=AX.X, op=ALU.add),
                     [sq[:]], [st6b[:]])
                P.rsqrt(st6b[:], st6b[:], 1.0 / 64, 64e-5)
                P.op("dve", (lambda d3=d3: nc.vector.tensor_tensor(out=d3, in0=d3, in1=st6b.ap.unsqueeze(2).broadcast_to([128, 6, 64]),
                                                                   op=ALU.mult)), [dd[:], st6b[:]], [dd[:]])
                for hp in range(3):
                    P.tr(ps[hp][:, tt_ * 128:(tt_ + 1) * 128], dd[:, hp * 128:(hp + 1) * 128], idf[:])
            for hp in range(3):
                P.ts("dve", ot32[:, 0:n], ps[hp][:, 0:n], vec_s[:, hp, 3:4], ALU.mult, vec_s[:, hp, 4:5], ALU.add)
                P.tt("pool", ot32[:, 0:n], ot32[:, 0:n], bonT[hp][:, 0:n], ALU.add)
                P.tt("dve", oT[hp][:, 0:n], ot32[:, 0:n], gT[hp][:, 0:n], ALU.mult)
            for tt_ in range(n // 128):
                tsl = slice(tt_ * 128, (tt_ + 1) * 128)
                og = ostg[tt_ % 2]
                for ch in range(2):
                    pw_ = ps[4 + ch]
                    for hp in range(3):
                        P.mm(pw_[:], oT[hp][:, tsl], wo_s[:, hp, ch * 512:(ch + 1) * 512], start=(hp == 0), stop=False)
                    P.mm(pw_[:], FT[:, g0 + tt_ * 128:g0 + (tt_ + 1) * 128], wo_s[:, 3, ch * 512:(ch + 1) * 512], start=False, stop=True)
                    P.copy("act" if ch == 0 else "dve", og[:, ch * 512:(ch + 1) * 512], pw_[:])
                P.dma("sp", hooks["out_dst"](g0 // 128 + tt_), og[:])
    if standalone:
        P.finish()
    return P


def l0_consts(mod_w, mod_b, norm1, w_in, shift_prev, shift_next, w0, w2, a0, a2, g2, k_k, k_a, r_k, lnx_g, lnx_b,
              w_out, s, S, CL):
    f = np.float32
    c384 = slice(384 * s, 384 * (s + 1))
    sel = np.concatenate([np.arange(384 * s, 384 * (s + 1)), 768 + np.arange(384 * s, 384 * (s + 1)),
                          1536 + np.arange(384 * s, 384 * (s + 1)), np.arange(2304, 2688)])
    fsel = 2688 + np.arange(128 * s, 128 * (s + 1))
    wsel = np.ascontiguousarray(np.concatenate([w_in[:, sel], w_in[:, fsel]], 1))
    ch = lambda v: np.asarray(v, f).reshape(-1)[c384].reshape(3, 128).T
    vecs = np.ascontiguousarray(np.stack([ch(k_k), ch(k_a), ch(r_k), ch(lnx_g), ch(lnx_b)], -1))
    w0a0 = np.zeros((128, 2, 2, 3), f)
    for d in range(2):
        w0a0[:, 0, d, :] = ch(w0[d])
        w0a0[:, 1, d, :] = ch(a0[d])
    wo = np.zeros((128, 4, D), f)
    wo[:, 0:3, :] = w_out[c384].reshape(3, 128, D).transpose(1, 0, 2)
    wo[:, 3, :] = w_out[768 + 128 * s:768 + 128 * (s + 1)]
    ii = np.arange(64)
    mAT = np.zeros((2, 128, 128), f)
    mN = np.zeros((2, 64, 64), f)
    mAK = np.zeros((2, 128, 64), f)
    for d in range(2):
        before = (ii[:, None] < ii[None, :]) if d == 0 else (ii[:, None] > ii[None, :])
        beq = before | (ii[:, None] == ii[None, :])
        for r0 in (0, 64):
            mAT[d, r0:r0 + 64, 0:64] = before
            mAT[d, r0:r0 + 64, 64:128] = beq
        mN[d] = before.T
        mAK[d, 64:128, :] = before
    rmask = np.ones((128, 512), f)
    rmask[:, ::64] = 0
    bones = np.zeros((128, 128), f)
    bones[0:64, 0:64] = 1
    bones[64:, 64:] = 1
    cc = np.arange(64)
    ang = 2 * np.pi * ((cc[:, None] * cc[None, :]) % 64) / 64.0
    C64 = np.zeros((128, 128)); S64 = np.zeros((128, 128))
    for g in range(2):
        C64[g * 64:(g + 1) * 64, g * 64:(g + 1) * 64] = np.cos(ang)
        S64[g * 64:(g + 1) * 64, g * 64:(g + 1) * 64] = np.sin(ang)
    al_c = 1.0 / np.sqrt(CL * 64.0)
    al_l = 1.0 / np.sqrt(S * 64.0)
    W64 = np.stack([np.concatenate([al_c * C64, -al_c * S64], 1), np.concatenate([-al_l * C64, al_l * S64], 1)], 0).astype(f)
    t = np.arange(S, dtype=np.int64)
    tabA = ((t[:, None] * np.arange(128)[None, :]) % S).astype(f)
    tabB = ((128 * t[:, None] * np.arange(S // 128)[None, :]) % S).astype(f)
    tc = np.arange(CL, dtype=np.int64)
    angc = 2 * np.pi * ((tc[:, None] * tc[None, :]) % CL) / float(CL)
    ctxCS = np.stack([np.cos(angc), np.sin(angc)], 0).astype(f)
    return {
        "modw": np.ascontiguousarray(mod_w[:, 0:2048]), "modbc": colmat(mod_b[0:2048], 16), "n1c": colmat(norm1, 8),
        "wsel": wsel, "mup": np.ascontiguousarray(shift_prev[sel][None].astype(f)),
        "mun": np.ascontiguousarray(shift_next[sel][None].astype(f)),
        "vecs": vecs, "w0a0": w0a0,
        "w2": np.ascontiguousarray(w2[:, :, c384].reshape(128, 384)), "a2": np.ascontiguousarray(a2[:, :, c384].reshape(128, 384)),
        "g2": np.ascontiguousarray(g2[:, c384]), "wo": wo, "mAT": mAT, "mN": mN, "mAK": mAK, "id64": np.eye(64, dtype=f),
        "rmask": rmask, "bones": bones, "onesD": np.ones((128, 128), f), "W64": W64,
        "tabA": tabA, "tabB": tabB, "ctxCS": ctxCS,
    }


def l0_core_inputs(xb, ctxb, c_b, c_ctx):
    return {"xT": np.ascontiguousarray(xb.T), "ctxT": np.ascontiguousarray(ctxb.T), "cT": c_cols(c_b, c_ctx)}


def build_fused(S=8192, CL=256, n_cores=8, stop_after=None, part=None):
    P = Prog(num_devices=n_cores)
    nc = P.nc
    TT = CL + S
    H = S // 2
    HC = CL // 2
    NTL = H // 128
    NTC = HC // 128
    NT0 = NTL + NTC
    CT = CL // 128
    TILES = TT // 128
    cT = P.dram("cT", [128, 8, 2], F32, kind="ExternalInput")
    xh = P.dram("xh", [NT0 * 128, D], F32, kind="ExternalInput") if part != 2 else None
    y = P.dram("y", [H, D], F32, kind="ExternalOutput") if part != 1 else None
    mixS = P.dram("mixS", [2 * TT, D], F32, shared=True, persist=True)
    if part == 2:
        x1T = P.dram("x1T", [TILES + 1, D, 128], F32, kind="ExternalInput")
    else:
        x1T = P.dram("x1T", [TILES + 1, D, 128], F32, shared=True, persist=True)
    paS = P.dram("paS", [2 * S, D], F32, shared=True, persist=True)
    mixL = P.dram("mixL", [TT, D], F32, persist=True)
    partL = [P.dram("partL%d" % i, [NT0 * 128, D], F32, persist=True) for i in range(2)]
    io_kind = {1: "ExternalOutput", 2: "ExternalInput"}.get(part, "Internal")
    x1L = P.dram("x1L", [NT0 * 128, D], F32, kind=io_kind, persist=True)
    x1TL = P.dram("x1TL", [NT0, D, 128], F32, kind="ExternalOutput" if part == 1 else "Internal", persist=True)
    xcL = P.dram("xcL", [NTL + 2, D, 128], F32, persist=True)
    paL = P.dram("paL", [S, D], F32, persist=True)
    paP = [P.dram("paP%d" % i, [H, D], F32, persist=True) for i in range(2)]
    pcL = P.dram("pcL", [H, D], F32, persist=True)

    def parity(q):
        e = P.e[q]
        return e.snap(e.partition_id() % 2, min_val=0, max_val=1)

    def tview(t, tile, lo=0, hi=128):
        return V(t, t.ap[tile:tile + 1].rearrange("o (k p) t -> p (o k) t", p=128)[:, :, lo:hi])

    r_sp = r_act = None
    if part == 2:
        r_sp = parity("sp")
        r_act = parity("act")
    if part != 2:
      P.stage_begin("A_")
      if part is None:
          zpad = P.sb("zpad", [128, 8, 128])
          P.memset("dve", zpad[:], 0.0)
          P.dma("sp", tview(x1T, TILES), zpad[:])
      build_l0(S, CL, P=P, hooks={"cT": cT, "out_dst": lambda tile: mixL[tile * 128:(tile + 1) * 128, :]})
      r_sp = parity("sp")
      P.dma("sp", V(mixS, mixS.ap[bass.ds(r_sp * TT, TT), :]), mixL[:])
      P.stage_end()
      P.stage_begin("B_")
      r_act = parity("act")
      for i in range(2):
          P.dma("act", partL[i][0:NTL * 128, :], V(mixS, mixS.ap[bass.ds(r_act * (NTL * 128) + (i * TT + CT * 128), NTL * 128), :]))
          P.dma("act", partL[i][NTL * 128:NT0 * 128, :], V(mixS, mixS.ap[bass.ds(r_act * (NTC * 128) + i * TT, NTC * 128), :]))
      build_post(NT0, NTL, 2, False, P=P, hooks={
          "cT": cT,
          "x_src": lambda it: xh[it * 128:(it + 1) * 128, :],
          "part_src": lambda i, it: partL[i][it * 128:(it + 1) * 128, :],
          "y_dst": lambda it: x1L[it * 128:(it + 1) * 128, :],
          "yT_dst": lambda it: tview(x1TL, it),
      })
      if part == 1:
          P.stage_end(core_sync=False)
          P.finish()
          return P
      r_pool = parity("pool")
      P.dma("pool", V(x1T, x1T.ap[bass.ds(r_pool * NTL + CT, NTL)]), x1TL[0:NTL])
      P.dma("pool", V(x1T, x1T.ap[bass.ds(r_pool * NTC, NTC)]), x1TL[NTL:NT0])
      P.stage_end()
      if stop_after == "B":
          P.finish()
          return P
    P.stage_begin("C_")
    P.dma("sp", xcL[:], V(x1T, x1T.ap[bass.ds(r_sp * NTL + (CT - 1), NTL + 2)]))

    def h_src(kind, c0, w):
        if kind == "lat":
            return [(i * 128, 128, tview(x1T, CT + c0 // 128 + i)) for i in range(w // 128)]
        if kind == "ctx":
            return [(i * 128, 128, tview(x1T, c0 // 128 + i)) for i in range(w // 128)]
        t0 = 1 + c0 // 128
        return [(0, 15, tview(xcL, t0 - 1, 113, 128)), (15, 128, tview(xcL, t0)), (143, 128, tview(xcL, t0 + 1)),
                (271, 15, tview(xcL, t0 + 2, 0, 15))]
    build_l1(S, CL, H, P=P, hooks={
        "cT": cT, "h_src": h_src,
        "pa_dst": lambda tile: paL[tile * 128:(tile + 1) * 128, :],
        "pc_dst": lambda tile: pcL[tile * 128:(tile + 1) * 128, :],
    })
    P.dma("sp", V(paS, paS.ap[bass.ds(r_sp * S, S), :]), paL[:])
    P.stage_end()
    if stop_after == "C":
        P.finish()
        return P
    P.stage_begin("D_")
    for i in range(2):
        P.dma("act", paP[i][:], V(paS, paS.ap[bass.ds(r_act * H + i * S, H), :]))
    build_post(NTL, NTL, 3, True, P=P, hooks={
        "cT": cT,
        "x_src": lambda it: x1L[it * 128:(it + 1) * 128, :],
        "part_src": lambda i, it: (paP[i] if i < 2 else pcL)[it * 128:(it + 1) * 128, :],
        "y_dst": lambda it: y[it * 128:(it + 1) * 128, :],
    })
    P.stage_end(core_sync=False)
    P.finish()
    return P


_L0N = ["mod_w", "mod_b", "norm1", "w_in", "shift_prev", "shift_next", "w0", "w2", "a0", "a2", "g2", "k_k", "k_a", "r_k",
        "lnx_g", "lnx_b", "w_out"]
_L1N = ["mod_w", "mod_b", "norm1", "w_in", "q_norm", "k_norm", "dw_w", "dw_b", "cn_g", "cn_b", "w_out"]
_PROGS = {}
FUSED = False


def kernel(**inp):
    f = np.float32
    g = lambda k: np.ascontiguousarray(np.asarray(inp[k], dtype=f))
    x = g("x"); ctx = g("ctx"); c = g("c"); c_ctx = g("c_ctx")
    B, S, _ = x.shape
    CL = ctx.shape[1]
    H = S // 2
    HC = CL // 2
    NTL, NTC, CT = H // 128, HC // 128, CL // 128
    NT0 = NTL + NTC
    TILES = (S + CL) // 128
    n_cores = 2 * B
    pre = lambda p, d: {p + k: v for k, v in d.items()}

    def progs(part):
        key = (S, CL, n_cores, part)
        if key not in _PROGS:
            _PROGS[key] = build_fused(S, CL, n_cores, part=part)
        return _PROGS[key]

    def edge_of(s):
        e = np.zeros((128, 2), f)
        e[:, 0] = 1.0 if s == 1 else 0.0
        e[:, 1] = 1.0 if s == 0 else 0.0
        return e

    def in_l0():
        cons0 = [pre("A_", l0_consts(*[g("l0_" + n) for n in _L0N], s, S, CL)) for s in range(2)]
        consB = pre("B_", post_consts(g("l0_mod_w"), g("l0_mod_b"), g("l0_norm2"), g("norm_f"), g("l0_pq"), g("l0_sk1"),
                                      g("l0_sk2"), g("l0_pu"), g("l0_pv")))
        ims = []
        for b in range(B):
            xT = np.ascontiguousarray(x[b].T)
            ctxT = np.ascontiguousarray(ctx[b].T)
            for s in range(2):
                im = {}
                im.update(cons0[s]); im.update(consB)
                im["A_xT"] = xT
                im["A_ctxT"] = ctxT
                im["cT"] = c_cols(c[b], c_ctx)
                im["xh"] = np.ascontiguousarray(np.concatenate([x[b, H * s:H * (s + 1)], ctx[b, HC * s:HC * (s + 1)]], 0))
                ims.append(im)
        return ims

    def in_l1():
        cons1 = [pre("C_", l1_consts(*[g("l1_" + n) for n in _L1N], s, S)) for s in range(2)]
        consD = pre("D_", post_consts(g("l1_mod_w"), g("l1_mod_b"), g("l1_norm2"), g("norm_f"), g("l1_pq"), g("l1_sk1"),
                                      g("l1_sk2"), g("l1_pu"), g("l1_pv")))
        ims = []
        for b in range(B):
            for s in range(2):
                im = {}
                im.update(cons1[s]); im.update(consD)
                im["cT"] = c_cols(c[b], c_ctx)
                im["C_edge"] = edge_of(s)
                ims.append(im)
        return ims

    if FUSED:
        P = progs(None)
        ims = [dict(a, **b_) for a, b_ in zip(in_l0(), in_l1())]
        res = run_bass_kernel_spmd(P.nc, ims, core_ids=list(range(n_cores))).results
    else:
        r1 = run_bass_kernel_spmd(progs(1).nc, in_l0(), core_ids=list(range(n_cores))).results
        x1 = np.zeros_like(x)
        ctx1 = np.zeros_like(ctx)
        for b in range(B):
            for s in range(2):
                yb = r1[2 * b + s]["x1L"]
                x1[b, H * s:H * (s + 1)] = yb[0:H]
                ctx1[b, HC * s:HC * (s + 1)] = yb[H:H + HC]
        del r1
        key = ("l1", S, CL)
        if key not in _PROGS:
            _PROGS[key] = build_l1(S, CL, H)
        cons1 = [l1_consts(*[g("l1_" + n) for n in _L1N], s, S) for s in range(2)]
        ims = []
        for b in range(B):
            for s in range(2):
                im = dict(cons1[s])
                im.update(l1_core_inputs(x1[b], ctx1[b], c[b], c_ctx, s, H))
                ims.append(im)
        r2 = run_bass_kernel_spmd(_PROGS[key].nc, ims, core_ids=list(range(n_cores))).results
        key = ("post1", H)
        if key not in _PROGS:
            _PROGS[key] = build_post(NTL, NTL, 3, True)
        consD = post_consts(g("l1_mod_w"), g("l1_mod_b"), g("l1_norm2"), g("norm_f"), g("l1_pq"), g("l1_sk1"),
                            g("l1_sk2"), g("l1_pu"), g("l1_pv"))
        ims = []
        for b in range(B):
            for s in range(2):
                im = dict(consD)
                im["x"] = np.ascontiguousarray(x1[b, H * s:H * (s + 1)])
                im["p0"] = np.ascontiguousarray(r2[2 * b]["pa"][H * s:H * (s + 1)])
                im["p1"] = np.ascontiguousarray(r2[2 * b + 1]["pa"][H * s:H * (s + 1)])
                im["p2"] = r2[2 * b + s]["pc"]
                im["cT"] = c_cols(c[b], c_ctx)
                ims.append(im)
        res = run_bass_kernel_spmd(_PROGS[key].nc, ims, core_ids=list(range(n_cores))).results
    out = np.zeros_like(x)
    for b in range(B):
        for s in range(2):
            out[b, H * s:H * (s + 1)] = res[2 * b + s]["y"]
    return out
```
